# Optimizing a Trainium2 kernel written in Bass

```python
import jax, jax.numpy as jnp
from jax import lax
import numpy as np


D_MODEL = 1024
BATCH = 8
SEQ = 4096
DEPTH = 2

D_MIX = D_MODEL
HEAD_DIM = 64
RWKV_WIDTH = D_MIX // 2
RWKV_HEADS = RWKV_WIDTH // HEAD_DIM
POOL_WIDTH = D_MIX // 4
POOL_WINDOWS = (2, 4, 8, 16)
POOL_GROUPS = len(POOL_WINDOWS)
POOL_GROUP_DIM = POOL_WIDTH // POOL_GROUPS
MLA_WIDTH = D_MIX - RWKV_WIDTH - POOL_WIDTH
MLA_V_DIM = HEAD_DIM
MLA_HEADS = MLA_WIDTH // MLA_V_DIM
MLA_QK_NOPE = 64
MLA_QK_ROPE = 32
MLA_QK_DIM = MLA_QK_NOPE + MLA_QK_ROPE
MLA_Q_LORA = 256
MLA_KV_LORA = 128
ROPE_BASE = 10000.0
Q_BLOCK = 128
RWKV_DECAY_LORA = 64
RWKV_ICLR_LORA = 64
RWKV_VRES_LORA = 32
RWKV_GATE_LORA = 160
RWKV_LNX_EPS = 64e-5
RWKV_SPLITS = (RWKV_WIDTH, 2 * RWKV_WIDTH, 3 * RWKV_WIDTH,
               3 * RWKV_WIDTH + RWKV_DECAY_LORA,
               3 * RWKV_WIDTH + RWKV_DECAY_LORA + RWKV_ICLR_LORA)
RWKV_SHIFT_COLS = 3 * RWKV_WIDTH + RWKV_DECAY_LORA + RWKV_ICLR_LORA + RWKV_GATE_LORA
OFF_POOL = RWKV_SHIFT_COLS
OFF_QLAT = OFF_POOL + POOL_WIDTH
OFF_KVLAT = OFF_QLAT + MLA_Q_LORA
OFF_KROPE = OFF_KVLAT + MLA_KV_LORA
N_IN_BASE = OFF_KROPE + MLA_QK_ROPE
N_IN_REST = N_IN_BASE + RWKV_VRES_LORA
D_FF = 2816
N_EXPERTS = 8
TOP_K = 2
D_FF_EXPERT = 3584
MOE_BLOCK = 512
N_DENSE = (DEPTH + 1) // 2
N_MOE = DEPTH // 2
NORM_EPS = 1e-6
NEG_INF = -1e30

kernel_name = "hymba_rwkv7_pool_mla_moe_adaln"


def rms_norm(x, gain):
    xf = x.astype(jnp.float32)
    xf = xf * lax.rsqrt(jnp.mean(xf * xf, axis=-1, keepdims=True) + NORM_EPS)
    return (xf * gain.astype(jnp.float32)).astype(x.dtype)


def token_shift(y, mu):
    y_prev = jnp.pad(y, ((0, 0), (1, 0), (0, 0)))[:, :-1]
    return y + mu * (y_prev - y)


def rope_tables(positions):
    inv_freq = ROPE_BASE ** (-jnp.arange(0, MLA_QK_ROPE, 2, dtype=jnp.float32) / MLA_QK_ROPE)
    ang = positions.astype(jnp.float32)[..., None] * inv_freq
    return jnp.cos(ang), jnp.sin(ang)


def apply_rope(x, cos, sin):
    cos = cos.astype(x.dtype)
    sin = sin.astype(x.dtype)
    x1, x2 = jnp.split(x, 2, axis=-1)
    return jnp.concatenate([x1 * cos - x2 * sin, x1 * sin + x2 * cos], axis=-1)


def wkv7_scan(r, w, k, v, a, b):
    B_, T_, H_, N_ = r.shape
    xs = tuple(jnp.moveaxis(t.astype(jnp.float32), 1, 0) for t in (r, w, k, v, a, b))

    def step(S, inp):
        r_t, w_t, k_t, v_t, a_t, b_t = inp
        sa = jnp.einsum('bhvk,bhk->bhv', S, a_t)
        S = (S * w_t[:, :, None, :] + sa[..., None] * b_t[:, :, None, :]
             + v_t[..., None] * k_t[:, :, None, :])
        return S, jnp.einsum('bhvk,bhk->bhv', S, r_t)

    S0 = jnp.zeros((B_, H_, N_, N_), jnp.float32)
    _, y = lax.scan(step, S0, xs)
    return jnp.moveaxis(y, 0, 1)


def rwkv7_time_mix(r, k, v, wd, ad, gd, vec, w2, a2, g2):
    w0, a0, k_k, k_a, r_k, lnx_gain, lnx_bias = (vec[i] for i in range(7))
    B_, T_, _ = r.shape

    def heads(t):
        return t.reshape(B_, T_, RWKV_HEADS, HEAD_DIM)

    w_log = -jax.nn.softplus(-(w0 + jnp.tanh(wd) @ w2)) - 0.5
    decay = jnp.exp(-jnp.exp(w_log.astype(jnp.float32)))
    a = jax.nn.sigmoid(a0 + ad @ a2)
    g = jax.nn.sigmoid(gd) @ g2
    kk = heads((k * k_k).astype(jnp.float32))
    kk = kk / jnp.maximum(jnp.sqrt(jnp.sum(kk * kk, axis=-1, keepdims=True)), 1e-12)
    k = k * (1 + (a - 1) * k_a)
    rh, kh, vh, ah = heads(r), heads(k), heads(v), heads(a)
    y = wkv7_scan(rh, heads(decay), kh, vh, -kk, kk * ah.astype(jnp.float32))
    mu = jnp.mean(y, axis=-1, keepdims=True)
    var = jnp.mean(jnp.square(y - mu), axis=-1, keepdims=True)
    y = ((y - mu) * lax.rsqrt(var + RWKV_LNX_EPS)).reshape(B_, T_, RWKV_WIDTH)
    y = (y * lnx_gain.astype(jnp.float32) + lnx_bias.astype(jnp.float32)).astype(r.dtype)
    bonus = jnp.sum(rh * kh * r_k.reshape(RWKV_HEADS, HEAD_DIM), axis=-1, keepdims=True) * vh
    y = y + bonus.reshape(B_, T_, RWKV_WIDTH)
    return y * g


def multiscale_pool(u, w_pool, scale):
    B_, T_, C_ = u.shape
    uf = u.astype(jnp.float32).reshape(B_, T_, POOL_GROUPS, POOL_GROUP_DIM)
    cs = jnp.cumsum(uf, axis=1)
    t_idx = jnp.arange(T_)
    outs = []
    for gi, win in enumerate(POOL_WINDOWS):
        csg = cs[:, :, gi]
        lag = jnp.pad(csg, ((0, 0), (win, 0), (0, 0)))[:, :T_]
        cnt = jnp.minimum(t_idx + 1, win).astype(jnp.float32)[None, :, None]
        outs.append((csg - lag) / cnt - uf[:, :, gi])
    p = jnp.stack(outs, axis=2).astype(u.dtype)
    y = jnp.einsum('btgc,gcd->btgd', p, w_pool)
    return y.reshape(B_, T_, C_) * scale


def causal_block_attention(q, k, v):
    B_, T_, H_, Dq = q.shape
    nb = T_ // Q_BLOCK
    scale = Dq ** -0.5
    qb = q.reshape(B_, nb, Q_BLOCK, H_, Dq).transpose(1, 0, 2, 3, 4)
    key_pos = jnp.arange(T_)

    def one_block(args):
        q_blk, blk = args
        s = jnp.einsum('bqhd,bkhd->bhqk', q_blk, k).astype(jnp.float32) * scale
        q_pos = blk * Q_BLOCK + jnp.arange(Q_BLOCK)
        s = jnp.where(key_pos[None, :] <= q_pos[:, None], s, NEG_INF)
        p = jax.nn.softmax(s, axis=-1).astype(v.dtype)
        return jnp.einsum('bhqk,bkhd->bqhd', p, v)

    o = lax.map(one_block, (qb, jnp.arange(nb)))
    return o.transpose(1, 0, 2, 3, 4).reshape(B_, T_, H_, v.shape[-1])


def mla_attention(q_lat, kv_lat, k_rope, cos, sin, q_lat_gain, kv_lat_gain, wq_up, wkv_up, qk_gain):
    B_, T_, _ = q_lat.shape
    q = (rms_norm(q_lat, q_lat_gain) @ wq_up).reshape(B_, T_, MLA_HEADS, MLA_QK_DIM)
    kv = (rms_norm(kv_lat, kv_lat_gain) @ wkv_up).reshape(B_, T_, MLA_HEADS, MLA_QK_NOPE + MLA_V_DIM)
    q_pe = apply_rope(q[..., MLA_QK_NOPE:], cos[:, :, None], sin[:, :, None])
    k_pe = apply_rope(k_rope, cos, sin)[:, :, None, :]
    q = jnp.concatenate([q[..., :MLA_QK_NOPE], q_pe], axis=-1)
    k = jnp.concatenate([kv[..., :MLA_QK_NOPE],
                         jnp.broadcast_to(k_pe, (B_, T_, MLA_HEADS, MLA_QK_ROPE))], axis=-1)
    v = kv[..., MLA_QK_NOPE:]
    q = rms_norm(q, qk_gain[0])
    k = rms_norm(k, qk_gain[1])
    o = causal_block_attention(q, k, v)
    return o.reshape(B_, T_, MLA_WIDTH)


def swiglu(h, w_in, w_out):
    g, u = jnp.split(h @ w_in, 2, axis=-1)
    return (jax.nn.silu(g) * u) @ w_out


def moe_swiglu(h, router, w_in, w_out):
    B_, T_, D_ = h.shape
    n_tok = B_ * T_
    hf = h.reshape(n_tok, D_)
    logits = (hf @ router).astype(jnp.float32)
    top_val, top_idx = lax.top_k(logits, TOP_K)
    gate = jax.nn.softmax(top_val, axis=-1)
    n_asg = n_tok * TOP_K
    exp_flat = top_idx.reshape(-1)
    tok_flat = (jnp.arange(n_asg) // TOP_K).astype(jnp.int32)
    gate_flat = gate.reshape(-1)
    order = jnp.argsort(exp_flat)
    exp_sorted = exp_flat[order]
    counts = jnp.bincount(exp_flat, length=N_EXPERTS)
    padded = (counts + MOE_BLOCK - 1) // MOE_BLOCK * MOE_BLOCK
    start = jnp.cumsum(counts) - counts
    pend = jnp.cumsum(padded)
    pstart = pend - padded
    dest = pstart[exp_sorted] + jnp.arange(n_asg) - start[exp_sorted]
    n_blocks = -(-n_asg // MOE_BLOCK) + N_EXPERTS
    n_rows = n_blocks * MOE_BLOCK
    row_tok = jnp.full((n_rows,), n_tok, jnp.int32).at[dest].set(tok_flat[order])
    row_gate = jnp.zeros((n_rows,), jnp.float32).at[dest].set(gate_flat[order])
    block_exp = jnp.clip(jnp.searchsorted(pend, jnp.arange(n_blocks) * MOE_BLOCK, side='right'),
                         0, N_EXPERTS - 1)
    x_pad = jnp.concatenate([hf, jnp.zeros((1, D_), hf.dtype)], axis=0)
    xb = x_pad[row_tok].reshape(n_blocks, MOE_BLOCK, D_)

    def expert_block(args):
        x_blk, e = args
        return swiglu(x_blk, w_in[e], w_out[e])

    yb = lax.map(expert_block, (xb, block_exp)).reshape(n_rows, D_)
    y = jax.ops.segment_sum(yb * row_gate[:, None].astype(yb.dtype), row_tok,
                            num_segments=n_tok + 1)[:n_tok]
    return y.reshape(B_, T_, D_)


def setup_inputs(seed: int = 0) -> dict:
    key = jax.random.key(seed)
    ks = iter(jax.random.split(key, 48))

    def nrm(shape, scale):
        return jax.random.normal(next(ks), shape, jnp.float32) * scale

    def near_one(shape):
        return 1.0 + nrm(shape, 0.1)

    L, Lr = DEPTH, DEPTH - 1
    x = nrm((BATCH, SEQ, D_MODEL), 1.0)
    c = nrm((BATCH, D_MODEL), 1.0)
    positions = (jax.random.randint(next(ks), (BATCH, 1), 0, 1024)
                 + jnp.arange(SEQ)[None, :]).astype(jnp.int32)
    w_ada = nrm((L, D_MODEL, 6 * D_MODEL), 0.5 * D_MODEL ** -0.5)
    b_ada = nrm((L, 6 * D_MODEL), 0.01)
    norm_gain = near_one((L, 2, D_MODEL))
    w_in_first = nrm((D_MODEL, N_IN_BASE), D_MODEL ** -0.5)
    w_in_rest = nrm((Lr, D_MODEL, N_IN_REST), D_MODEL ** -0.5)
    mu_shift = jax.random.uniform(next(ks), (L, RWKV_SHIFT_COLS), jnp.float32)
    mu_shift_v = jax.random.uniform(next(ks), (Lr, RWKV_VRES_LORA), jnp.float32)
    w0 = jax.random.uniform(next(ks), (L, RWKV_WIDTH), jnp.float32, -4.0, 0.0)
    rwkv_vec = jnp.stack([
        w0,
        nrm((L, RWKV_WIDTH), 0.1),
        0.85 + nrm((L, RWKV_WIDTH), 0.05),
        near_one((L, RWKV_WIDTH)),
        nrm((L, RWKV_WIDTH), 0.1),
        near_one((L, RWKV_WIDTH)),
        nrm((L, RWKV_WIDTH), 0.01),
    ], axis=1)
    rwkv_v0 = near_one((Lr, RWKV_WIDTH))
    rwkv_w2 = nrm((L, RWKV_DECAY_LORA, RWKV_WIDTH), 0.1)
    rwkv_a2 = nrm((L, RWKV_ICLR_LORA, RWKV_WIDTH), 0.1)
    rwkv_g2 = nrm((L, RWKV_GATE_LORA, RWKV_WIDTH), RWKV_GATE_LORA ** -0.5)
    rwkv_v2 = nrm((Lr, RWKV_VRES_LORA, RWKV_WIDTH), 0.1)
    pool_w = nrm((L, POOL_GROUPS, POOL_GROUP_DIM, POOL_GROUP_DIM), POOL_GROUP_DIM ** -0.5)
    pool_scale = near_one((L, POOL_WIDTH))
    mla_q_lat_gain = near_one((L, MLA_Q_LORA))
    mla_kv_lat_gain = near_one((L, MLA_KV_LORA))
    mla_wq_up = nrm((L, MLA_Q_LORA, MLA_HEADS * MLA_QK_DIM), MLA_Q_LORA ** -0.5)
    mla_wkv_up = nrm((L, MLA_KV_LORA, MLA_HEADS * (MLA_QK_NOPE + MLA_V_DIM)), MLA_KV_LORA ** -0.5)
    mla_qk_gain = near_one((L, 2, MLA_QK_DIM))
    w_out = nrm((L, D_MIX, D_MODEL), D_MIX ** -0.5)
    ffn_w_in = nrm((N_DENSE, D_MODEL, 2 * D_FF), D_MODEL ** -0.5)
    ffn_w_out = nrm((N_DENSE, D_FF, D_MODEL), D_FF ** -0.5)
    moe_router = nrm((N_MOE, D_MODEL, N_EXPERTS), D_MODEL ** -0.5)
    moe_w_in = nrm((N_MOE, N_EXPERTS, D_MODEL, 2 * D_FF_EXPERT), D_MODEL ** -0.5)
    moe_w_out = nrm((N_MOE, N_EXPERTS, D_FF_EXPERT, D_MODEL), D_FF_EXPERT ** -0.5)
    return {"x": x, "c": c, "positions": positions, "w_ada": w_ada, "b_ada": b_ada,
            "norm_gain": norm_gain, "w_in_first": w_in_first, "w_in_rest": w_in_rest,
            "mu_shift": mu_shift, "mu_shift_v": mu_shift_v, "rwkv_vec": rwkv_vec,
            "rwkv_v0": rwkv_v0, "rwkv_w2": rwkv_w2, "rwkv_a2": rwkv_a2, "rwkv_g2": rwkv_g2,
            "rwkv_v2": rwkv_v2, "pool_w": pool_w, "pool_scale": pool_scale,
            "mla_q_lat_gain": mla_q_lat_gain, "mla_kv_lat_gain": mla_kv_lat_gain,
            "mla_wq_up": mla_wq_up, "mla_wkv_up": mla_wkv_up, "mla_qk_gain": mla_qk_gain,
            "w_out": w_out, "ffn_w_in": ffn_w_in, "ffn_w_out": ffn_w_out,
            "moe_router": moe_router, "moe_w_in": moe_w_in, "moe_w_out": moe_w_out}


def reference(x, c, positions, w_ada, b_ada, norm_gain, w_in_first, w_in_rest, mu_shift,
              mu_shift_v, rwkv_vec, rwkv_v0, rwkv_w2, rwkv_a2, rwkv_g2, rwkv_v2, pool_w,
              pool_scale, mla_q_lat_gain, mla_kv_lat_gain, mla_wq_up, mla_wkv_up, mla_qk_gain,
              w_out, ffn_w_in, ffn_w_out, moe_router, moe_w_in, moe_w_out):
    cos, sin = rope_tables(positions)
    c_act = jax.nn.silu(c)
    v_first = None
    for l in range(DEPTH):
        mod = (c_act @ w_ada[l] + b_ada[l]).reshape(-1, 6, 1, D_MODEL)
        shift1, scale1, gate1, shift2, scale2, gate2 = (mod[:, i] for i in range(6))

        h = rms_norm(x, norm_gain[l, 0]) * (1 + scale1) + shift1
        z = h @ (w_in_first if l == 0 else w_in_rest[l - 1])
        zs = token_shift(z[..., :RWKV_SHIFT_COLS], mu_shift[l])
        r, k, v, wd, ad, gd = jnp.split(zs, RWKV_SPLITS, axis=-1)
        if l == 0:
            v_first = v
        else:
            vd = token_shift(z[..., N_IN_BASE:], mu_shift_v[l - 1])
            v = v + (v_first - v) * jax.nn.sigmoid(rwkv_v0[l - 1] + vd @ rwkv_v2[l - 1])
        y_rwkv = rwkv7_time_mix(r, k, v, wd, ad, gd, rwkv_vec[l], rwkv_w2[l], rwkv_a2[l], rwkv_g2[l])
        y_pool = multiscale_pool(z[..., OFF_POOL:OFF_QLAT], pool_w[l], pool_scale[l])
        y_mla = mla_attention(z[..., OFF_QLAT:OFF_KVLAT], z[..., OFF_KVLAT:OFF_KROPE],
                              z[..., OFF_KROPE:N_IN_BASE], cos, sin, mla_q_lat_gain[l],
                              mla_kv_lat_gain[l], mla_wq_up[l], mla_wkv_up[l], mla_qk_gain[l])
        mix = jnp.concatenate([y_rwkv, y_pool, y_mla], axis=-1) @ w_out[l]
        x = x + gate1 * mix

        h = rms_norm(x, norm_gain[l, 1]) * (1 + scale2) + shift2
        if l % 2 == 0:
            f = swiglu(h, ffn_w_in[l // 2], ffn_w_out[l // 2])
        else:
            f = moe_swiglu(h, moe_router[l // 2], moe_w_in[l // 2], moe_w_out[l // 2])
        x = x + gate2 * f
    return x
```

```python
import threading
import numpy as np
from contextlib import ExitStack
import concourse.bass as bass
import concourse.mybir as mybir
from concourse.bass_utils import run_bass_kernel_spmd

F32 = mybir.dt.float32
BF16 = mybir.dt.bfloat16
I32 = mybir.dt.int32
ALU = mybir.AluOpType
AF = mybir.ActivationFunctionType
AX = mybir.AxisListType


class Buf:
    __slots__ = ("name", "writers", "readers", "sem", "cnt", "old", "genr", "late")

    def __init__(self, name):
        self.name = name
        self.writers = []
        self.readers = []
        self.sem = None
        self.cnt = 0
        self.old = []
        self.genr = False
        self.late = False


class Rec:
    __slots__ = ("idx", "eng", "dma", "sem", "val", "ins", "buf")


COMPUTE = ("pe", "act", "dve", "pool")


class K:
    def __init__(self, nc, sigset=None):
        self.nc = nc
        self.engs = {"pe": nc.tensor, "act": nc.scalar, "dve": nc.vector,
                     "pool": nc.gpsimd, "sp": nc.sync}
        self.esem = {}
        for e in self.engs:
            self.esem[e] = nc.alloc_semaphore("es_" + e)
        self.ecnt = {e: 0 for e in self.engs}
        self.last = {e: None for e in self.engs}
        self.widx = {e: {p: -1 for p in self.engs} for e in self.engs}
        self.wdma = {e: {} for e in self.engs}
        self.nops = 0
        self.needed = set()
        self.sigset = sigset
        self.sempool = []
        self.dmabufs = []
        self.nsem = 0
        self.ninstr = {e: 0 for e in self.engs}

    def _bufsem(self, b):
        if getattr(b, "late", False):
            if b.sem is None:
                b.sem = self.nc.alloc_semaphore("ls_%d" % self.nsem)
                self.nsem += 1
                b.cnt = 0
            return b.sem
        if b.sem is None:
            if self.sempool:
                b.sem, b.cnt = self.sempool.pop()
            else:
                b.sem = self.nc.alloc_semaphore("ds_%d" % self.nsem)
                self.nsem += 1
                b.cnt = 0
            self.dmabufs.append(b)
        return b.sem

    def release(self, bufs):
        for b in bufs:
            if b.sem is not None:
                self.sempool.append((b.sem, b.cnt))
                self.dmabufs.remove(b)
                b.sem = None

    def _wait(self, eng, r):
        e = self.engs[eng]
        if r.dma:
            key = r.sem.num
            val = r.val
            if r.buf.sem is r.sem and r.buf.cnt > val:
                val = r.buf.cnt
            cur = self.wdma[eng].get(key, 0)
            if cur >= val:
                return
            e.wait_ge(r.sem, val)
            self.ninstr[eng] += 1
            self.wdma[eng][key] = val
        else:
            if r.eng == eng and eng in ("pe", "sp"):
                return
            if self.widx[eng][r.eng] >= r.idx:
                return
            self.widx[eng][r.eng] = r.idx
            self.needed.add(r.idx)
            if r.val is None:
                raise RuntimeError("op %d needed but not signalled" % r.idx)
            e.wait_ge(self.esem[r.eng], r.val)
            self.ninstr[eng] += 1

    def _deps(self, eng, reads, writes, partial):
        deps = []
        for b in reads:
            deps.extend(b.writers)
        for b in writes:
            deps.extend(b.writers)
            deps.extend(b.readers)
        for b in partial:
            deps.extend(b.writers)
            deps.extend(b.readers)
            deps.extend(b.old)
        seen = set()
        for d in deps:
            if id(d) in seen:
                continue
            seen.add(id(d))
            self._wait(eng, d)

    def _post(self, rec, reads, writes, partial):
        for b in reads:
            b.readers.append(rec)
            b.old = []
            b.genr = True
        for b in writes:
            b.writers = [rec]
            b.readers = []
            b.old = []
            b.genr = False
        for b in partial:
            if b.genr:
                b.writers = [rec]
                b.genr = False
            else:
                b.writers.append(rec)
            b.old = b.old + b.readers
            b.readers = []

    def op(self, eng, fn, reads=(), writes=(), partial=()):
        self._deps(eng, reads, writes, partial)
        ins = fn(self.engs[eng])
        self.ninstr[eng] += 1
        rec = Rec()
        rec.idx = self.nops
        self.nops += 1
        rec.eng = eng
        rec.dma = False
        rec.sem = None
        rec.ins = ins
        if self.sigset is None or rec.idx in self.sigset:
            self.ecnt[eng] += 1
            ins.then_inc(self.esem[eng], 1)
            rec.val = self.ecnt[eng]
        else:
            rec.val = None
        self.last[eng] = rec
        self._post(rec, reads, writes, partial)
        if eng != "pe":
            self.yp()
        return rec

    def dma(self, q, out, in_, sembuf, reads=(), writes=(), partial=(), nodeps=False, **kw):
        if not nodeps:
            self._deps(q, reads, writes, partial)
        sem = self._bufsem(sembuf)
        ins = self.engs[q].dma_start(out=out, in_=in_, **kw)
        self.ninstr[q] += 1
        sembuf.cnt += 16
        ins.then_inc(sem, 16)
        rec = Rec()
        rec.idx = self.nops
        self.nops += 1
        rec.eng = q
        rec.dma = True
        rec.sem = sem
        rec.val = sembuf.cnt
        rec.buf = sembuf
        rec.ins = ins
        self._post(rec, reads, writes, partial)
        return rec

    def coop_run(self, streams):
        if len(streams) == 1:
            streams[0][1]()
            return
        main = threading.Semaphore(0)
        st = {}
        order = []
        err = []
        for name, fn in streams:
            sem = threading.Semaphore(0)
            d = {"sem": sem, "alive": True}
            st[name] = d
            order.append(name)

            def runner(fn=fn, d=d):
                d["sem"].acquire()
                try:
                    fn()
                except BaseException as ex:
                    err.append(ex)
                d["alive"] = False
                main.release()
            d["th"] = threading.Thread(target=runner)
            d["th"].start()
        self._coop = (st, main)
        while any(st[n]["alive"] for n in order):
            for n in order:
                if st[n]["alive"]:
                    self._cur = n
                    st[n]["sem"].release()
                    main.acquire()
        self._coop = None
        for n in order:
            st[n]["th"].join()
        if err:
            raise err[0]

    def yp(self):
        co = getattr(self, "_coop", None)
        if co is None:
            return
        st, main = co
        me = st[self._cur]
        main.release()
        me["sem"].acquire()

    def coop_wait(self, name):
        co = getattr(self, "_coop", None)
        if co is None:
            return
        st, main = co
        while name in st and st[name]["alive"]:
            self.yp()

    def barrier(self):
        sp = self.engs["sp"]
        for e in COMPUTE:
            if self.last[e] is not None:
                self._wait("sp", self.last[e])
        for b in self.dmabufs:
            if b.cnt > self.wdma["sp"].get(b.sem.num, 0):
                sp.wait_ge(b.sem, b.cnt)
                self.wdma["sp"][b.sem.num] = b.cnt
        ins = sp.nop()
        rec = Rec()
        rec.idx = self.nops
        self.nops += 1
        rec.eng = "sp"
        rec.dma = False
        rec.sem = None
        rec.ins = ins
        self.ecnt["sp"] += 1
        ins.then_inc(self.esem["sp"], 1)
        rec.val = self.ecnt["sp"]
        self.needed.add(rec.idx)
        for e in COMPUTE:
            self.engs[e].wait_ge(self.esem["sp"], rec.val)
            self.widx[e]["sp"] = rec.idx
            for p in COMPUTE:
                if self.last[p] is not None:
                    self.widx[e][p] = max(self.widx[e][p], self.last[p].idx)
            for b in self.dmabufs:
                self.wdma[e][b.sem.num] = b.cnt
        for b in self.dmabufs:
            self.sempool.append((b.sem, b.cnt))
            b.sem = None
        self.dmabufs = []
        return rec

T = 4096
D = 1024
NT = T // 512
KO = D // 128


class Ctx:
    pass


def setup_common(nc, k, es, c):
    c.ps = []
    c.psb = []
    for i in range(8):
        t = es.enter_context(nc.psum_tensor("ps%d" % i, [128, 512], F32))
        c.ps.append(t)
        c.psb.append(Buf("ps%d" % i))
    c.ident = es.enter_context(nc.sbuf_tensor("ident", [128, 128], F32))
    c.identb = Buf("ident")
    c.ones = es.enter_context(nc.sbuf_tensor("ones", [128, 128], F32))
    c.onesb = Buf("ones")
    k.op("pool", lambda e: e.memset(c.ones[:], 1.0), writes=[c.onesb])
    k.op("pool", lambda e: e.affine_select(out=c.ident[:], in_=c.ones[:], pattern=[[-1, 128]],
                                           compare_op=ALU.is_equal, fill=0.0, base=0,
                                           channel_multiplier=1),
         reads=[c.onesb], writes=[c.identb])


def phase_transpose_in(nc, k, c, x, xT):
    with ExitStack() as es:
        xin = [es.enter_context(nc.sbuf_tensor("xin%d" % i, [128, D], F32)) for i in range(2)]
        xinb = [Buf("xin%d" % i) for i in range(2)]
        st = [es.enter_context(nc.sbuf_tensor("xst%d" % i, [128, KO, 512], F32)) for i in range(2)]
        stb = [Buf("xst%d" % i) for i in range(2)]
        pi = 0
        for tt in range(NT):
            s = st[tt % 2]
            sb = stb[tt % 2]
            for j in range(4):
                tb = tt * 4 + j
                xi = xin[tb % 2]
                xib = xinb[tb % 2]
                k.dma("sp", xi[:], x[tb * 128:(tb + 1) * 128, :], xib, writes=[xib])
                for half in range(2):
                    p = c.ps[pi % 8]
                    pb = c.psb[pi % 8]
                    pi += 1
                    for q in range(4):
                        ko = half * 4 + q
                        k.op("pe", lambda e, p=p, xi=xi, ko=ko, q=q: e.transpose(
                            out=p[:, q * 128:(q + 1) * 128], in_=xi[:, ko * 128:(ko + 1) * 128],
                            identity=c.ident[:]),
                            reads=[xib, c.identb], writes=[pb] if q == 0 else (), partial=[pb] if q else ())
                    eng = "dve" if (half == 0 or MODE_IN == 0) else "act"
                    if eng == "dve":
                        k.op("dve", lambda e, p=p, s=s, half=half, j=j: e.tensor_copy(
                            out=s[:, half * 4:(half + 1) * 4, j * 128:(j + 1) * 128],
                            in_=p[:].rearrange("p (q t) -> p q t", q=4)),
                            reads=[pb], partial=[sb])
                    else:
                        k.op("act", lambda e, p=p, s=s, half=half, j=j: e.copy(
                            out=s[:, half * 4:(half + 1) * 4, j * 128:(j + 1) * 128],
                            in_=p[:].rearrange("p (q t) -> p q t", q=4)),
                            reads=[pb], partial=[sb])
            k.dma("sp", xT[:, :, tt * 512:(tt + 1) * 512].rearrange("k p t -> p k t"), s[:], sb, reads=[sb])
    k.barrier()


def phase_transpose_out(nc, k, c, xT, out):
    with ExitStack() as es:
        xin = [es.enter_context(nc.sbuf_tensor("yin%d" % i, [128, KO, 512], F32)) for i in range(2)]
        xinb = [Buf("yin%d" % i) for i in range(2)]
        st = [es.enter_context(nc.sbuf_tensor("yst%d" % i, [128, D], F32)) for i in range(2)]
        stb = [Buf("yst%d" % i) for i in range(2)]
        pi = 0
        for tt in range(NT):
            xi = xin[tt % 2]
            xib = xinb[tt % 2]
            k.dma("sp", xi[:], xT[:, :, tt * 512:(tt + 1) * 512].rearrange("k p t -> p k t"), xib, writes=[xib])
            for j in range(4):
                tb = tt * 4 + j
                s = st[tb % 2]
                sb = stb[tb % 2]
                for half in range(2):
                    p = c.ps[pi % 8]
                    pb = c.psb[pi % 8]
                    pi += 1
                    for q in range(4):
                        ko = half * 4 + q
                        k.op("pe", lambda e, p=p, xi=xi, ko=ko, q=q, j=j: e.transpose(
                            out=p[:, q * 128:(q + 1) * 128], in_=xi[:, ko, j * 128:(j + 1) * 128],
                            identity=c.ident[:]),
                            reads=[xib, c.identb], writes=[pb] if q == 0 else (), partial=[pb] if q else ())
                    if half == 0 or MODE_OUT == 0:
                        k.op("dve", lambda e, p=p, s=s, half=half: e.tensor_copy(
                            out=s[:, half * 512:(half + 1) * 512], in_=p[:]), reads=[pb], partial=[sb])
                    else:
                        k.op("act", lambda e, p=p, s=s, half=half: e.copy(
                            out=s[:, half * 512:(half + 1) * 512], in_=p[:]), reads=[pb], partial=[sb])
                k.dma("sp", out[tb * 128:(tb + 1) * 128, :], s[:], sb, reads=[sb])
    k.barrier()


RW = 512
NZ = 21
NSH = 15
C_WA, C_GD0, C_GD1, C_POOL, C_QLAT, C_KVLAT, C_KROPE = 12, 13, 14, 15, 17, 19, 20
DFF = 2816
DFE = 3584
NE = 8
EPS = 1e-6

PCOL = {}
_off = 0
for _n, _w in [("b_ada", 48), ("g1", 8), ("g2", 8), ("mu", NSH), ("w0", 4), ("a0", 4), ("kk", 4),
               ("ka", 4), ("rk", 4), ("lng", 4), ("lnb", 4), ("v0", 4), ("pscale", 2), ("qlg", 2),
               ("kvlg", 1), ("qkq", 1), ("qkk", 1)]:
    PCOL[_n] = (_off, _w)
    _off += _w
NPCOL = _off


def fm(v, n):
    return np.ascontiguousarray(np.asarray(v, np.float32).reshape(n, 128).T)


def prep_layer(inp, l):
    f32 = np.float32
    d = {}
    P = np.zeros((128, NPCOL), f32)

    def put(name, arr):
        o, w = PCOL[name]
        assert arr.shape == (128, w), (name, arr.shape)
        P[:, o:o + w] = arr
    put("b_ada", fm(inp["b_ada"][l], 48))
    put("g1", fm(inp["norm_gain"][l, 0], 8))
    put("g2", fm(inp["norm_gain"][l, 1], 8))
    w_in = inp["w_in_first"] if l == 0 else inp["w_in_rest"][l - 1]
    mu = inp["mu_shift"][l]
    cols = np.zeros((NZ * 128,), np.int64) - 1
    cols[0:1536] = np.arange(0, 1536)
    cols[1536:1536 + 128] = np.arange(1536, 1664)
    cols[1664:1664 + 128] = np.arange(1664, 1792)
    cols[1792:1792 + 32] = np.arange(1792, 1824)
    muc = np.zeros((NZ * 128,), f32)
    muc[0:1824] = mu
    if l > 0:
        cols[1824:1856] = np.arange(2496, 2528)
        muc[1824:1856] = inp["mu_shift_v"][l - 1]
    cols[1920:1920 + 256] = np.arange(1824, 2080)
    cols[2176:2176 + 256] = np.arange(2080, 2336)
    cols[2432:2432 + 128] = np.arange(2336, 2464)
    kr = 2464
    cols[2560:2560 + 32] = np.arange(kr, kr + 32)
    cols[2592:2592 + 16] = np.arange(kr + 16, kr + 32)
    cols[2608:2608 + 16] = np.arange(kr, kr + 16)
    W = np.zeros((D, NZ * 128), f32)
    m = cols >= 0
    W[:, m] = w_in[:, cols[m]]
    d["win"] = np.ascontiguousarray(W.reshape(8, 128, NZ * 128).transpose(1, 0, 2))
    put("mu", fm(muc[:NSH * 128], NSH))
    rv = inp["rwkv_vec"][l]
    for i, nm in enumerate(["w0", "a0", "kk", "ka", "rk", "lng", "lnb"]):
        put(nm, fm(rv[i], 4))
    if l > 0:
        put("v0", fm(inp["rwkv_v0"][l - 1], 4))
    put("pscale", fm(inp["pool_scale"][l], 2))
    put("qlg", fm(inp["mla_q_lat_gain"][l], 2))
    put("kvlg", fm(inp["mla_kv_lat_gain"][l], 1))
    g = np.zeros((128, 1), f32); g[:96, 0] = inp["mla_qk_gain"][l, 0]; put("qkq", g)
    g = np.zeros((128, 1), f32); g[:96, 0] = inp["mla_qk_gain"][l, 1]; put("qkk", g)
    d["par"] = P
    d["wada"] = np.ascontiguousarray(inp["w_ada"][l].reshape(8, 128, 6144).transpose(1, 0, 2))
    w2a2 = np.concatenate([inp["rwkv_w2"][l], inp["rwkv_a2"][l]], axis=0)
    g2 = inp["rwkv_g2"][l]
    g2b = np.zeros((128, 512), f32)
    g2b[0:32] = g2[128:160]
    if l > 0:
        g2b[32:64] = inp["rwkv_v2"][l - 1]
    d["rw"] = np.ascontiguousarray(np.stack([w2a2, g2[0:128], g2b], axis=1))
    pw = np.zeros((128, 2, 128), f32)
    for gi in range(4):
        pc, o = gi // 2, (gi % 2) * 64
        pw[o:o + 64, pc, o:o + 64] = inp["pool_w"][l, gi]
    d["poolw"] = pw
    wq = inp["mla_wq_up"][l]
    WQ = np.zeros((256, 4, 2, 96), f32)
    for h in range(4):
        WQ[:, h, 0, :] = wq[:, h * 96:(h + 1) * 96]
        WQ[:, h, 1, 64:80] = wq[:, h * 96 + 80:h * 96 + 96]
        WQ[:, h, 1, 80:96] = wq[:, h * 96 + 64:h * 96 + 80]
    d["wq"] = np.ascontiguousarray(WQ.reshape(2, 128, 4 * 2 * 96).transpose(1, 0, 2))
    wkv = inp["mla_wkv_up"][l].reshape(128, 4, 128)
    d["wkv"] = np.ascontiguousarray(np.concatenate(
        [wkv[:, :, :64].reshape(128, 256), wkv[:, :, 64:].reshape(128, 256)], axis=1))
    d["wout"] = np.ascontiguousarray(inp["w_out"][l].reshape(8, 128, 1024).transpose(1, 0, 2))
    return d


LAYER_SHAPES = {"win": [128, 8, NZ * 128], "par": [128, NPCOL], "wada": [128, 8, 6144], "rw": [128, 3, 512],
                "poolw": [128, 2, 128], "wq": [128, 2, 768], "wkv": [128, 512], "wout": [128, 8, 1024]}

_SBN = [0]


def sb(nc, es, name, shape, dt=F32):
    _SBN[0] += 1
    t = es.enter_context(nc.sbuf_tensor("s%d_%s" % (_SBN[0], name), shape, dt))
    return t, Buf(name)


def load_params(nc, k, es, c, L):
    par, parb = sb(nc, es, "par", [128, NPCOL])
    k.dma("sp", par[:], L["par"], parb, writes=[parb])
    c.par, c.parb = par, parb
    mod, modb = sb(nc, es, "mod", [128, 64])
    c.mod, c.modb = mod, modb
    cact, cactb = sb(nc, es, "cact", [128, 8])
    k.op("act", lambda e: e.activation(out=cact[:], in_=c.cT[:], func=AF.Silu), reads=[c.cTb], writes=[cactb])
    with ExitStack() as es2:
        wa = [sb(nc, es2, "wada%d" % i, [128, 8, 512]) for i in range(2)]
        p, pb = c.ps[0], c.psb[0]
        for nt in range(12):
            w, wb = wa[nt % 2]
            k.dma("sp", w[:], L["wada"][:, :, nt * 512:(nt + 1) * 512], wb, writes=[wb])
            for j in range(4):
                col = nt * 4 + j
                for ko in range(8):
                    k.op("pe", lambda e, w=w, j=j, ko=ko, col=col: e.matmul(
                        p[:, col:col + 1], lhsT=w[:, ko, j * 128:(j + 1) * 128], rhs=cact[:, ko:ko + 1],
                        start=(ko == 0), stop=(ko == 7)),
                        reads=[wb, cactb], writes=[pb] if (col == 0 and ko == 0) else (),
                        partial=() if (col == 0 and ko == 0) else [pb])
        o, _ = PCOL["b_ada"]
        k.op("dve", lambda e: e.tensor_tensor(out=mod[:, 0:48], in0=p[:, 0:48], in1=par[:, o:o + 48], op=ALU.add),
             reads=[pb, parb], partial=[modb])
        for (dst, sc, gn) in [(48, 8, "g1"), (56, 32, "g2")]:
            go, _ = PCOL[gn]
            k.op("dve", lambda e, dst=dst, sc=sc, go=go: e.scalar_tensor_tensor(
                out=mod[:, dst:dst + 8], in0=mod[:, sc:sc + 8], scalar=1.0, in1=par[:, go:go + 8],
                op0=ALU.add, op1=ALU.mult), reads=[modb, parb], partial=[modb])
        k.barrier()


def norm_mod(nc, k, c, xt, xtb, tmp, tmpb, sq, sqb, rstd, rstdb, hT, hTb, pbank, acol, bcol, h32=None, h32b=None):
    p, pb = c.ps[pbank], c.psb[pbank]
    k.op("act", lambda e: e.activation(out=sq[:], in_=xt[:], func=AF.Square), reads=[xtb], writes=[sqb])
    for ko in range(8):
        k.op("pe", lambda e, ko=ko: e.matmul(p[:], lhsT=c.onesbf[:], rhs=sq[:, ko, :], start=(ko == 0), stop=(ko == 7)),
             reads=[sqb, c.onesbfb], writes=[pb] if ko == 0 else (), partial=[pb] if ko else ())
    k.op("act", lambda e: e.activation(out=rstd[:], in_=p[:], func=AF.Sqrt, scale=1.0 / D, bias=c.epsc[:, 0:1]),
         reads=[pb, c.epscb], writes=[rstdb])
    k.op("dve", lambda e: e.reciprocal(out=rstd[:], in_=rstd[:]), reads=[rstdb], writes=[rstdb])
    k.op("dve", lambda e: e.tensor_tensor(out=tmp[:], in0=xt[:], in1=rstd[:].unsqueeze(1).to_broadcast([128, 8, 512]),
                                          op=ALU.mult), reads=[xtb, rstdb], writes=[tmpb])
    for ko in range(8):
        k.op("act", lambda e, ko=ko: e.activation(out=hT[:, ko, :], in_=tmp[:, ko, :], func=AF.Identity,
                                                  scale=c.mod[:, acol + ko:acol + ko + 1],
                                                  bias=c.mod[:, bcol + ko:bcol + ko + 1]),
             reads=[tmpb, c.modb], writes=[hTb] if ko == 0 else (), partial=[hTb] if ko else ())
        if h32 is not None:
            k.op("pool", lambda e, ko=ko: e.tensor_scalar(out=h32[:, ko, :], in0=tmp[:, ko, :],
                                                          scalar1=c.mod[:, acol + ko:acol + ko + 1],
                                                          scalar2=c.mod[:, bcol + ko:bcol + ko + 1],
                                                          op0=ALU.mult, op1=ALU.add),
                 reads=[tmpb, c.modb], writes=[h32b] if ko == 0 else (), partial=[h32b] if ko else ())


def phase_inproj(nc, k, c, L, xT, zT, l):
    with ExitStack() as es:
        win, winb = sb(nc, es, "win", [128, 8, NZ * 128], BF16)
        for ko in range(8):
            k.dma("pool", win[:, ko, :], L["win"][:, ko, :], winb, partial=[winb])
        xts = [sb(nc, es, "xt%d" % i, [128, 8, 512]) for i in range(2)]
        tmp, tmpb = sb(nc, es, "tmp", [128, 8, 512])
        sq, sqb = sb(nc, es, "sq", [128, 8, 512], BF16)
        rstd, rstdb = sb(nc, es, "rstd", [128, 512])
        hTs = [sb(nc, es, "hT%d" % i, [128, 8, 512], BF16) for i in range(2)]
        zc, zcb = sb(nc, es, "zc", [128, NSH, 513])
        dd = [sb(nc, es, "dd%d" % i, [128, 512]) for i in range(2)]
        zo = [sb(nc, es, "zo%d" % i, [128, 512]) for i in range(4)]
        k.op("pool", lambda e: e.memset(zc[:], 0.0), writes=[zcb])
        mo, _ = PCOL["mu"]
        zi = [0]

        def norm_body(tt):
            xt, xtb = xts[tt % 2]
            hT, hTb = hTs[tt % 2]
            k.dma("sp", xt[:], xT[:, :, tt * 512:(tt + 1) * 512].rearrange("k p t -> p k t"), xtb, writes=[xtb])
            norm_mod(nc, k, c, xt, xtb, tmp, tmpb, sq, sqb, rstd, rstdb, hT, hTb, 7, 48, 0)

        def mm_body(tt):
            hT, hTb = hTs[tt % 2]
            k.op("pool", lambda e: e.tensor_copy(out=zc[:, :, 0:1], in_=zc[:, :, 512:513]), reads=[zcb], writes=[zcb])
            for nch in range(NZ):
                p, pb = c.ps[nch % 6], c.psb[nch % 6]
                for ko in range(8):
                    k.op("pe", lambda e, p=p, ko=ko, nch=nch: e.matmul(
                        p[:], lhsT=win[:, ko, nch * 128:(nch + 1) * 128], rhs=hT[:, ko, :],
                        start=(ko == 0), stop=(ko == 7)),
                        reads=[winb, hTb], writes=[pb] if ko == 0 else (), partial=[pb] if ko else ())
                z, zb = zo[zi[0] % 4]
                zi[0] += 1
                if nch < NSH:
                    d_, db = dd[nch % 2]
                    k.op("act", lambda e, p=p, nch=nch: e.copy(out=zc[:, nch, 1:513], in_=p[:]), reads=[pb], partial=[zcb])
                    k.op("dve", lambda e, d_=d_, nch=nch: e.tensor_tensor(out=d_[:], in0=zc[:, nch, 0:512], in1=zc[:, nch, 1:513],
                                                                     op=ALU.subtract), reads=[zcb], writes=[db])
                    k.op("dve", lambda e, d_=d_, z=z, nch=nch: e.scalar_tensor_tensor(
                        out=z[:], in0=d_[:], scalar=c.par[:, mo + nch:mo + nch + 1], in1=zc[:, nch, 1:513],
                        op0=ALU.mult, op1=ALU.add), reads=[db, zcb, c.parb], writes=[zb])
                else:
                    k.op("act", lambda e, p=p, z=z: e.copy(out=z[:], in_=p[:]), reads=[pb], writes=[zb])
                k.dma("sp", zT[nch, :, tt * 512:(tt + 1) * 512], z[:], zb, reads=[zb])

        norm_body(0)
        for tt in range(NT):
            streams = [("M", lambda tt=tt: mm_body(tt))]
            if tt + 1 < NT:
                streams.append(("N", lambda tt=tt: norm_body(tt + 1)))
            k.coop_run(streams)
        k.barrier()

C0 = float(np.exp(-0.5))
LNX_EPS = 64e-5


def setup_rwkv_consts(nc, k, es, c):
    c.bones, c.bonesb = sb(nc, es, "bones", [128, 128])
    k.op("pool", lambda e: e.memset(c.bones[:], 0.0), writes=[c.bonesb])
    for h in range(2):
        k.op("pool", lambda e, h=h: e.memset(c.bones[h * 64:(h + 1) * 64, h * 64:(h + 1) * 64], 1.0), partial=[c.bonesb])
    c.ones3, c.ones3b = sb(nc, es, "ones3", [128, 8, 128])
    k.op("pool", lambda e: e.memset(c.ones3[:], 1.0), writes=[c.ones3b])
    c.maskL, c.maskLb = sb(nc, es, "maskL", [128, 8, 64])
    c.maskNM, c.maskNMb = sb(nc, es, "maskNM", [128, 4, 128])
    c.i64, c.i64b = sb(nc, es, "i64", [128, 8, 64])
    for h in range(2):
        ps_ = slice(h * 64, (h + 1) * 64)
        k.op("pool", lambda e, ps_=ps_: e.affine_select(out=c.maskL[ps_], in_=c.ones3[ps_, :, 0:64], pattern=[[0, 8], [-1, 64]],
                                                        compare_op=ALU.is_gt, fill=0.0, base=0, channel_multiplier=1),
             reads=[c.ones3b], partial=[c.maskLb])
        k.op("pool", lambda e, ps_=ps_: e.affine_select(out=c.maskNM[ps_, :, 0:64], in_=c.ones3[ps_, 0:4, 0:64], pattern=[[0, 4], [1, 64]],
                                                        compare_op=ALU.is_gt, fill=0.0, base=0, channel_multiplier=-1),
             reads=[c.ones3b], partial=[c.maskNMb])
        k.op("pool", lambda e, ps_=ps_: e.affine_select(out=c.maskNM[ps_, :, 64:128], in_=c.ones3[ps_, 0:4, 0:64], pattern=[[0, 4], [1, 64]],
                                                        compare_op=ALU.is_ge, fill=0.0, base=0, channel_multiplier=-1),
             reads=[c.ones3b], partial=[c.maskNMb])
        k.op("pool", lambda e, ps_=ps_: e.affine_select(out=c.i64[ps_], in_=c.ones3[ps_, :, 0:64], pattern=[[0, 8], [-1, 64]],
                                                        compare_op=ALU.is_equal, fill=0.0, base=0, channel_multiplier=1),
             reads=[c.ones3b], partial=[c.i64b])


class PsRot:
    def __init__(self, c, banks):
        self.c = c
        self.banks = list(banks)
        self.i = 0

    def get(self):
        b = self.banks[self.i % len(self.banks)]
        self.i += 1
        return self.c.ps[b], self.c.psb[b]


FAST32 = False


def R32(ap):
    return ap.bitcast(mybir.dt.float32r) if FAST32 else ap


def umm(k, p, pb, chunks, width, terms, reads):
    first = True
    for j, cc in enumerate(chunks):
        for h in range(2):
            nt = len(terms)
            for ti, (lf, rf) in enumerate(terms):
                k.op("pe", lambda e, h=h, cc=cc, j=j, lf=lf, rf=rf, ti=ti, nt=nt: e.matmul(
                    p[h * 64:(h + 1) * 64, j * width:(j + 1) * width], lhsT=R32(lf(h, cc)), rhs=R32(rf(h, cc)),
                    start=(ti == 0), stop=(ti == nt - 1)),
                    reads=reads, writes=[pb] if first else (), partial=() if first else [pb])
                first = False


def phase_rwkv(nc, k, c, L, zT, vfT, mixT, l):
    HS = lambda h: slice(h * 64, (h + 1) * 64)
    with ExitStack() as es:
        rw, rwb = sb(nc, es, "rw", [128, 3, 512], BF16)
        k.dma("pool", rw[:], L["rw"], rwb, writes=[rwb])
        par, parb = c.par, c.parb
        pc = lambda nm, i: par[:, PCOL[nm][0] + i:PCOL[nm][0] + i + 1]
        omka, omkab = sb(nc, es, "omka", [128, 4])
        k.op("dve", lambda e: e.tensor_scalar(out=omka[:], in0=par[:, PCOL["ka"][0]:PCOL["ka"][0] + 4], scalar1=-1.0, scalar2=1.0,
                                              op0=ALU.mult, op1=ALU.add), reads=[parb], writes=[omkab])
        lnxe, lnxeb = sb(nc, es, "lnxe", [128, 2])
        k.op("dve", lambda e: e.memset(lnxe[:, 0:1], LNX_EPS), partial=[lnxeb])
        k.op("dve", lambda e: e.memset(lnxe[:, 1:2], 1e-24), partial=[lnxeb])
        T2 = lambda nm, shape=[128, 512], dt=F32: sb(nc, es, nm, shape, dt)
        wa, wab = T2("wa"); gd0, gd0b = T2("gd0"); gd1, gd1b = T2("gd1")
        twa, twab = T2("twa", dt=BF16); sg0, sg0b = T2("sg0", dt=BF16); sg1, sg1b = T2("sg1", dt=BF16)
        rt = [T2("rt%d" % i) for i in range(2)]
        kt = [T2("kt%d" % i) for i in range(2)]
        vt = [T2("vt%d" % i) for i in range(2)]
        vft, vftb = T2("vft")
        sgm, sgmb = T2("sgm"); aa, aab = T2("aa"); kkr, kkrb = T2("kkr"); sq, sqb = T2("sqr")
        rn, rnb = T2("rn"); kk, kkb = T2("kk"); kp, kpb = T2("kp"); bs, bsb = T2("bs"); tA, tAb = T2("tA")
        rk, rkb = T2("rk"); cs, csb = T2("cs"); q, qb = T2("q", [128, 8, 64]); qm, qmb = T2("qm", [128, 8, 64])
        basec, basecb = T2("basec", [128, 8])
        E1, E1b = T2("E1", [128, 8, 64]); E2, E2b = T2("E2", [128, 8, 64]); E3, E3b = T2("E3", [128, 8, 64]); Eh, Ehb = T2("Eh", [128, 8, 64])
        IF = [dict(AR=T2("AR%d" % i, [128, 8, 128]), BtT=T2("BtT%d" % i, [128, 8, 64]), KtT=T2("KtT%d" % i, [128, 8, 64]),
                   Xtok=T2("Xtok%d" % i, [128, 8, 128]), Bhk=T2("Bhk%d" % i, [128, 8, 64]), Khk=T2("Khk%d" % i, [128, 8, 64]),
                   Vtok=T2("Vtok%d" % i, [128, 8, 64]), gam=T2("gam%d" % i, [128, 8])) for i in range(2)]
        BhT, BhTb = T2("BhT", [128, 8, 64]); KhT, KhTb = T2("KhT", [128, 8, 64])
        Lp = [T2("Lp%d" % i, [128, 8, 64]) for i in range(2)]
        Np = [T2("Np%d" % i, [128, 8, 64]) for i in range(2)]
        Pp = [T2("Pp%d" % i, [128, 8, 64]) for i in range(2)]
        NM, NMb = T2("NM", [128, 8, 128]); KM, KMb = T2("KM", [128, 8, 128]); WU, WUb = T2("WU", [128, 8, 128])
        Igam, Igamb = T2("Igam", [128, 8, 64])
        GT, GTb = T2("GT", [128, 4, 8, 64]); HH, HHb = T2("HH", [128, 4, 8, 64])
        R2T, R2Tb = T2("R2T", [128, 4, 8, 64]); Y0T, Y0Tb = T2("Y0T", [128, 4, 8, 64])
        gg, ggb = T2("gg", [128, 2, 4, 512], BF16); bon, bonb = T2("bon", [128, 2, 4, 512], BF16); yT, yTb = T2("yT", [128, 4, 512])
        vp, vpb = T2("vp")
        S, Sb = T2("S", [128, 4, 64])
        dd, ddb = T2("gnd"); yo = [T2("yo%d" % i, dt=BF16) for i in range(2)]
        k.op("pool", lambda e: e.memset(S[:], 0.0), writes=[Sb])
        prP = PsRot(c, [0, 1])
        prC = PsRot(c, [2, 3, 4, 5])
        prS = PsRot(c, [6, 7])

        def P_body(tt, hp):
            ifs = IF[(tt * 4 + hp) % 2]
            AR, ARb = ifs["AR"]; BtT, BtTb = ifs["BtT"]; KtT, KtTb = ifs["KtT"]; Xtok, Xtokb = ifs["Xtok"]
            Bhk, Bhkb = ifs["Bhk"]; Khk, Khkb = ifs["Khk"]; Vtok, Vtokb = ifs["Vtok"]; gam, gamb = ifs["gam"]
            ts_ = slice(tt * 512, (tt + 1) * 512)
            if l == 0 and hp == 0 and c.cast_moe is not None:
                c.cast_moe(tt)
            if hp == 0:
                k.dma("sp", wa[:], zT[C_WA, :, ts_], wab, writes=[wab])
                k.dma("sp", gd0[:], zT[C_GD0, :, ts_], gd0b, writes=[gd0b])
                k.dma("sp", gd1[:], zT[C_GD1, :, ts_], gd1b, writes=[gd1b])
                k.op("act", lambda e: e.activation(out=twa[0:64], in_=wa[0:64], func=AF.Tanh), reads=[wab], writes=[twab])
                k.op("act", lambda e: e.copy(out=twa[64:128], in_=wa[64:128]), reads=[wab], partial=[twab])
                k.op("act", lambda e: e.activation(out=sg0[:], in_=gd0[:], func=AF.Sigmoid), reads=[gd0b], writes=[sg0b])
                k.op("act", lambda e: e.activation(out=sg1[0:32], in_=gd1[0:32], func=AF.Sigmoid), reads=[gd1b], writes=[sg1b])
                k.op("act", lambda e: e.copy(out=sg1[32:64], in_=gd1[32:64]), reads=[gd1b], partial=[sg1b])
            hc = slice(hp * 128, (hp + 1) * 128)
            ii = (tt * 4 + hp) % 2
            r_, rb = rt[ii]; k_, kb = kt[ii]; v_, vb = vt[ii]
            k.dma("sp", r_[:], zT[hp, :, ts_], rb, writes=[rb])
            k.dma("sp", k_[:], zT[4 + hp, :, ts_], kb, writes=[kb])
            k.dma("sp", v_[:], zT[8 + hp, :, ts_], vb, writes=[vb])
            p, pb = prP.get()
            k.op("pe", lambda e, p=p: e.matmul(p[:], lhsT=rw[0:64, 0, hc], rhs=twa[0:64], start=True, stop=True),
                 reads=[rwb, twab], writes=[pb])
            k.op("act", lambda e, p=p: e.activation(out=sgm[:], in_=p[:], func=AF.Sigmoid, bias=pc("w0", hp)),
                 reads=[pb, parb], writes=[sgmb])
            p, pb = prP.get()
            k.op("pe", lambda e, p=p: e.matmul(p[:], lhsT=rw[64:128, 0, hc], rhs=twa[64:128], start=True, stop=True),
                 reads=[rwb, twab], writes=[pb])
            k.op("act", lambda e, p=p: e.activation(out=aa[:], in_=p[:], func=AF.Sigmoid, bias=pc("a0", hp)),
                 reads=[pb, parb], writes=[aab])
            p, pb = prP.get()
            k.op("pe", lambda e, p=p: e.matmul(p[:], lhsT=rw[:, 1, hc], rhs=sg0[:], start=True, stop=False),
                 reads=[rwb, sg0b], writes=[pb])
            k.op("pe", lambda e, p=p: e.matmul(p[:], lhsT=rw[0:32, 2, hc], rhs=sg1[0:32], start=False, stop=True),
                 reads=[rwb, sg1b], partial=[pb])
            k.op("act", lambda e, p=p: e.copy(out=gg[:, tt % 2, hp, :], in_=p[:]), reads=[pb], partial=[ggb])
            if l == 0:
                k.dma("sp", vfT[hp, :, ts_], v_[:], vb, reads=[vb])
                vq, vqb = v_, vb
            else:
                k.dma("sp", vft[:], vfT[hp, :, ts_], vftb, writes=[vftb])
                p, pb = prP.get()
                k.op("pe", lambda e, p=p: e.matmul(p[:], lhsT=rw[32:64, 2, hc], rhs=sg1[32:64], start=True, stop=True),
                     reads=[rwb, sg1b], writes=[pb])
                k.op("act", lambda e, p=p: e.activation(out=vp[:], in_=p[:], func=AF.Sigmoid, bias=pc("v0", hp)),
                     reads=[pb, parb], writes=[vpb])
                k.op("pool", lambda e: e.tensor_tensor(out=vft[:], in0=vft[:], in1=v_[:], op=ALU.subtract),
                     reads=[vftb, vb], writes=[vftb])
                k.op("pool", lambda e: e.tensor_tensor(out=vp[:], in0=vp[:], in1=vft[:], op=ALU.mult),
                     reads=[vpb, vftb], writes=[vpb])
                k.op("pool", lambda e: e.tensor_tensor(out=vp[:], in0=vp[:], in1=v_[:], op=ALU.add),
                     reads=[vpb, vb], writes=[vpb])
                vq, vqb = vp, vpb
            k.op("dve", lambda e: e.tensor_scalar(out=kkr[:], in0=k_[:], scalar1=pc("kk", hp), scalar2=None, op0=ALU.mult),
                 reads=[kb, parb], writes=[kkrb])
            k.op("act", lambda e: e.activation(out=sq[:], in_=kkr[:], func=AF.Square), reads=[kkrb], writes=[sqb])
            p, pb = prP.get()
            k.op("pe", lambda e, p=p: e.matmul(p[:], lhsT=c.bones[:], rhs=sq[:], start=True, stop=True),
                 reads=[c.bonesb, sqb], writes=[pb])
            k.op("act", lambda e, p=p: e.activation(out=rn[:], in_=p[:], func=AF.Sqrt, bias=lnxe[:, 1:2]),
                 reads=[pb, lnxeb], writes=[rnb])
            k.op("dve", lambda e: e.reciprocal(out=rn[:], in_=rn[:]), reads=[rnb], writes=[rnb])
            k.op("dve", lambda e: e.tensor_tensor(out=kk[:], in0=kkr[:], in1=rn[:], op=ALU.mult), reads=[kkrb, rnb], writes=[kkb])
            k.op("dve", lambda e: e.tensor_scalar(out=tA[:], in0=aa[:], scalar1=pc("ka", hp), scalar2=omka[:, hp:hp + 1],
                                                  op0=ALU.mult, op1=ALU.add), reads=[aab, parb, omkab], writes=[tAb])
            k.op("pool", lambda e: e.tensor_tensor(out=kp[:], in0=k_[:], in1=tA[:], op=ALU.mult), reads=[kb, tAb], writes=[kpb])
            k.op("pool", lambda e: e.tensor_tensor(out=bs[:], in0=kk[:], in1=aa[:], op=ALU.mult), reads=[kkb, aab], writes=[bsb])
            k.op("dve", lambda e: e.scalar_tensor_tensor(out=rk[:], in0=r_[:], scalar=pc("rk", hp), in1=kp[:],
                                                         op0=ALU.mult, op1=ALU.mult), reads=[rb, kpb, parb], writes=[rkb])
            p, pb = prP.get()
            k.op("pe", lambda e, p=p: e.matmul(p[:], lhsT=c.bones[:], rhs=rk[:], start=True, stop=True),
                 reads=[c.bonesb, rkb], writes=[pb])
            k.op("dve", lambda e, p=p: e.tensor_tensor(out=bon[:, tt % 2, hp, :], in0=p[:], in1=vq[:], op=ALU.mult),
                 reads=[pb, vqb], partial=[bonb])
            k.op("dve", lambda e: e.tensor_tensor_scan(out=cs[:], data0=c.ones3[:, 0:4, :].rearrange("p a b -> p (a b)"), data1=sgm[:],
                                                       initial=0.0, op0=ALU.mult, op1=ALU.add),
                 reads=[c.ones3b, sgmb], writes=[csb])
            k.op("pool", lambda e: e.memset(basec[:, 0:1], 0.0), partial=[basecb])
            k.op("pool", lambda e: e.tensor_copy(out=basec[:, 1:8], in_=cs[:].rearrange("p (c t) -> p c t", t=64)[:, 0:7, 63]),
                 reads=[csb], partial=[basecb])
            k.op("dve", lambda e: e.tensor_tensor(out=q[:], in0=cs[:].rearrange("p (c t) -> p c t", t=64),
                                                  in1=basec[:].unsqueeze(2).to_broadcast([128, 8, 64]), op=ALU.subtract),
                 reads=[csb, basecb], writes=[qb])
            k.op("pool", lambda e: e.tensor_tensor(out=qm[:], in0=q[:], in1=sgm[:].rearrange("p (c t) -> p c t", t=64), op=ALU.subtract),
                 reads=[qb, sgmb], writes=[qmb])
            k.op("act", lambda e: e.activation(out=E1[:], in_=qm[:], func=AF.Exp, scale=-C0), reads=[qmb], writes=[E1b])
            k.op("act", lambda e: e.activation(out=E2[:], in_=q[:], func=AF.Exp, scale=C0), reads=[qb], writes=[E2b])
            k.op("act", lambda e: e.activation(out=E3[:], in_=q[:], func=AF.Exp, scale=-C0), reads=[qb], writes=[E3b])
            k.op("pool", lambda e: e.tensor_tensor(out=qm[:], in0=q[:], in1=q[:, :, 63:64].to_broadcast([128, 8, 64]), op=ALU.subtract),
                 reads=[qb, E1b], writes=[qmb])
            k.op("act", lambda e: e.activation(out=Eh[:], in_=qm[:], func=AF.Exp, scale=C0), reads=[qmb], writes=[Ehb])
            c3 = lambda t: t[:].rearrange("p (c t) -> p c t", t=64)
            k.op("dve", lambda e: e.scalar_tensor_tensor(out=R32(AR[:, :, 0:64]), in0=c3(kk), scalar=-1.0, in1=E1[:], op0=ALU.mult, op1=ALU.mult),
                 reads=[kkb, E1b], partial=[ARb])
            k.op("pool", lambda e: e.tensor_tensor(out=R32(AR[:, :, 64:128]), in0=c3(r_), in1=E3[:], op=ALU.mult), reads=[rb, E3b], partial=[ARb])
            k.op("dve", lambda e: e.tensor_tensor(out=R32(BtT[:]), in0=c3(bs), in1=E2[:], op=ALU.mult), reads=[bsb, E2b], writes=[BtTb])
            k.op("pool", lambda e: e.tensor_tensor(out=R32(KtT[:]), in0=c3(kp), in1=E2[:], op=ALU.mult), reads=[kpb, E2b], writes=[KtTb])
            k.op("dve", lambda e: e.tensor_tensor(out=BhT[:], in0=c3(bs), in1=Eh[:], op=ALU.mult), reads=[bsb, Ehb], writes=[BhTb])
            k.op("pool", lambda e: e.tensor_tensor(out=KhT[:], in0=c3(kp), in1=Eh[:], op=ALU.mult), reads=[kpb, Ehb], writes=[KhTb])
            for (src, srcb, srcf, dst, dstb, dstf) in [
                    (AR, ARb, lambda cc: AR[:, cc, 0:64], Xtok, Xtokb, lambda: Xtok[:, :, 0:64]),
                    (BhT, BhTb, lambda cc: BhT[:, cc, :], Bhk, Bhkb, lambda: Bhk[:]),
                    (KhT, KhTb, lambda cc: KhT[:, cc, :], Khk, Khkb, lambda: Khk[:]),
                    (vq, vqb, lambda cc: vq[:, cc * 64:(cc + 1) * 64], Vtok, Vtokb, lambda: Vtok[:])]:
                p, pb = prP.get()
                first = True
                for cc in range(8):
                    for h in range(2):
                        k.op("pe", lambda e, p=p, h=h, cc=cc, srcf=srcf: e.matmul(
                            p[HS(h), cc * 64:(cc + 1) * 64], lhsT=srcf(cc)[HS(h)], rhs=c.ident[HS(h), HS(h)], start=True, stop=True),
                            reads=[srcb, c.identb], writes=[pb] if first else (), partial=() if first else [pb])
                        first = False
                k.op("act", lambda e, p=p, dstf=dstf: e.copy(out=R32(dstf()), in_=p[:].rearrange("p (c t) -> p c t", t=64)),
                     reads=[pb], partial=[dstb])
            k.op("pool", lambda e: e.tensor_copy(out=gam[:], in_=E3[:, :, 63]), reads=[E3b], writes=[gamb])

        def C_body(tt, hp):
            ifs = IF[(tt * 4 + hp) % 2]
            AR, ARb = ifs["AR"]; BtT, BtTb = ifs["BtT"]; KtT, KtTb = ifs["KtT"]; Xtok, Xtokb = ifs["Xtok"]
            Bhk, Bhkb = ifs["Bhk"]; Khk, Khkb = ifs["Khk"]; Vtok, Vtokb = ifs["Vtok"]; gam, gamb = ifs["gam"]
            ts_ = slice(tt * 512, (tt + 1) * 512)
            p, pb = prC.get()
            umm(k, p, pb, range(8), 64, [(lambda h, cc: AR[HS(h), cc, 0:64], lambda h, cc: BtT[HS(h), cc, :])], [ARb, BtTb])
            Lc, Lcb = Lp[0]
            k.op("dve", lambda e, p=p: e.tensor_tensor(out=R32(Lc[:]), in0=p[:].rearrange("p (c t) -> p c t", t=64), in1=c.maskL[:], op=ALU.mult),
                 reads=[pb, c.maskLb], writes=[Lcb])
            for (lt, ltb, dst, dstb) in [(BtT, BtTb, NM, NMb), (KtT, KtTb, KM, KMb)]:
                for half in range(2):
                    p, pb = prC.get()
                    umm(k, p, pb, range(half * 4, half * 4 + 4), 128,
                        [(lambda h, cc, lt=lt: lt[HS(h), cc, :], lambda h, cc: AR[HS(h), cc, :])], [ltb, ARb])
                    k.op("dve", lambda e, p=p, dst=dst, half=half: e.tensor_tensor(
                        out=R32(dst[:, half * 4:half * 4 + 4, :]), in0=p[:].rearrange("p (c t) -> p c t", t=128), in1=c.maskNM[:], op=ALU.mult),
                        reads=[pb, c.maskNMb], partial=[dstb])
            Nc, Ncb = Np[0]
            k.op("pool", lambda e: e.tensor_copy(out=R32(Nc[:]), in_=NM[:, :, 0:64]), reads=[NMb], writes=[Ncb])
            Pc, Pcb = Pp[0]
            k.op("pool", lambda e: e.tensor_tensor(out=R32(Pc[:]), in0=NM[:, :, 0:64], in1=c.i64[:], op=ALU.add), reads=[NMb, c.i64b], writes=[Pcb])
            for j in range(5):
                Ln, Lnb = Lp[(j + 1) % 2]
                Nn, Nnb = Np[(j + 1) % 2]
                Pn, Pnb = Pp[(j + 1) % 2]
                p, pb = prC.get()
                umm(k, p, pb, range(8), 64, [(lambda h, cc, Nc=Nc: Nc[HS(h), cc, :], lambda h, cc, Lc=Lc: Lc[HS(h), cc, :])], [Ncb, Lcb])
                if j < 4:
                    p2, p2b = prC.get()
                    umm(k, p2, p2b, range(8), 64, [(lambda h, cc, Lc=Lc: Lc[HS(h), cc, :], lambda h, cc, Nc=Nc: Nc[HS(h), cc, :])], [Ncb, Lcb])
                k.op("act", lambda e, p=p, Ln=Ln: e.copy(out=R32(Ln[:]), in_=p[:].rearrange("p (c t) -> p c t", t=64)), reads=[pb], writes=[Lnb])
                if j < 4:
                    k.op("dve", lambda e, p2=p2, Nn=Nn: e.tensor_copy(out=R32(Nn[:]), in_=p2[:].rearrange("p (c t) -> p c t", t=64)), reads=[p2b], writes=[Nnb])
                p3, p3b = prC.get()
                umm(k, p3, p3b, range(8), 64, [(lambda h, cc, Ln=Ln: Ln[HS(h), cc, :], lambda h, cc, Pc=Pc: Pc[HS(h), cc, :])], [Lnb, Pcb])
                k.op("dve", lambda e, p3=p3, Pn=Pn, Pc=Pc: e.tensor_tensor(out=R32(Pn[:]), in0=p3[:].rearrange("p (c t) -> p c t", t=64), in1=Pc[:], op=ALU.add),
                     reads=[p3b, Pcb], writes=[Pnb])
                Lc, Lcb, Nc, Ncb, Pc, Pcb = Ln, Lnb, Nn, Nnb, Pn, Pnb
            p, pb = prC.get()
            umm(k, p, pb, range(8), 64, [(lambda h, cc: KM[HS(h), cc, 0:64], lambda h, cc: Vtok[HS(h), cc, :])], [KMb, Vtokb])
            k.op("act", lambda e, p=p: e.copy(out=R32(Xtok[:, :, 64:128]), in_=p[:].rearrange("p (c t) -> p c t", t=64)), reads=[pb], partial=[Xtokb])
            for half in range(2):
                p, pb = prC.get()
                umm(k, p, pb, range(half * 4, half * 4 + 4), 128,
                    [(lambda h, cc, Pc=Pc: Pc[HS(h), cc, :], lambda h, cc: Xtok[HS(h), cc, :])], [Pcb, Xtokb])
                eng = "act" if half == 0 else "dve"
                if eng == "act":
                    k.op("act", lambda e, p=p, half=half: e.copy(out=R32(WU[:, half * 4:half * 4 + 4, :]), in_=p[:].rearrange("p (c t) -> p c t", t=128)),
                         reads=[pb], partial=[WUb])
                else:
                    k.op("dve", lambda e, p=p, half=half: e.tensor_copy(out=R32(WU[:, half * 4:half * 4 + 4, :]), in_=p[:].rearrange("p (c t) -> p c t", t=128)),
                         reads=[pb], partial=[WUb])
            k.coop_wait("S")
            p, pb = prC.get()
            umm(k, p, pb, range(8), 64, [(lambda h, cc: WU[HS(h), cc, 0:64], lambda h, cc: NM[HS(h), cc, 64:128])], [WUb, NMb])
            k.op("dve", lambda e, p=p: e.tensor_tensor(out=R2T[:, hp], in0=p[:].rearrange("p (c t) -> p c t", t=64), in1=AR[:, :, 64:128], op=ALU.add),
                 reads=[pb, ARb], partial=[R2Tb])
            p, pb = prC.get()
            umm(k, p, pb, range(8), 64, [(lambda h, cc: WU[HS(h), cc, 64:128], lambda h, cc: NM[HS(h), cc, 64:128]),
                                         (lambda h, cc: Vtok[HS(h), cc, :], lambda h, cc: KM[HS(h), cc, 64:128])], [WUb, NMb, Vtokb, KMb])
            k.op("act", lambda e, p=p: e.copy(out=Y0T[:, hp], in_=p[:].rearrange("p (c t) -> p c t", t=64)), reads=[pb], partial=[Y0Tb])
            p, pb = prC.get()
            umm(k, p, pb, range(8), 64, [(lambda h, cc: WU[HS(h), cc, 0:64], lambda h, cc: Bhk[HS(h), cc, :])], [WUb, Bhkb])
            k.op("pool", lambda e: e.tensor_tensor(out=Igam[:], in0=c.i64[:], in1=gam[:].unsqueeze(2).to_broadcast([128, 8, 64]), op=ALU.mult),
                 reads=[c.i64b, gamb], writes=[Igamb])
            k.op("dve", lambda e, p=p: e.tensor_tensor(out=GT[:, hp], in0=p[:].rearrange("p (c t) -> p c t", t=64), in1=Igam[:], op=ALU.add),
                 reads=[pb, Igamb], partial=[GTb])
            p, pb = prC.get()
            umm(k, p, pb, range(8), 64, [(lambda h, cc: Bhk[HS(h), cc, :], lambda h, cc: WU[HS(h), cc, 64:128]),
                                         (lambda h, cc: Khk[HS(h), cc, :], lambda h, cc: Vtok[HS(h), cc, :])], [Bhkb, WUb, Khkb, Vtokb])
            k.op("act", lambda e, p=p: e.copy(out=HH[:, hp], in_=p[:].rearrange("p (c t) -> p c t", t=64)), reads=[pb], partial=[HHb])

        def S_body(tt):
            ts_ = slice(tt * 512, (tt + 1) * 512)
            for cc in range(8):
                pY, pYb = prS.get()
                pS, pSb = prS.get()
                first = True
                for hp in range(4):
                    for h in range(2):
                        k.op("pe", lambda e, hp=hp, h=h, pY=pY: e.matmul(pY[HS(h), hp * 64:(hp + 1) * 64], lhsT=S[HS(h), hp, :], rhs=R2T[HS(h), hp, cc, :],
                                                                           start=True, stop=True),
                             reads=[Sb, R2Tb], writes=[pYb] if first else (), partial=() if first else [pYb])
                        k.op("pe", lambda e, hp=hp, h=h, pS=pS: e.matmul(pS[HS(h), hp * 64:(hp + 1) * 64], lhsT=GT[HS(h), hp, cc, :], rhs=S[HS(h), hp, :],
                                                                           start=True, stop=True),
                             reads=[Sb, GTb], writes=[pSb] if first else (), partial=() if first else [pSb])
                        first = False
                k.op("dve", lambda e, pY=pY: e.tensor_tensor(out=yT[:, :, cc * 64:(cc + 1) * 64], in0=pY[:, 0:256].rearrange("p (a t) -> p a t", t=64),
                                                              in1=Y0T[:, :, cc, :], op=ALU.add), reads=[pYb, Y0Tb], partial=[yTb])
                k.op("dve", lambda e, pS=pS: e.tensor_tensor(out=S[:], in0=pS[:, 0:256].rearrange("p (a t) -> p a t", t=64),
                                                              in1=HH[:, :, cc, :], op=ALU.add), reads=[pSb, HHb], writes=[Sb])
            for hp in range(4):
                p, pb = prS.get()
                k.op("pe", lambda e, p=p: e.matmul(p[:], lhsT=c.bones[:], rhs=yT[:, hp, :], start=True, stop=True),
                     reads=[c.bonesb, yTb], writes=[pb])
                k.op("dve", lambda e, p=p: e.scalar_tensor_tensor(out=dd[:], in0=p[:], scalar=-1.0 / 64, in1=yT[:, hp, :], op0=ALU.mult, op1=ALU.add),
                     reads=[pb, yTb], writes=[ddb])
                k.op("act", lambda e: e.activation(out=sq[:], in_=dd[:], func=AF.Square), reads=[ddb], writes=[sqb])
                p, pb = prS.get()
                k.op("pe", lambda e, p=p: e.matmul(p[:], lhsT=c.bones[:], rhs=sq[:], start=True, stop=True),
                     reads=[c.bonesb, sqb], writes=[pb])
                k.op("act", lambda e, p=p: e.activation(out=rn[:], in_=p[:], func=AF.Sqrt, scale=1.0 / 64, bias=lnxe[:, 0:1]),
                     reads=[pb, lnxeb], writes=[rnb])
                k.op("dve", lambda e: e.reciprocal(out=rn[:], in_=rn[:]), reads=[rnb], writes=[rnb])
                k.op("dve", lambda e: e.tensor_tensor(out=dd[:], in0=dd[:], in1=rn[:], op=ALU.mult), reads=[ddb, rnb], writes=[ddb])
                k.op("dve", lambda e: e.tensor_scalar(out=dd[:], in0=dd[:], scalar1=pc("lng", hp), scalar2=pc("lnb", hp), op0=ALU.mult, op1=ALU.add),
                     reads=[ddb, parb], writes=[ddb])
                k.op("pool", lambda e: e.tensor_tensor(out=dd[:], in0=dd[:], in1=bon[:, tt % 2, hp, :], op=ALU.add), reads=[ddb, bonb], writes=[ddb])
                y_, yb = yo[hp % 2]
                k.op("pool", lambda e, y_=y_: e.tensor_tensor(out=y_[:], in0=dd[:], in1=gg[:, tt % 2, hp, :], op=ALU.mult), reads=[ddb, ggb], writes=[yb])
                k.dma("sp", mixT[hp, :, ts_], y_[:], yb, reads=[yb])

        its = [(tt, hp) for tt in range(NT) for hp in range(4)]
        P_body(*its[0])
        pendS = None
        for i, (tt, hp) in enumerate(its):
            streams = [("C", lambda tt=tt, hp=hp: C_body(tt, hp))]
            if i + 1 < len(its):
                streams.append(("P", lambda n=its[i + 1]: P_body(*n)))
            if pendS is not None:
                streams.append(("S", lambda t_=pendS: S_body(t_)))
                pendS = None
            k.coop_run(streams)
            if hp == 3:
                pendS = tt
        S_body(pendS)
        k.barrier()

TWO_PI = float(2 * np.pi)
CW1 = 6.28125
CW2 = float(2 * np.pi - 6.28125)


def host_consts():
    f32 = np.float32
    invf = (np.float32(10000.0) ** (-np.arange(0, 32, 2, dtype=np.float32) / np.float32(32))).astype(f32)
    cc = np.zeros((128, 40), f32)
    p = np.arange(128)
    cc[:, 0] = invf[p % 16]
    cc[:, 1] = np.where((p % 32) < 16, -1.0, 1.0)
    wins = {0: (2, 4), 1: (8, 16)}
    for pc in range(2):
        w = np.where(p < 64, wins[pc][0], wins[pc][1]).astype(f32)
        cc[:, 2 + pc] = 1.0 / w
        for t in range(16):
            cc[:, 4 + pc * 16 + t] = w / np.minimum(t + 1, w)
    return cc


def phase_rope_tables(nc, k, c, pos, csT, snT):
    with ExitStack() as es:
        pi_, pib = sb(nc, es, "posi", [128, 1024], I32)
        ang, angb = sb(nc, es, "ang", [128, 1024])
        a2, a2b = sb(nc, es, "ang2", [128, 1024])
        qi, qib = sb(nc, es, "qi", [128, 1024], I32)
        qf, qfb = sb(nc, es, "qf", [128, 1024])
        m, mb = sb(nc, es, "msk", [128, 1024])
        o, ob = sb(nc, es, "tab", [128, 1024])
        halfpi, hpb = sb(nc, es, "halfpi", [128, 2])
        for piece in range(4):
            sl = slice(piece * 1024, (piece + 1) * 1024)
            k.dma("sp", pi_[:], pos[sl].partition_broadcast(128), pib, writes=[pib])
            k.op("dve", lambda e: e.tensor_copy(out=ang[:], in_=pi_[:]), reads=[pib], writes=[angb])
            k.op("dve", lambda e: e.tensor_scalar(out=ang[:], in0=ang[:], scalar1=c.hc[:, 0:1], scalar2=None, op0=ALU.mult),
                 reads=[angb, c.hcb], writes=[angb])
            for which, dst in ((0, snT), (1, csT)):
                if which == 1:
                    k.op("dve", lambda e: e.tensor_scalar(out=a2[:], in0=ang[:], scalar1=float(np.pi / 2), scalar2=None, op0=ALU.add),
                         reads=[angb], writes=[a2b])
                else:
                    k.op("dve", lambda e: e.tensor_copy(out=a2[:], in_=ang[:]), reads=[angb], writes=[a2b])
                k.op("dve", lambda e: e.tensor_scalar(out=qf[:], in0=a2[:], scalar1=float(1.0 / TWO_PI), scalar2=None, op0=ALU.mult),
                     reads=[a2b], writes=[qfb])
                k.op("dve", lambda e: e.tensor_copy(out=qi[:], in_=qf[:]), reads=[qfb], writes=[qib])
                k.op("dve", lambda e: e.tensor_copy(out=qf[:], in_=qi[:]), reads=[qib], writes=[qfb])
                k.op("dve", lambda e: e.scalar_tensor_tensor(out=a2[:], in0=qf[:], scalar=-CW1, in1=a2[:], op0=ALU.mult, op1=ALU.add),
                     reads=[qfb, a2b], writes=[a2b])
                k.op("dve", lambda e: e.scalar_tensor_tensor(out=a2[:], in0=qf[:], scalar=-CW2, in1=a2[:], op0=ALU.mult, op1=ALU.add),
                     reads=[qfb, a2b], writes=[a2b])
                k.op("dve", lambda e: e.tensor_scalar(out=m[:], in0=a2[:], scalar1=float(np.pi), scalar2=-TWO_PI, op0=ALU.is_gt, op1=ALU.mult),
                     reads=[a2b], writes=[mb])
                k.op("dve", lambda e: e.tensor_tensor(out=a2[:], in0=a2[:], in1=m[:], op=ALU.add), reads=[a2b, mb], writes=[a2b])
                k.op("dve", lambda e: e.tensor_scalar(out=m[:], in0=a2[:], scalar1=float(-np.pi), scalar2=TWO_PI, op0=ALU.is_lt, op1=ALU.mult),
                     reads=[a2b], writes=[mb])
                k.op("dve", lambda e: e.tensor_tensor(out=a2[:], in0=a2[:], in1=m[:], op=ALU.add), reads=[a2b, mb], writes=[a2b])
                k.op("dve", lambda e: e.tensor_scalar(out=a2[:], in0=a2[:], scalar1=float(np.pi), scalar2=float(-np.pi), op0=ALU.min, op1=ALU.max),
                     reads=[a2b], writes=[a2b])
                k.op("act", lambda e: e.activation(out=o[:], in_=a2[:], func=AF.Sin), reads=[a2b], writes=[ob])
                if which == 0:
                    k.op("dve", lambda e: e.tensor_scalar(out=o[:], in0=o[:], scalar1=c.hc[:, 1:2], scalar2=None, op0=ALU.mult),
                         reads=[ob, c.hcb], writes=[ob])
                k.dma("sp", dst[:, sl], o[:], ob, reads=[ob])
        k.barrier()


def phase_pool_mla(nc, k, c, L, zT, csT, snT, mixT, l):
    SC = float(96 ** -0.5)
    with ExitStack() as es:
        par, parb = c.par, c.parb
        pc = lambda nm, i: par[:, PCOL[nm][0] + i:PCOL[nm][0] + i + 1]
        T2 = lambda nm, shape=[128, 512], dt=F32: sb(nc, es, nm, shape, dt)
        poolw, poolwb = T2("poolw", [128, 2, 128], BF16)
        k.dma("pool", poolw[:], L["poolw"], poolwb, writes=[poolwb])
        wq, wqb = T2("wq", [128, 2, 768], BF16)
        k.dma("pool", wq[:], L["wq"], wqb, writes=[wqb])
        wkv, wkvb = T2("wkv", [128, 512], BF16)
        k.dma("pool", wkv[:], L["wkv"], wkvb, writes=[wkvb])
        KTbs = [Buf("KTb%d" % i) for i in range(NT)]
        V1bs = [Buf("V1b%d" % i) for i in range(NT)]
        KT, KTb = T2("KT", [128, 4, T], BF16)
        V1, V1b = T2("V1", [128, 32, 4, 65], BF16)
        k.op("pool", lambda e: e.memset(V1[:, :, :, 64:65], 1.0), partial=V1bs)
        tri, trib = T2("tri", [128, 128], BF16)
        k.op("pool", lambda e: e.affine_select(out=tri[:], in_=c.onesbf[:], pattern=[[1, 128]], compare_op=ALU.is_ge, fill=0.0,
                                               base=0, channel_multiplier=-1), reads=[c.onesbfb], writes=[trib])
        ub = [T2("ub%d" % i, [128, 528]) for i in range(2)]
        s2, s2b = T2("s2", [128, 528]); s4, s4b = T2("s4", [128, 528]); s8, s8b = T2("s8", [128, 528])
        pp, ppb = T2("pp"); ppbf, ppbfb = T2("ppbf", dt=BF16)
        yo = [T2("pyo%d" % i, dt=BF16) for i in range(2)]
        ql, qlb = T2("ql", [128, 2, 512]); kvl, kvlb = T2("kvl")
        krA, krAb = T2("krA"); krB, krBb = T2("krB"); cst, cstb = T2("cst"); snt, sntb = T2("snt")
        sqb_, sqbb = T2("msq", [128, 2, 512], BF16); rs, rsb = T2("mrs")
        qn, qnb = T2("qn", [128, 2, 512], BF16); kvn, kvnb = T2("kvn", dt=BF16)
        kr, krb = T2("kr"); t1, t1b = T2("mt1"); t2, t2b = T2("mt2")
        pre, preb = T2("pre"); QTs = [T2("QT%d" % i, [128, 4, 512], BF16) for i in range(2)]
        PT = [T2("PT%d" % i, dt=BF16) for i in range(2)]
        rec, recb = T2("rec", [128, 4]); ytok, ytokb = T2("ytok", [128, 4, 256])
        ymT = [T2("ymT%d" % i, [128, 512], BF16) for i in range(2)]
        for u_, ubb in ub:
            k.op("pool", lambda e, u_=u_: e.memset(u_[:], 0.0), writes=[ubb])
        prm = PsRot(c, [4, 5, 6])
        prl = PsRot(c, [7])
        hcol = lambda i: c.hc[:, i:i + 1]

        def rms_rows(src, srcb, nrows, nko, dim):
            p, pb = prm.get()
            for ko in range(nko):
                s_ap = src[0:nrows, ko, :] if nko > 1 else src[0:nrows]
                q_ap = sqb_[0:nrows, ko, :]
                k.op("act", lambda e, s_ap=s_ap, q_ap=q_ap: e.activation(out=q_ap, in_=s_ap, func=AF.Square), reads=[srcb],
                     writes=[sqbb] if ko == 0 else (), partial=[sqbb] if ko else ())
            for ko in range(nko):
                k.op("pe", lambda e, p=p, ko=ko: e.matmul(p[0:nrows, :], lhsT=c.onesbf[0:nrows, 0:nrows], rhs=sqb_[0:nrows, ko, :],
                                                          start=(ko == 0), stop=(ko == nko - 1)),
                     reads=[sqbb, c.onesbfb], writes=[pb] if ko == 0 else (), partial=[pb] if ko else ())
            k.op("act", lambda e, p=p: e.activation(out=rs[0:nrows], in_=p[0:nrows, :], func=AF.Sqrt, scale=1.0 / dim, bias=c.epsc[0:nrows, 0:1]),
                 reads=[pb, c.epscb], writes=[rsb])
            k.op("dve", lambda e: e.reciprocal(out=rs[0:nrows], in_=rs[0:nrows]), reads=[rsb], writes=[rsb])

        def pool_body(tt):
            ts_ = slice(tt * 512, (tt + 1) * 512)
            for pcn in range(2):
                u_, ubb = ub[pcn]
                if tt > 0:
                    k.op("pool", lambda e, u_=u_: e.tensor_copy(out=u_[:, 0:16], in_=u_[:, 512:528]), reads=[ubb], writes=[ubb])
                k.dma("sp", u_[:, 16:528], zT[C_POOL + pcn, :, ts_], ubb, partial=[ubb], reads=[ubb])
                k.op("pool", lambda e, u_=u_: e.tensor_tensor(out=s2[:, 1:528], in0=u_[:, 1:528], in1=u_[:, 0:527], op=ALU.add), reads=[ubb], writes=[s2b])
                if pcn == 0:
                    k.op("pool", lambda e: e.tensor_tensor(out=s2[64:128, 3:528], in0=s2[64:128, 3:528], in1=s2[64:128, 1:526], op=ALU.add),
                         reads=[s2b], writes=[s2b])
                    res, resb = s2, s2b
                else:
                    k.op("pool", lambda e: e.tensor_tensor(out=s4[:, 3:528], in0=s2[:, 3:528], in1=s2[:, 1:526], op=ALU.add), reads=[s2b], writes=[s4b])
                    k.op("pool", lambda e: e.tensor_tensor(out=s8[:, 7:528], in0=s4[:, 7:528], in1=s4[:, 3:524], op=ALU.add), reads=[s4b], writes=[s8b])
                    k.op("pool", lambda e: e.tensor_tensor(out=s8[64:128, 15:528], in0=s8[64:128, 15:528], in1=s8[64:128, 7:520], op=ALU.add),
                         reads=[s8b], writes=[s8b])
                    res, resb = s8, s8b
                if tt == 0:
                    k.op("dve", lambda e, res=res, pcn=pcn: e.tensor_tensor(out=res[:, 16:32], in0=res[:, 16:32], in1=c.hc[:, 4 + pcn * 16:20 + pcn * 16], op=ALU.mult),
                         reads=[resb, c.hcb], writes=[resb])
                k.op("dve", lambda e, res=res, u_=u_, pcn=pcn: e.scalar_tensor_tensor(out=ppbf[:], in0=res[:, 16:528], scalar=hcol(2 + pcn), in1=u_[:, 16:528],
                                                                               op0=ALU.mult, op1=ALU.subtract), reads=[resb, ubb, c.hcb], writes=[ppbfb])
                p, pb = prl.get()
                k.op("pe", lambda e, p=p, pcn=pcn: e.matmul(p[:], lhsT=poolw[:, pcn, :], rhs=ppbf[:], start=True, stop=True), reads=[poolwb, ppbfb], writes=[pb])
                y_, yb = yo[pcn]
                k.op("act", lambda e, p=p, y_=y_, pcn=pcn: e.activation(out=y_[:], in_=p[:], func=AF.Copy, scale=pc("pscale", pcn)), reads=[pb, parb], writes=[yb])
                k.dma("sp", mixT[4 + pcn, :, ts_], y_[:], yb, reads=[yb])

        def prep_body(tt):
            ts_ = slice(tt * 512, (tt + 1) * 512)
            QT, QTb = QTs[tt % 2]
            k.dma("sp", ql[:], zT[C_QLAT:C_QLAT + 2, :, ts_].rearrange("k p t -> p k t"), qlb, writes=[qlb])
            k.dma("sp", kvl[:], zT[C_KVLAT, :, ts_], kvlb, writes=[kvlb])
            k.dma("sp", krA[64:96], zT[C_KROPE, 0:32, ts_], krAb, writes=[krAb])
            k.dma("sp", krB[64:96], zT[C_KROPE, 32:64, ts_], krBb, writes=[krBb])
            k.dma("sp", cst[:], csT[:, ts_], cstb, writes=[cstb])
            k.dma("sp", snt[:], snT[:, ts_], sntb, writes=[sntb])
            rms_rows(ql, qlb, 128, 2, 256.0)
            for ko in range(2):
                k.op("dve", lambda e, ko=ko: e.scalar_tensor_tensor(out=qn[:, ko, :], in0=ql[:, ko, :], scalar=pc("qlg", ko), in1=rs[:],
                                                                    op0=ALU.mult, op1=ALU.mult), reads=[qlb, rsb, parb],
                     writes=[qnb] if ko == 0 else (), partial=[qnb] if ko else ())
            rms_rows(kvl, kvlb, 128, 1, 128.0) if False else None
            p, pb = prm.get()
            k.op("act", lambda e: e.activation(out=sqb_[:, 0, :], in_=kvl[:], func=AF.Square), reads=[kvlb], writes=[sqbb])
            k.op("pe", lambda e, p=p: e.matmul(p[:], lhsT=c.onesbf[:], rhs=sqb_[:, 0, :], start=True, stop=True), reads=[sqbb, c.onesbfb], writes=[pb])
            k.op("act", lambda e, p=p: e.activation(out=rs[:], in_=p[:], func=AF.Sqrt, scale=1.0 / 128, bias=c.epsc[:, 0:1]), reads=[pb, c.epscb], writes=[rsb])
            k.op("dve", lambda e: e.reciprocal(out=rs[:], in_=rs[:]), reads=[rsb], writes=[rsb])
            k.op("dve", lambda e: e.scalar_tensor_tensor(out=kvn[:], in0=kvl[:], scalar=pc("kvlg", 0), in1=rs[:], op0=ALU.mult, op1=ALU.mult),
                 reads=[kvlb, rsb, parb], writes=[kvnb])
            R_ = slice(64, 96)
            k.op("dve", lambda e: e.tensor_tensor(out=t1[R_], in0=krA[R_], in1=cst[R_], op=ALU.mult), reads=[krAb, cstb], writes=[t1b])
            k.op("dve", lambda e: e.tensor_tensor(out=t2[R_], in0=krB[R_], in1=snt[R_], op=ALU.mult), reads=[krBb, sntb], writes=[t2b])
            k.op("dve", lambda e: e.tensor_tensor(out=kr[R_], in0=t1[R_], in1=t2[R_], op=ALU.add), reads=[t1b, t2b], writes=[krb])
            for j in range(4):
                blk = tt * 4 + j
                p, pb = prm.get()
                k.op("pe", lambda e, p=p, j=j: e.matmul(p[:, 0:256], lhsT=kvn[:, j * 128:(j + 1) * 128], rhs=wkv[:, 256:512], start=True, stop=True),
                     reads=[kvnb, wkvb], writes=[pb])
                k.op("act", lambda e, p=p, blk=blk: e.copy(out=V1[:, blk, :, 0:64], in_=p[:, 0:256].rearrange("p (h d) -> p h d", d=64)),
                     reads=[pb], partial=[V1bs[tt]])
            for h in range(4):
                p, pb = prm.get()
                k.op("pe", lambda e, p=p, h=h: e.matmul(p[0:64, :], lhsT=wkv[:, h * 64:(h + 1) * 64], rhs=kvn[:], start=True, stop=True),
                     reads=[wkvb, kvnb], writes=[pb])
                k.op("act", lambda e, p=p: e.copy(out=pre[0:64], in_=p[0:64, :]), reads=[pb], writes=[preb])
                k.op("pool", lambda e: e.tensor_copy(out=pre[R_], in_=kr[R_]), reads=[krb], partial=[preb])
                p, pb = prm.get()
                k.op("act", lambda e: e.activation(out=sqb_[0:96, 0, :], in_=pre[0:96], func=AF.Square), reads=[preb], writes=[sqbb])
                k.op("pe", lambda e, p=p: e.matmul(p[0:96, :], lhsT=c.onesbf[0:96, 0:96], rhs=sqb_[0:96, 0, :], start=True, stop=True),
                     reads=[sqbb, c.onesbfb], writes=[pb])
                k.op("act", lambda e, p=p: e.activation(out=rs[0:96], in_=p[0:96, :], func=AF.Sqrt, scale=1.0 / 96, bias=c.epsc[0:96, 0:1]),
                     reads=[pb, c.epscb], writes=[rsb])
                k.op("dve", lambda e: e.reciprocal(out=rs[0:96], in_=rs[0:96]), reads=[rsb], writes=[rsb])
                k.op("dve", lambda e, h=h: e.scalar_tensor_tensor(out=KT[0:96, h, ts_], in0=pre[0:96], scalar=par[0:96, PCOL["qkk"][0]:PCOL["qkk"][0] + 1],
                                                                  in1=rs[0:96], op0=ALU.mult, op1=ALU.mult), reads=[preb, rsb, parb], partial=[KTbs[tt]])
                pA, pAb = prm.get()
                pB, pBb = prm.get()
                for ko in range(2):
                    k.op("pe", lambda e, pA=pA, ko=ko, h=h: e.matmul(pA[0:96, :], lhsT=wq[:, ko, (h * 2) * 96:(h * 2 + 1) * 96], rhs=qn[:, ko, :],
                                                                     start=(ko == 0), stop=(ko == 1)),
                         reads=[wqb, qnb], writes=[pAb] if ko == 0 else (), partial=[pAb] if ko else ())
                for ko in range(2):
                    k.op("pe", lambda e, pB=pB, ko=ko, h=h: e.matmul(pB[0:96, :], lhsT=wq[:, ko, (h * 2 + 1) * 96:(h * 2 + 2) * 96], rhs=qn[:, ko, :],
                                                                     start=(ko == 0), stop=(ko == 1)),
                         reads=[wqb, qnb], writes=[pBb] if ko == 0 else (), partial=[pBb] if ko else ())
                k.op("act", lambda e, pA=pA: e.copy(out=pre[0:64], in_=pA[0:64, :]), reads=[pAb], writes=[preb])
                k.op("dve", lambda e, pA=pA: e.tensor_tensor(out=t1[R_], in0=pA[R_, :], in1=cst[R_], op=ALU.mult), reads=[pAb, cstb], writes=[t1b])
                k.op("dve", lambda e, pB=pB: e.tensor_tensor(out=t2[R_], in0=pB[R_, :], in1=snt[R_], op=ALU.mult), reads=[pBb, sntb], writes=[t2b])
                k.op("dve", lambda e: e.tensor_tensor(out=pre[R_], in0=t1[R_], in1=t2[R_], op=ALU.add), reads=[t1b, t2b], partial=[preb])
                p, pb = prm.get()
                k.op("act", lambda e: e.activation(out=sqb_[0:96, 0, :], in_=pre[0:96], func=AF.Square), reads=[preb], writes=[sqbb])
                k.op("pe", lambda e, p=p: e.matmul(p[0:96, :], lhsT=c.onesbf[0:96, 0:96], rhs=sqb_[0:96, 0, :], start=True, stop=True),
                     reads=[sqbb, c.onesbfb], writes=[pb])
                k.op("act", lambda e, p=p: e.activation(out=rs[0:96], in_=p[0:96, :], func=AF.Sqrt, scale=1.0 / 96, bias=c.epsc[0:96, 0:1]),
                     reads=[pb, c.epscb], writes=[rsb])
                k.op("dve", lambda e: e.reciprocal(out=rs[0:96], in_=rs[0:96]), reads=[rsb], writes=[rsb])
                k.op("dve", lambda e: e.tensor_scalar(out=rs[0:96], in0=rs[0:96], scalar1=SC, scalar2=None, op0=ALU.mult), reads=[rsb], writes=[rsb])
                k.op("dve", lambda e, h=h: e.scalar_tensor_tensor(out=QT[0:96, h, :], in0=pre[0:96], scalar=par[0:96, PCOL["qkq"][0]:PCOL["qkq"][0] + 1],
                                                                  in1=rs[0:96], op0=ALU.mult, op1=ALU.mult), reads=[preb, rsb, parb], partial=[QTb])

        def attn_body(tt):
            ts_ = slice(tt * 512, (tt + 1) * 512)
            QT, QTb = QTs[tt % 2]
            for h in range(4):
                po, pob = c.ps[2 + h % 2], c.psb[2 + h % 2]
                nj = 4 * tt + 4
                for j in range(nj):
                    pS, pSb = c.ps[j % 2], c.psb[j % 2]
                    k.op("pe", lambda e, pS=pS, j=j, h=h: e.matmul(pS[:], lhsT=KT[0:96, h, j * 128:(j + 1) * 128], rhs=QT[0:96, h, :], start=True, stop=True),
                         reads=[KTbs[j // 4], QTb], writes=[pSb])
                    P_, Pb_ = PT[j % 2]
                    k.op("act", lambda e, pS=pS, P_=P_: e.activation(out=P_[:], in_=pS[:], func=AF.Exp), reads=[pSb], writes=[Pb_])
                    r = j - 4 * tt
                    if r >= 0:
                        k.op("pool", lambda e, P_=P_, r=r: e.tensor_tensor(out=P_[:, r * 128:(r + 1) * 128], in0=P_[:, r * 128:(r + 1) * 128], in1=tri[:], op=ALU.mult),
                             reads=[Pb_, trib], writes=[Pb_])
                    for i_ in range(max(r, 0), 4):
                        first = (j == 0 and i_ == max(r, 0))
                        k.op("pe", lambda e, P_=P_, i_=i_, j=j, h=h, po=po: e.matmul(po[:, i_ * 65:(i_ + 1) * 65], lhsT=P_[:, i_ * 128:(i_ + 1) * 128], rhs=V1[:, j, h, :],
                                                                                 start=(j == 0 and i_ == 0), stop=(j == 4 * tt + i_), skip_group_check=True),
                             reads=[Pb_, V1bs[j // 4]], writes=[pob] if first else (), partial=() if first else [pob])
                k.op("dve", lambda e, po=po: e.reciprocal(out=rec[:], in_=po[:, 0:260].rearrange("p (i d) -> p i d", d=65)[:, :, 64]), reads=[pob], writes=[recb])
                for i_ in range(4):
                    k.op("dve", lambda e, po=po, i_=i_, h=h: e.tensor_scalar(out=ytok[:, i_, h * 64:(h + 1) * 64], in0=po[:, i_ * 65:i_ * 65 + 64],
                                                                             scalar1=rec[:, i_:i_ + 1], scalar2=None, op0=ALU.mult),
                         reads=[pob, recb], partial=[ytokb])
            for half in range(2):
                p, pb = prm.get()
                for i_ in range(4):
                    k.op("pe", lambda e, p=p, i_=i_, half=half: e.transpose(out=p[:, i_ * 128:(i_ + 1) * 128], in_=ytok[:, i_, half * 128:(half + 1) * 128],
                                                                            identity=c.ident[:]),
                         reads=[ytokb, c.identb], writes=[pb] if i_ == 0 else (), partial=[pb] if i_ else ())
                y_, yb = ymT[half]
                k.op("act", lambda e, p=p, y_=y_: e.copy(out=y_[:], in_=p[:]), reads=[pb], writes=[yb])
                k.dma("sp", mixT[6 + half, :, ts_], y_[:], yb, reads=[yb])

        pool_body(0)
        prep_body(0)
        for tt in range(NT):
            streams = [("A", lambda tt=tt: attn_body(tt))]
            if tt + 1 < NT:
                streams.append(("P", lambda tt=tt: prep_body(tt + 1)))
                streams.append(("L", lambda tt=tt: pool_body(tt + 1)))
            k.coop_run(streams)
        k.barrier()

def phase_ffn(nc, k, c, L, xT, mixT, l, W):
    moe = (l == 1)
    if moe:
        NF = DFE // 128
        experts = [(W["moe_w_in_b"][e], W["moe_w_out_b"][e]) for e in range(NE)]
        F = DFE
    else:
        NF = DFF // 128
        experts = [(W["ffn_w_in_b"], W["ffn_w_out_b"])]
        F = DFF
    NG = NF // 2
    with ExitStack() as es:
        T2 = lambda nm, shape=[128, 512], dt=F32: sb(nc, es, nm, shape, dt)
        wout, woutb = T2("wout", [128, 8, 1024], BF16)
        for ko in range(8):
            k.dma("pool", wout[:, ko, :], L["wout"][:, ko, :], woutb, partial=[woutb])
        x1s = [T2("x1_%d" % i, [128, 8, 512]) for i in range(2)]; mixt, mixtb = T2("mixt", [128, 8, 512], BF16)
        tmp, tmpb = T2("ftmp", [128, 8, 512]); sq, sqb = T2("fsq", [128, 8, 512], BF16)
        hTs = [T2("fhT%d" % i, [128, 8, 512], BF16) for i in range(2)]; rstd, rstdb = T2("frstd")
        acc, accb = T2("acc", [128, 8, 512]); aT, aTb = T2("aT", [128, NF, 512], BF16)
        wi = [T2("wi%d" % i, [128, 8, 2, 256], BF16) for i in range(2)]
        wo = [T2("wo%d" % i, [128, NF, 256], BF16) for i in range(2)]
        sg = [T2("sg%d" % i, dt=BF16) for i in range(2)]
        ta = [T2("ta%d" % i, dt=BF16) for i in range(1)] * 2
        Gb = [T2("Gb%d" % i, dt=BF16) for i in range(1)] * 2
        if moe:
            rt_, rtb = T2("router", [128, 8, 8])
            k.dma("sp", rt_[:], W["moe_router"].rearrange("(ko p) e -> p ko e", p=128), rtb, writes=[rtb])
            Rp, Rpb = T2("Rp", [128, 8, 8])
            for ko in range(8):
                k.op("dve", lambda e, ko=ko: e.tensor_scalar(out=Rp[:, ko, :], in0=rt_[:, ko, :], scalar1=c.mod[:, 56 + ko:57 + ko], scalar2=None, op0=ALU.mult),
                     reads=[rtb, c.modb], writes=[Rpb] if ko == 0 else (), partial=[Rpb] if ko else ())
            brow, browb = T2("brow", [128, 8])
            p, pb = c.ps[6], c.psb[6]
            for ko in range(8):
                k.op("pe", lambda e, ko=ko: e.matmul(p[0:1, 0:8], lhsT=c.mod[:, 24 + ko:25 + ko], rhs=rt_[:, ko, :], start=(ko == 0), stop=(ko == 7)),
                     reads=[c.modb, rtb], writes=[pb] if ko == 0 else (), partial=[pb] if ko else ())
            k.op("dve", lambda e: e.tensor_copy(out=brow[0:1, :], in_=p[0:1, 0:8]), reads=[pb], writes=[browb])
            sel8, sel8b = T2("sel8", [128, 8, 128], BF16)
            k.op("dve", lambda e: e.tensor_copy(out=sel8[0:8], in_=c.ident[0:8, 0:8].unsqueeze(2).to_broadcast([8, 8, 128])), reads=[c.identb], writes=[sel8b])
            Lg, Lgb = T2("Lg", [128, 4, 8]); L2, L2b = T2("L2", [128, 4, 8]); mk, mkb = T2("mk", [128, 4, 8])
            m1, m1b = T2("m1", [128, 4]); m2, m2b = T2("m2", [128, 4]); ex, exb = L2, L2b
            den, denb = T2("den", [128, 4]); Gt, Gtb = L2, L2b; GTs = [T2("GTr%d" % i, [128, 512], BF16) for i in range(2)]
        wq_i = [0]
        woq_i = [0]
        ps1 = PsRot(c, [0, 1, 2, 3])
        ps2 = PsRot(c, [4, 5])
        def pro_body(tt):
            ts_ = slice(tt * 512, (tt + 1) * 512)
            x1, x1b = x1s[tt % 2]
            hT, hTb = hTs[tt % 2]
            if moe:
                GT, GTb = GTs[tt % 2]
            k.dma("sp", x1[:], xT[:, :, ts_].rearrange("k p t -> p k t"), x1b, writes=[x1b])
            k.dma("sp", mixt[:], mixT[:, :, ts_].rearrange("k p t -> p k t"), mixtb, writes=[mixtb])
            for n in range(8):
                p, pb = ps1.get()
                for ko in range(8):
                    k.op("pe", lambda e, p=p, n=n, ko=ko: e.matmul(p[:], lhsT=wout[:, ko, n * 128:(n + 1) * 128], rhs=mixt[:, ko, :], start=(ko == 0), stop=(ko == 7)),
                         reads=[woutb, mixtb], writes=[pb] if ko == 0 else (), partial=[pb] if ko else ())
                k.op("dve", lambda e, p=p, n=n: e.scalar_tensor_tensor(out=x1[:, n, :], in0=p[:], scalar=c.mod[:, 16 + n:17 + n], in1=x1[:, n, :],
                                                                      op0=ALU.mult, op1=ALU.add), reads=[pb, c.modb, x1b], partial=[x1b])
            norm_mod(nc, k, c, x1, x1b, tmp, tmpb, sq, sqb, rstd, rstdb, hT, hTb, 7, 56, 24)
            if moe:
                p, pb = c.ps[6], c.psb[6]
                first = True
                for blk in range(4):
                    for ko in range(8):
                        k.op("pe", lambda e, blk=blk, ko=ko: e.matmul(p[:, blk * 8:(blk + 1) * 8], lhsT=tmp[:, ko, blk * 128:(blk + 1) * 128], rhs=Rp[:, ko, :],
                                                                      start=(ko == 0), stop=False),
                             reads=[tmpb, Rpb], writes=[pb] if first else (), partial=() if first else [pb])
                        first = False
                    k.op("pe", lambda e, blk=blk: e.matmul(p[:, blk * 8:(blk + 1) * 8], lhsT=c.ones[0:1, 0:128], rhs=brow[0:1, 0:8], start=False, stop=True),
                         reads=[c.onesb, browb], partial=[pb])
                k.op("dve", lambda e: e.tensor_copy(out=Lg[:], in_=p[:, 0:32].rearrange("p (b e) -> p b e", e=8)), reads=[pb], writes=[Lgb])
                k.op("dve", lambda e: e.tensor_reduce(out=m1[:], in_=Lg[:], axis=AX.X, op=ALU.max), reads=[Lgb], writes=[m1b])
                k.op("dve", lambda e: e.tensor_tensor(out=mk[:], in0=Lg[:], in1=m1[:].unsqueeze(2).to_broadcast([128, 4, 8]), op=ALU.is_ge), reads=[Lgb, m1b], writes=[mkb])
                k.op("dve", lambda e: e.scalar_tensor_tensor(out=L2[:], in0=mk[:], scalar=-1e30, in1=Lg[:], op0=ALU.mult, op1=ALU.add), reads=[mkb, Lgb], writes=[L2b])
                k.op("dve", lambda e: e.tensor_reduce(out=m2[:], in_=L2[:], axis=AX.X, op=ALU.max), reads=[L2b], writes=[m2b])
                k.op("dve", lambda e: e.tensor_tensor(out=mk[:], in0=Lg[:], in1=m2[:].unsqueeze(2).to_broadcast([128, 4, 8]), op=ALU.is_ge), reads=[Lgb, m2b], writes=[mkb])
                k.op("dve", lambda e: e.tensor_tensor(out=L2[:], in0=Lg[:], in1=m1[:].unsqueeze(2).to_broadcast([128, 4, 8]), op=ALU.subtract), reads=[Lgb, m1b], writes=[L2b])
                k.op("act", lambda e: e.activation(out=ex[:], in_=L2[:], func=AF.Exp), reads=[L2b], writes=[exb])
                k.op("dve", lambda e: e.tensor_tensor(out=den[:], in0=m2[:], in1=m1[:], op=ALU.subtract), reads=[m1b, m2b], writes=[denb])
                k.op("act", lambda e: e.activation(out=den[:], in_=den[:], func=AF.Exp), reads=[denb], writes=[denb])
                k.op("dve", lambda e: e.tensor_scalar(out=den[:], in0=den[:], scalar1=1.0, scalar2=None, op0=ALU.add), reads=[denb], writes=[denb])
                k.op("dve", lambda e: e.reciprocal(out=den[:], in_=den[:]), reads=[denb], writes=[denb])
                k.op("dve", lambda e: e.tensor_tensor(out=Gt[:], in0=ex[:], in1=mk[:], op=ALU.mult), reads=[exb, mkb], writes=[Gtb])
                k.op("dve", lambda e: e.tensor_tensor(out=Gt[:], in0=Gt[:], in1=den[:].unsqueeze(2).to_broadcast([128, 4, 8]), op=ALU.mult), reads=[Gtb, denb], writes=[Gtb])
                p, pb = c.ps[6], c.psb[6]
                for blk in range(4):
                    k.op("pe", lambda e, blk=blk: e.transpose(out=p[0:8, blk * 128:(blk + 1) * 128], in_=Gt[:, blk, :], identity=c.ident[:]),
                         reads=[Gtb, c.identb], writes=[pb] if blk == 0 else (), partial=[pb] if blk else ())
                k.op("dve", lambda e: e.tensor_copy(out=GT[0:8, :], in_=p[0:8, :]), reads=[pb], writes=[GTb])

        def exp_body(tt):
            ts_ = slice(tt * 512, (tt + 1) * 512)
            x1, x1b = x1s[tt % 2]
            hT, hTb = hTs[tt % 2]
            if moe:
                GT, GTb = GTs[tt % 2]
            for ei, (w_in, w_out) in enumerate(experts):
                if moe:
                    p, pb = c.ps[6], c.psb[6]
                    k.op("pe", lambda e, ei=ei: e.matmul(p[:], lhsT=sel8[0:8, ei, :], rhs=GT[0:8, :], start=True, stop=True), reads=[sel8b, GTb], writes=[pb])
                    g_, gb_ = Gb[ei % 2]
                    k.op("act", lambda e, g_=g_: e.copy(out=g_[:], in_=p[:]), reads=[pb], writes=[gb_])
                w_in_v = w_in.rearrange("(ko p) n -> p ko n", p=128)
                w_out_v = w_out.rearrange("(f p) n -> p f n", p=128)
                for grp in range(NG):
                    w_, wb_ = wi[wq_i[0] % 2]
                    wq_i[0] += 1
                    f0 = grp * 256
                    k.dma("sp", w_[:, :, 0, :], w_in_v[:, :, f0:f0 + 256], wb_, writes=[wb_], reads=[c.castb1 if moe else c.castb])
                    k.dma("sp", w_[:, :, 1, :], w_in_v[:, :, F + f0:F + f0 + 256], wb_, partial=[wb_])
                    for fc in range(2):
                        f = grp * 2 + fc
                        pG, pGb = ps1.get()
                        pU, pUb = ps1.get()
                        for ko in range(8):
                            k.op("pe", lambda e, pG=pG, w_=w_, ko=ko, fc=fc: e.matmul(pG[:], lhsT=w_[:, ko, 0, fc * 128:(fc + 1) * 128], rhs=hT[:, ko, :],
                                                                                     start=(ko == 0), stop=(ko == 7)),
                                 reads=[wb_, hTb], writes=[pGb] if ko == 0 else (), partial=[pGb] if ko else ())
                        for ko in range(8):
                            k.op("pe", lambda e, pU=pU, w_=w_, ko=ko, fc=fc: e.matmul(pU[:], lhsT=w_[:, ko, 1, fc * 128:(fc + 1) * 128], rhs=hT[:, ko, :],
                                                                                     start=(ko == 0), stop=(ko == 7)),
                                 reads=[wb_, hTb], writes=[pUb] if ko == 0 else (), partial=[pUb] if ko else ())
                        s_, sb_ = sg[f % 2]
                        k.op("act", lambda e, pG=pG, s_=s_: e.activation(out=s_[:], in_=pG[:], func=AF.Silu), reads=[pGb], writes=[sb_])
                        if moe:
                            t_, tb_ = ta[f % 2]
                            k.op("dve", lambda e, pU=pU, s_=s_, t_=t_: e.tensor_tensor(out=t_[:], in0=pU[:], in1=s_[:], op=ALU.mult), reads=[pUb, sb_], writes=[tb_])
                            k.op("pool", lambda e, t_=t_, g_=g_, f=f: e.tensor_tensor(out=aT[:, f, :], in0=t_[:], in1=g_[:], op=ALU.mult), reads=[tb_, gb_], partial=[aTb])
                        else:
                            k.op("dve", lambda e, pU=pU, s_=s_, f=f: e.tensor_tensor(out=aT[:, f, :], in0=pU[:], in1=s_[:], op=ALU.mult), reads=[pUb, sb_], partial=[aTb])
                for qn_ in range(4):
                    w_, wb_ = wo[woq_i[0] % 2]
                    woq_i[0] += 1
                    k.dma("sp", w_[:], w_out_v[:, :, qn_ * 256:(qn_ + 1) * 256], wb_, writes=[wb_], reads=[c.castb1 if moe else c.castb])
                    for nn in range(2):
                        n = qn_ * 2 + nn
                        p, pb = ps2.get()
                        for f in range(NF):
                            k.op("pe", lambda e, p=p, w_=w_, f=f, nn=nn: e.matmul(p[:], lhsT=w_[:, f, nn * 128:(nn + 1) * 128], rhs=aT[:, f, :],
                                                                                 start=(f == 0), stop=(f == NF - 1)),
                                 reads=[wb_, aTb], writes=[pb] if f == 0 else (), partial=[pb] if f else ())
                        if ei == 0:
                            k.op("act", lambda e, p=p, n=n: e.copy(out=acc[:, n, :], in_=p[:]), reads=[pb], partial=[accb])
                        else:
                            k.op("dve", lambda e, p=p, n=n: e.tensor_tensor(out=acc[:, n, :], in0=p[:], in1=acc[:, n, :], op=ALU.add), reads=[pb, accb], partial=[accb])
            for n in range(8):
                k.op("dve", lambda e, n=n: e.scalar_tensor_tensor(out=acc[:, n, :], in0=acc[:, n, :], scalar=c.mod[:, 40 + n:41 + n], in1=x1[:, n, :],
                                                                 op0=ALU.mult, op1=ALU.add), reads=[accb, c.modb, x1b], partial=[accb])
            k.dma("sp", xT[:, :, ts_].rearrange("k p t -> p k t"), acc[:], accb, reads=[accb])

        pro_body(0)
        for tt in range(NT):
            streams = [("E", lambda tt=tt: exp_body(tt))]
            if tt + 1 < NT:
                streams.append(("G", lambda tt=tt: pro_body(tt + 1)))
            k.coop_run(streams)
        k.barrier()


def declare_inputs(nc, dbg):
    A = {}
    A["x"] = nc.dram_tensor("x", [T, D], F32, kind="ExternalInput").ap()
    A["cT"] = nc.dram_tensor("cT", [128, 8], F32, kind="ExternalInput").ap()
    A["pos"] = nc.dram_tensor("pos", [T], I32, kind="ExternalInput").ap()
    A["hc"] = nc.dram_tensor("hc", [128, 40], F32, kind="ExternalInput").ap()
    A["W"] = {}
    for nm, shp in [("ffn_w_in", [D, 2 * DFF]), ("ffn_w_out", [DFF, D]), ("moe_router", [D, NE]),
                    ("moe_w_in", [NE, D, 2 * DFE]), ("moe_w_out", [NE, DFE, D])]:
        A["W"][nm] = nc.dram_tensor(nm, shp, F32, kind="ExternalInput").ap()
    A["L"] = []
    for l in range(2):
        Ld = {}
        for nm, shp in LAYER_SHAPES.items():
            Ld[nm] = nc.dram_tensor("%s%d" % (nm, l), shp, F32, kind="ExternalInput").ap()
        A["L"].append(Ld)
    return A


def scratch(nc, name, shape, dt, dbg):
    kind = "ExternalOutput" if (dbg and name in dbg) else "Internal"
    return nc.dram_tensor(name, shape, dt, kind=kind).ap()


def build(nc, sigset, dbg=None, stop=None):
    k = K(nc, sigset)
    c = Ctx()
    A = declare_inputs(nc, dbg)
    out = nc.dram_tensor("out", [T, D], F32, kind="ExternalOutput").ap()
    xT = scratch(nc, "xT", [KO, 128, T], F32, dbg)
    zT = scratch(nc, "zT", [NZ, 128, T], F32, dbg)
    vfT = scratch(nc, "vfT", [4, 128, T], F32, dbg)
    mixT = scratch(nc, "mixT", [8, 128, T], BF16, dbg)
    csT = scratch(nc, "csT", [128, T], F32, dbg)
    c.dbgK = scratch(nc, "dbgK", [128, 4, T], BF16, dbg) if (dbg and "dbgK" in dbg) else None
    c.dbgQ = scratch(nc, "dbgQ", [128, 4, T], BF16, dbg) if (dbg and "dbgK" in dbg) else None
    snT = scratch(nc, "snT", [128, T], F32, dbg)
    with ExitStack() as es:
        setup_common(nc, k, es, c)
        c.cT, c.cTb = sb(nc, es, "cT", [128, 8])
        k.dma("sp", c.cT[:], A["cT"], c.cTb, writes=[c.cTb])
        c.onesbf, c.onesbfb = sb(nc, es, "onesbf", [128, 128], BF16)
        k.op("dve", lambda e: e.tensor_copy(out=c.onesbf[:], in_=c.ones[:]), reads=[c.onesb], writes=[c.onesbfb])
        c.epsc, c.epscb = sb(nc, es, "epsc", [128, 4])
        k.op("dve", lambda e: e.memset(c.epsc[:, 0:1], EPS), partial=[c.epscb])
        setup_rwkv_consts(nc, k, es, c)
        c.hc, c.hcb = sb(nc, es, "hc", [128, 40])
        k.dma("sp", c.hc[:], A["hc"], c.hcb, writes=[c.hcb])
        k.op("dve", lambda e: e.memset(c.epsc[:, 1:2], 0.0), partial=[c.epscb])
        c.castb = Buf("cast")
        c.castb.late = True
        c.castb1 = Buf("cast1")
        c.castb1.late = True
        W = A["W"]
        W["ffn_w_in_b"] = nc.dram_tensor("ffn_w_in_b", [D, 2 * DFF], BF16).ap()
        W["ffn_w_out_b"] = nc.dram_tensor("ffn_w_out_b", [DFF, D], BF16).ap()
        W["moe_w_in_b"] = nc.dram_tensor("moe_w_in_b", [NE, D, 2 * DFE], BF16).ap()
        W["moe_w_out_b"] = nc.dram_tensor("moe_w_out_b", [NE, DFE, D], BF16).ap()
        k.dma("pool", W["ffn_w_in_b"], W["ffn_w_in"], c.castb, partial=[c.castb], nodeps=True)
        k.dma("pool", W["ffn_w_out_b"], W["ffn_w_out"], c.castb, partial=[c.castb], nodeps=True)
        phase_rope_tables(nc, k, c, A["pos"], csT, snT)
        phase_transpose_in(nc, k, c, A["x"], xT)
        for l in range(2):
            with ExitStack() as esl:
                load_params(nc, k, esl, c, A["L"][l])
                phase_inproj(nc, k, c, A["L"][l], xT, zT, l)
                if stop == "inproj%d" % l:
                    break
                c.cast_moe = None
                if not (dbg and "skiprwkv" in dbg):
                    phase_rwkv(nc, k, c, A["L"][l], zT, vfT, mixT, l)
                if stop == "rwkv%d" % l:
                    break
                phase_pool_mla(nc, k, c, A["L"][l], zT, csT, snT, mixT, l)
                if stop == "mla%d" % l:
                    break
                if l == 0:
                    for e_ in range(NE):
                        k.dma("pool", W["moe_w_in_b"][e_], W["moe_w_in"][e_], c.castb1, partial=[c.castb1], nodeps=True)
                        k.dma("pool", W["moe_w_out_b"][e_], W["moe_w_out"][e_], c.castb1, partial=[c.castb1], nodeps=True)
                phase_ffn(nc, k, c, A["L"][l], xT, mixT, l, A["W"])
                if stop == "ffn%d" % l:
                    break
        phase_transpose_out(nc, k, c, xT, out)
    return k


ALLSIG = False
MODE_IN = 1
MODE_OUT = 1


def make_nc(dbg=None, stop=None):
    nc0 = bass.Bass("TRN2", target_bir_lowering=False)
    k0 = build(nc0, None, dbg, stop)
    nc = bass.Bass("TRN2", target_bir_lowering=False)
    k = build(nc, (None if ALLSIG else k0.needed), dbg, stop)
    return nc, k


def host_prep(inputs):
    shared = {}
    for l in range(2):
        d = prep_layer(inputs, l)
        for nm, v in d.items():
            shared["%s%d" % (nm, l)] = v
    x = np.ascontiguousarray(inputs["x"], dtype=np.float32)
    c = np.asarray(inputs["c"], np.float32)
    pos = np.asarray(inputs["positions"], np.int32)
    shared["hc"] = host_consts()
    shared["ffn_w_in"] = np.ascontiguousarray(inputs["ffn_w_in"][0], dtype=np.float32)
    shared["ffn_w_out"] = np.ascontiguousarray(inputs["ffn_w_out"][0], dtype=np.float32)
    shared["moe_router"] = np.ascontiguousarray(inputs["moe_router"][0], dtype=np.float32)
    shared["moe_w_in"] = np.ascontiguousarray(inputs["moe_w_in"][0], dtype=np.float32)
    shared["moe_w_out"] = np.ascontiguousarray(inputs["moe_w_out"][0], dtype=np.float32)
    maps = []
    for b in range(8):
        m = dict(shared)
        m["x"] = x[b]
        m["cT"] = np.ascontiguousarray(c[b].reshape(8, 128).T)
        m["pos"] = np.ascontiguousarray(pos[b])
        maps.append(m)
    return maps


def kernel(**inputs):
    nc, k = make_nc()
    in_maps = host_prep(inputs)
    res = run_bass_kernel_spmd(nc, in_maps, core_ids=list(range(8)))
    return np.stack([r["out"] for r in res.results], axis=0)
```

```python
import threading
import numpy as np
from contextlib import ExitStack
import concourse.bass as bass
import concourse.mybir as mybir
from concourse.bass_utils import run_bass_kernel_spmd

F32 = mybir.dt.float32
BF16 = mybir.dt.bfloat16
I32 = mybir.dt.int32
ALU = mybir.AluOpType
AF = mybir.ActivationFunctionType
AX = mybir.AxisListType


class Buf:
    __slots__ = ("name", "writers", "readers", "sem", "cnt", "old", "genr", "late")

    def __init__(self, name):
        self.name = name
        self.writers = []
        self.readers = []
        self.sem = None
        self.cnt = 0
        self.old = []
        self.genr = False
        self.late = False


class Rec:
    __slots__ = ("idx", "eng", "dma", "sem", "val", "ins", "buf")


COMPUTE = ("pe", "act", "dve", "pool")


class K:
    def __init__(self, nc, sigset=None):
        self.nc = nc
        self.engs = {"pe": nc.tensor, "act": nc.scalar, "dve": nc.vector,
                     "pool": nc.gpsimd, "sp": nc.sync}
        self.esem = {}
        for e in self.engs:
            self.esem[e] = nc.alloc_semaphore("es_" + e)
        self.ecnt = {e: 0 for e in self.engs}
        self.last = {e: None for e in self.engs}
        self.widx = {e: {p: -1 for p in self.engs} for e in self.engs}
        self.wdma = {e: {} for e in self.engs}
        self.nops = 0
        self.needed = set()
        self.sigset = sigset
        self.sempool = []
        self.dmabufs = []
        self.nsem = 0
        self.ninstr = {e: 0 for e in self.engs}

    def _bufsem(self, b):
        if getattr(b, "late", False):
            if b.sem is None:
                b.sem = self.nc.alloc_semaphore("ls_%d" % self.nsem)
                self.nsem += 1
                b.cnt = 0
            return b.sem
        if b.sem is None:
            if self.sempool:
                b.sem, b.cnt = self.sempool.pop()
            else:
                b.sem = self.nc.alloc_semaphore("ds_%d" % self.nsem)
                self.nsem += 1
                b.cnt = 0
            self.dmabufs.append(b)
        return b.sem

    def release(self, bufs):
        for b in bufs:
            if b.sem is not None:
                self.sempool.append((b.sem, b.cnt))
                self.dmabufs.remove(b)
                b.sem = None

    def _wait(self, eng, r):
        e = self.engs[eng]
        if r.dma:
            key = r.sem.num
            val = r.val
            if r.buf.sem is r.sem and r.buf.cnt > val:
                val = r.buf.cnt
            cur = self.wdma[eng].get(key, 0)
            if cur >= val:
                return
            e.wait_ge(r.sem, val)
            self.ninstr[eng] += 1
            self.wdma[eng][key] = val
        else:
            if r.eng == eng and eng in ("pe", "sp"):
                return
            if self.widx[eng][r.eng] >= r.idx:
                return
            self.widx[eng][r.eng] = r.idx
            self.needed.add(r.idx)
            if r.val is None:
                raise RuntimeError("op %d needed but not signalled" % r.idx)
            e.wait_ge(self.esem[r.eng], r.val)
            self.ninstr[eng] += 1

    def _deps(self, eng, reads, writes, partial):
        deps = []
        for b in reads:
            deps.extend(b.writers)
        for b in writes:
            deps.extend(b.writers)
            deps.extend(b.readers)
        for b in partial:
            deps.extend(b.writers)
            deps.extend(b.readers)
            deps.extend(b.old)
        seen = set()
        for d in deps:
            if id(d) in seen:
                continue
            seen.add(id(d))
            self._wait(eng, d)

    def _post(self, rec, reads, writes, partial):
        for b in reads:
            b.readers.append(rec)
            b.old = []
            b.genr = True
        for b in writes:
            b.writers = [rec]
            b.readers = []
            b.old = []
            b.genr = False
        for b in partial:
            if b.genr:
                b.writers = [rec]
                b.genr = False
            else:
                b.writers.append(rec)
            b.old = b.old + b.readers
            b.readers = []

    def op(self, eng, fn, reads=(), writes=(), partial=()):
        self._deps(eng, reads, writes, partial)
        ins = fn(self.engs[eng])
        self.ninstr[eng] += 1
        rec = Rec()
        rec.idx = self.nops
        self.nops += 1
        rec.eng = eng
        rec.dma = False
        rec.sem = None
        rec.ins = ins
        if self.sigset is None or rec.idx in self.sigset:
            self.ecnt[eng] += 1
            ins.then_inc(self.esem[eng], 1)
            rec.val = self.ecnt[eng]
        else:
            rec.val = None
        self.last[eng] = rec
        self._post(rec, reads, writes, partial)
        if eng != "pe":
            self.yp()
        return rec

    def dma(self, q, out, in_, sembuf, reads=(), writes=(), partial=(), nodeps=False, **kw):
        if not nodeps:
            self._deps(q, reads, writes, partial)
        sem = self._bufsem(sembuf)
        ins = self.engs[q].dma_start(out=out, in_=in_, **kw)
        self.ninstr[q] += 1
        sembuf.cnt += 16
        ins.then_inc(sem, 16)
        rec = Rec()
        rec.idx = self.nops
        self.nops += 1
        rec.eng = q
        rec.dma = True
        rec.sem = sem
        rec.val = sembuf.cnt
        rec.buf = sembuf
        rec.ins = ins
        self._post(rec, reads, writes, partial)
        return rec

    def coop_run(self, streams):
        if len(streams) == 1:
            streams[0][1]()
            return
        main = threading.Semaphore(0)
        st = {}
        order = []
        err = []
        for name, fn in streams:
            sem = threading.Semaphore(0)
            d = {"sem": sem, "alive": True}
            st[name] = d
            order.append(name)

            def runner(fn=fn, d=d):
                d["sem"].acquire()
                try:
                    fn()
                except BaseException as ex:
                    err.append(ex)
                d["alive"] = False
                main.release()
            d["th"] = threading.Thread(target=runner)
            d["th"].start()
        self._coop = (st, main)
        while any(st[n]["alive"] for n in order):
            for n in order:
                if st[n]["alive"]:
                    self._cur = n
                    st[n]["sem"].release()
                    main.acquire()
        self._coop = None
        for n in order:
            st[n]["th"].join()
        if err:
            raise err[0]

    def yp(self):
        co = getattr(self, "_coop", None)
        if co is None:
            return
        st, main = co
        me = st[self._cur]
        main.release()
        me["sem"].acquire()

    def coop_wait(self, name):
        co = getattr(self, "_coop", None)
        if co is None:
            return
        st, main = co
        while name in st and st[name]["alive"]:
            self.yp()

    def barrier(self):
        sp = self.engs["sp"]
        for e in COMPUTE:
            if self.last[e] is not None:
                self._wait("sp", self.last[e])
        for b in self.dmabufs:
            if b.cnt > self.wdma["sp"].get(b.sem.num, 0):
                sp.wait_ge(b.sem, b.cnt)
                self.wdma["sp"][b.sem.num] = b.cnt
        ins = sp.nop()
        rec = Rec()
        rec.idx = self.nops
        self.nops += 1
        rec.eng = "sp"
        rec.dma = False
        rec.sem = None
        rec.ins = ins
        self.ecnt["sp"] += 1
        ins.then_inc(self.esem["sp"], 1)
        rec.val = self.ecnt["sp"]
        self.needed.add(rec.idx)
        for e in COMPUTE:
            self.engs[e].wait_ge(self.esem["sp"], rec.val)
            self.widx[e]["sp"] = rec.idx
            for p in COMPUTE:
                if self.last[p] is not None:
                    self.widx[e][p] = max(self.widx[e][p], self.last[p].idx)
            for b in self.dmabufs:
                self.wdma[e][b.sem.num] = b.cnt
        for b in self.dmabufs:
            self.sempool.append((b.sem, b.cnt))
            b.sem = None
        self.dmabufs = []
        return rec

T = 4096
D = 1024
NT = T // 512
KO = D // 128


class Ctx:
    pass


def setup_common(nc, k, es, c):
    c.ps = []
    c.psb = []
    for i in range(8):
        t = es.enter_context(nc.psum_tensor("ps%d" % i, [128, 512], F32))
        c.ps.append(t)
        c.psb.append(Buf("ps%d" % i))
    c.ident = es.enter_context(nc.sbuf_tensor("ident", [128, 128], F32))
    c.identb = Buf("ident")
    c.ones = es.enter_context(nc.sbuf_tensor("ones", [128, 128], F32))
    c.onesb = Buf("ones")
    k.op("pool", lambda e: e.memset(c.ones[:], 1.0), writes=[c.onesb])
    k.op("pool", lambda e: e.affine_select(out=c.ident[:], in_=c.ones[:], pattern=[[-1, 128]],
                                           compare_op=ALU.is_equal, fill=0.0, base=0,
                                           channel_multiplier=1),
         reads=[c.onesb], writes=[c.identb])


def phase_transpose_in(nc, k, c, x, xT):
    with ExitStack() as es:
        xin = [es.enter_context(nc.sbuf_tensor("xin%d" % i, [128, D], F32)) for i in range(2)]
        xinb = [Buf("xin%d" % i) for i in range(2)]
        st = [es.enter_context(nc.sbuf_tensor("xst%d" % i, [128, KO, 512], F32)) for i in range(2)]
        stb = [Buf("xst%d" % i) for i in range(2)]
        pi = 0
        for tt in range(NT):
            s = st[tt % 2]
            sb = stb[tt % 2]
            for j in range(4):
                tb = tt * 4 + j
                xi = xin[tb % 2]
                xib = xinb[tb % 2]
                k.dma("sp", xi[:], x[tb * 128:(tb + 1) * 128, :], xib, writes=[xib])
                for half in range(2):
                    p = c.ps[pi % 8]
                    pb = c.psb[pi % 8]
                    pi += 1
                    for q in range(4):
                        ko = half * 4 + q
                        k.op("pe", lambda e, p=p, xi=xi, ko=ko, q=q: e.transpose(
                            out=p[:, q * 128:(q + 1) * 128], in_=xi[:, ko * 128:(ko + 1) * 128],
                            identity=c.ident[:]),
                            reads=[xib, c.identb], writes=[pb] if q == 0 else (), partial=[pb] if q else ())
                    eng = "dve" if (half == 0 or MODE_IN == 0) else "act"
                    if eng == "dve":
                        k.op("dve", lambda e, p=p, s=s, half=half, j=j: e.tensor_copy(
                            out=s[:, half * 4:(half + 1) * 4, j * 128:(j + 1) * 128],
                            in_=p[:].rearrange("p (q t) -> p q t", q=4)),
                            reads=[pb], partial=[sb])
                    else:
                        k.op("act", lambda e, p=p, s=s, half=half, j=j: e.copy(
                            out=s[:, half * 4:(half + 1) * 4, j * 128:(j + 1) * 128],
                            in_=p[:].rearrange("p (q t) -> p q t", q=4)),
                            reads=[pb], partial=[sb])
            k.dma("sp", xT[:, :, tt * 512:(tt + 1) * 512].rearrange("k p t -> p k t"), s[:], sb, reads=[sb])
    k.barrier()


def phase_transpose_out(nc, k, c, xT, out):
    with ExitStack() as es:
        xin = [es.enter_context(nc.sbuf_tensor("yin%d" % i, [128, KO, 512], F32)) for i in range(2)]
        xinb = [Buf("yin%d" % i) for i in range(2)]
        st = [es.enter_context(nc.sbuf_tensor("yst%d" % i, [128, D], F32)) for i in range(2)]
        stb = [Buf("yst%d" % i) for i in range(2)]
        pi = 0
        for tt in range(NT):
            xi = xin[tt % 2]
            xib = xinb[tt % 2]
            k.dma("sp", xi[:], xT[:, :, tt * 512:(tt + 1) * 512].rearrange("k p t -> p k t"), xib, writes=[xib])
            for j in range(4):
                tb = tt * 4 + j
                s = st[tb % 2]
                sb = stb[tb % 2]
                for half in range(2):
                    p = c.ps[pi % 8]
                    pb = c.psb[pi % 8]
                    pi += 1
                    for q in range(4):
                        ko = half * 4 + q
                        k.op("pe", lambda e, p=p, xi=xi, ko=ko, q=q, j=j: e.transpose(
                            out=p[:, q * 128:(q + 1) * 128], in_=xi[:, ko, j * 128:(j + 1) * 128],
                            identity=c.ident[:]),
                            reads=[xib, c.identb], writes=[pb] if q == 0 else (), partial=[pb] if q else ())
                    if half == 0 or MODE_OUT == 0:
                        k.op("dve", lambda e, p=p, s=s, half=half: e.tensor_copy(
                            out=s[:, half * 512:(half + 1) * 512], in_=p[:]), reads=[pb], partial=[sb])
                    else:
                        k.op("act", lambda e, p=p, s=s, half=half: e.copy(
                            out=s[:, half * 512:(half + 1) * 512], in_=p[:]), reads=[pb], partial=[sb])
                k.dma("sp", out[tb * 128:(tb + 1) * 128, :], s[:], sb, reads=[sb])
    k.barrier()


RW = 512
NZ = 21
NSH = 15
C_WA, C_GD0, C_GD1, C_POOL, C_QLAT, C_KVLAT, C_KROPE = 12, 13, 14, 15, 17, 19, 20
DFF = 2816
DFE = 3584
NE = 8
EPS = 1e-6

PCOL = {}
_off = 0
for _n, _w in [("b_ada", 48), ("g1", 8), ("g2", 8), ("mu", NSH), ("w0", 4), ("a0", 4), ("kk", 4),
               ("ka", 4), ("rk", 4), ("lng", 4), ("lnb", 4), ("v0", 4), ("pscale", 2), ("qlg", 2),
               ("kvlg", 1), ("qkq", 1), ("qkk", 1)]:
    PCOL[_n] = (_off, _w)
    _off += _w
NPCOL = _off


def fm(v, n):
    return np.ascontiguousarray(np.asarray(v, np.float32).reshape(n, 128).T)


def prep_layer(inp, l):
    f32 = np.float32
    d = {}
    P = np.zeros((128, NPCOL), f32)

    def put(name, arr):
        o, w = PCOL[name]
        assert arr.shape == (128, w), (name, arr.shape)
        P[:, o:o + w] = arr
    put("b_ada", fm(inp["b_ada"][l], 48))
    put("g1", fm(inp["norm_gain"][l, 0], 8))
    put("g2", fm(inp["norm_gain"][l, 1], 8))
    w_in = inp["w_in_first"] if l == 0 else inp["w_in_rest"][l - 1]
    mu = inp["mu_shift"][l]
    cols = np.zeros((NZ * 128,), np.int64) - 1
    cols[0:1536] = np.arange(0, 1536)
    cols[1536:1536 + 128] = np.arange(1536, 1664)
    cols[1664:1664 + 128] = np.arange(1664, 1792)
    cols[1792:1792 + 32] = np.arange(1792, 1824)
    muc = np.zeros((NZ * 128,), f32)
    muc[0:1824] = mu
    if l > 0:
        cols[1824:1856] = np.arange(2496, 2528)
        muc[1824:1856] = inp["mu_shift_v"][l - 1]
    cols[1920:1920 + 256] = np.arange(1824, 2080)
    cols[2176:2176 + 256] = np.arange(2080, 2336)
    cols[2432:2432 + 128] = np.arange(2336, 2464)
    kr = 2464
    cols[2560:2560 + 32] = np.arange(kr, kr + 32)
    cols[2592:2592 + 16] = np.arange(kr + 16, kr + 32)
    cols[2608:2608 + 16] = np.arange(kr, kr + 16)
    W = np.zeros((D, NZ * 128), f32)
    m = cols >= 0
    W[:, m] = w_in[:, cols[m]]
    d["win"] = np.ascontiguousarray(W.reshape(8, 128, NZ * 128).transpose(1, 0, 2))
    put("mu", fm(muc[:NSH * 128], NSH))
    rv = inp["rwkv_vec"][l]
    for i, nm in enumerate(["w0", "a0", "kk", "ka", "rk", "lng", "lnb"]):
        put(nm, fm(rv[i], 4))
    if l > 0:
        put("v0", fm(inp["rwkv_v0"][l - 1], 4))
    put("pscale", fm(inp["pool_scale"][l], 2))
    put("qlg", fm(inp["mla_q_lat_gain"][l], 2))
    put("kvlg", fm(inp["mla_kv_lat_gain"][l], 1))
    g = np.zeros((128, 1), f32); g[:96, 0] = inp["mla_qk_gain"][l, 0]; put("qkq", g)
    g = np.zeros((128, 1), f32); g[:96, 0] = inp["mla_qk_gain"][l, 1]; put("qkk", g)
    d["par"] = P
    d["wada"] = np.ascontiguousarray(inp["w_ada"][l].reshape(8, 128, 6144).transpose(1, 0, 2))
    w2a2 = np.concatenate([inp["rwkv_w2"][l], inp["rwkv_a2"][l]], axis=0)
    g2 = inp["rwkv_g2"][l]
    g2b = np.zeros((128, 512), f32)
    g2b[0:32] = g2[128:160]
    if l > 0:
        g2b[32:64] = inp["rwkv_v2"][l - 1]
    d["rw"] = np.ascontiguousarray(np.stack([w2a2, g2[0:128], g2b], axis=1))
    pw = np.zeros((128, 2, 128), f32)
    for gi in range(4):
        pc, o = gi // 2, (gi % 2) * 64
        pw[o:o + 64, pc, o:o + 64] = inp["pool_w"][l, gi]
    d["poolw"] = pw
    wq = inp["mla_wq_up"][l]
    WQ = np.zeros((256, 4, 2, 96), f32)
    for h in range(4):
        WQ[:, h, 0, :] = wq[:, h * 96:(h + 1) * 96]
        WQ[:, h, 1, 64:80] = wq[:, h * 96 + 80:h * 96 + 96]
        WQ[:, h, 1, 80:96] = wq[:, h * 96 + 64:h * 96 + 80]
    d["wq"] = np.ascontiguousarray(WQ.reshape(2, 128, 4 * 2 * 96).transpose(1, 0, 2))
    wkv = inp["mla_wkv_up"][l].reshape(128, 4, 128)
    d["wkv"] = np.ascontiguousarray(np.concatenate(
        [wkv[:, :, :64].reshape(128, 256), wkv[:, :, 64:].reshape(128, 256)], axis=1))
    d["wout"] = np.ascontiguousarray(inp["w_out"][l].reshape(8, 128, 1024).transpose(1, 0, 2))
    return d


LAYER_SHAPES = {"win": [128, 8, NZ * 128], "par": [128, NPCOL], "wada": [128, 8, 6144], "rw": [128, 3, 512],
                "poolw": [128, 2, 128], "wq": [128, 2, 768], "wkv": [128, 512], "wout": [128, 8, 1024]}

_SBN = [0]


def sb(nc, es, name, shape, dt=F32):
    _SBN[0] += 1
    t = es.enter_context(nc.sbuf_tensor("s%d_%s" % (_SBN[0], name), shape, dt))
    return t, Buf(name)


def load_params(nc, k, es, c, L):
    par, parb = sb(nc, es, "par", [128, NPCOL])
    k.dma("sp", par[:], L["par"], parb, writes=[parb])
    c.par, c.parb = par, parb
    mod, modb = sb(nc, es, "mod", [128, 64])
    c.mod, c.modb = mod, modb
    cact, cactb = sb(nc, es, "cact", [128, 8])
    k.op("act", lambda e: e.activation(out=cact[:], in_=c.cT[:], func=AF.Silu), reads=[c.cTb], writes=[cactb])
    with ExitStack() as es2:
        wa = [sb(nc, es2, "wada%d" % i, [128, 8, 512]) for i in range(2)]
        p, pb = c.ps[0], c.psb[0]
        for nt in range(12):
            w, wb = wa[nt % 2]
            k.dma("sp", w[:], L["wada"][:, :, nt * 512:(nt + 1) * 512], wb, writes=[wb])
            for j in range(4):
                col = nt * 4 + j
                for ko in range(8):
                    k.op("pe", lambda e, w=w, j=j, ko=ko, col=col: e.matmul(
                        p[:, col:col + 1], lhsT=w[:, ko, j * 128:(j + 1) * 128], rhs=cact[:, ko:ko + 1],
                        start=(ko == 0), stop=(ko == 7)),
                        reads=[wb, cactb], writes=[pb] if (col == 0 and ko == 0) else (),
                        partial=() if (col == 0 and ko == 0) else [pb])
        o, _ = PCOL["b_ada"]
        k.op("dve", lambda e: e.tensor_tensor(out=mod[:, 0:48], in0=p[:, 0:48], in1=par[:, o:o + 48], op=ALU.add),
             reads=[pb, parb], partial=[modb])
        for (dst, sc, gn) in [(48, 8, "g1"), (56, 32, "g2")]:
            go, _ = PCOL[gn]
            k.op("dve", lambda e, dst=dst, sc=sc, go=go: e.scalar_tensor_tensor(
                out=mod[:, dst:dst + 8], in0=mod[:, sc:sc + 8], scalar=1.0, in1=par[:, go:go + 8],
                op0=ALU.add, op1=ALU.mult), reads=[modb, parb], partial=[modb])
        k.barrier()


def norm_mod(nc, k, c, xt, xtb, tmp, tmpb, sq, sqb, rstd, rstdb, hT, hTb, pbank, acol, bcol, h32=None, h32b=None):
    p, pb = c.ps[pbank], c.psb[pbank]
    k.op("act", lambda e: e.activation(out=sq[:], in_=xt[:], func=AF.Square), reads=[xtb], writes=[sqb])
    for ko in range(8):
        k.op("pe", lambda e, ko=ko: e.matmul(p[:], lhsT=c.onesbf[:], rhs=sq[:, ko, :], start=(ko == 0), stop=(ko == 7)),
             reads=[sqb, c.onesbfb], writes=[pb] if ko == 0 else (), partial=[pb] if ko else ())
    k.op("act", lambda e: e.activation(out=rstd[:], in_=p[:], func=AF.Sqrt, scale=1.0 / D, bias=c.epsc[:, 0:1]),
         reads=[pb, c.epscb], writes=[rstdb])
    k.op("dve", lambda e: e.reciprocal(out=rstd[:], in_=rstd[:]), reads=[rstdb], writes=[rstdb])
    k.op("dve", lambda e: e.tensor_tensor(out=tmp[:], in0=xt[:], in1=rstd[:].unsqueeze(1).to_broadcast([128, 8, 512]),
                                          op=ALU.mult), reads=[xtb, rstdb], writes=[tmpb])
    for ko in range(8):
        k.op("act", lambda e, ko=ko: e.activation(out=hT[:, ko, :], in_=tmp[:, ko, :], func=AF.Identity,
                                                  scale=c.mod[:, acol + ko:acol + ko + 1],
                                                  bias=c.mod[:, bcol + ko:bcol + ko + 1]),
             reads=[tmpb, c.modb], writes=[hTb] if ko == 0 else (), partial=[hTb] if ko else ())
        if h32 is not None:
            k.op("pool", lambda e, ko=ko: e.tensor_scalar(out=h32[:, ko, :], in0=tmp[:, ko, :],
                                                          scalar1=c.mod[:, acol + ko:acol + ko + 1],
                                                          scalar2=c.mod[:, bcol + ko:bcol + ko + 1],
                                                          op0=ALU.mult, op1=ALU.add),
                 reads=[tmpb, c.modb], writes=[h32b] if ko == 0 else (), partial=[h32b] if ko else ())


def phase_inproj(nc, k, c, L, xT, zT, l):
    with ExitStack() as es:
        win, winb = sb(nc, es, "win", [128, 8, NZ * 128], BF16)
        for ko in range(8):
            k.dma("pool", win[:, ko, :], L["win"][:, ko, :], winb, partial=[winb])
        xts = [sb(nc, es, "xt%d" % i, [128, 8, 512]) for i in range(2)]
        tmp, tmpb = sb(nc, es, "tmp", [128, 8, 512])
        sq, sqb = sb(nc, es, "sq", [128, 8, 512], BF16)
        rstd, rstdb = sb(nc, es, "rstd", [128, 512])
        hTs = [sb(nc, es, "hT%d" % i, [128, 8, 512], BF16) for i in range(2)]
        zc, zcb = sb(nc, es, "zc", [128, NSH, 513])
        dd = [sb(nc, es, "dd%d" % i, [128, 512]) for i in range(2)]
        zo = [sb(nc, es, "zo%d" % i, [128, 512]) for i in range(4)]
        k.op("pool", lambda e: e.memset(zc[:], 0.0), writes=[zcb])
        mo, _ = PCOL["mu"]
        zi = [0]

        fuse_in = (l == 0 and getattr(c, "x_ap", None) is not None)
        if fuse_in:
            xins = [sb(nc, es, "xin%d" % i, [128, D]) for i in range(2)]

        def norm_body(tt):
            xt, xtb = xts[tt % 2]
            hT, hTb = hTs[tt % 2]
            if fuse_in:
                for j in range(4):
                    tb = tt * 4 + j
                    xi, xib = xins[tb % 2]
                    k.dma("sp", xi[:], c.x_ap[tb * 128:(tb + 1) * 128, :], xib, writes=[xib])
                    for half in range(2):
                        p, pb = c.ps[6], c.psb[6]
                        for q_ in range(4):
                            ko = half * 4 + q_
                            k.op("pe", lambda e, xi=xi, ko=ko, q_=q_: e.transpose(
                                out=p[:, q_ * 128:(q_ + 1) * 128], in_=xi[:, ko * 128:(ko + 1) * 128], identity=c.ident[:]),
                                reads=[xib, c.identb], writes=[pb] if q_ == 0 else (), partial=[pb] if q_ else ())
                        if half == 0:
                            k.op("dve", lambda e, half=half, j=j: e.tensor_copy(
                                out=xt[:, half * 4:(half + 1) * 4, j * 128:(j + 1) * 128], in_=p[:].rearrange("p (q t) -> p q t", q=4)),
                                reads=[pb], partial=[xtb])
                        else:
                            k.op("act", lambda e, half=half, j=j: e.copy(
                                out=xt[:, half * 4:(half + 1) * 4, j * 128:(j + 1) * 128], in_=p[:].rearrange("p (q t) -> p q t", q=4)),
                                reads=[pb], partial=[xtb])
                k.dma("sp", xT[:, :, tt * 512:(tt + 1) * 512].rearrange("k p t -> p k t"), xt[:], xtb, reads=[xtb])
            else:
                k.dma("sp", xt[:], xT[:, :, tt * 512:(tt + 1) * 512].rearrange("k p t -> p k t"), xtb, writes=[xtb])
            norm_mod(nc, k, c, xt, xtb, tmp, tmpb, sq, sqb, rstd, rstdb, hT, hTb, 7, 48, 0)

        def mm_body(tt):
            hT, hTb = hTs[tt % 2]
            k.op("pool", lambda e: e.tensor_copy(out=zc[:, :, 0:1], in_=zc[:, :, 512:513]), reads=[zcb], writes=[zcb])
            for nch in range(NZ):
                p, pb = c.ps[nch % 6], c.psb[nch % 6]
                for ko in range(8):
                    k.op("pe", lambda e, p=p, ko=ko, nch=nch: e.matmul(
                        p[:], lhsT=win[:, ko, nch * 128:(nch + 1) * 128], rhs=hT[:, ko, :],
                        start=(ko == 0), stop=(ko == 7)),
                        reads=[winb, hTb], writes=[pb] if ko == 0 else (), partial=[pb] if ko else ())
                z, zb = zo[zi[0] % 4]
                zi[0] += 1
                if nch < NSH:
                    d_, db = dd[nch % 2]
                    k.op("act", lambda e, p=p, nch=nch: e.copy(out=zc[:, nch, 1:513], in_=p[:]), reads=[pb], partial=[zcb])
                    k.op("dve", lambda e, d_=d_, nch=nch: e.tensor_tensor(out=d_[:], in0=zc[:, nch, 0:512], in1=zc[:, nch, 1:513],
                                                                     op=ALU.subtract), reads=[zcb], writes=[db])
                    k.op("dve", lambda e, d_=d_, z=z, nch=nch: e.scalar_tensor_tensor(
                        out=z[:], in0=d_[:], scalar=c.par[:, mo + nch:mo + nch + 1], in1=zc[:, nch, 1:513],
                        op0=ALU.mult, op1=ALU.add), reads=[db, zcb, c.parb], writes=[zb])
                else:
                    k.op("act", lambda e, p=p, z=z: e.copy(out=z[:], in_=p[:]), reads=[pb], writes=[zb])
                k.dma("sp", zT[nch, :, tt * 512:(tt + 1) * 512], z[:], zb, reads=[zb])

        norm_body(0)
        for tt in range(NT):
            streams = [("M", lambda tt=tt: mm_body(tt))]
            if tt + 1 < NT:
                streams.append(("N", lambda tt=tt: norm_body(tt + 1)))
            k.coop_run(streams)
        k.barrier()

C0 = float(np.exp(-0.5))
LNX_EPS = 64e-5


def setup_rwkv_consts(nc, k, es, c):
    c.bones, c.bonesb = sb(nc, es, "bones", [128, 128])
    k.op("pool", lambda e: e.memset(c.bones[:], 0.0), writes=[c.bonesb])
    for h in range(2):
        k.op("pool", lambda e, h=h: e.memset(c.bones[h * 64:(h + 1) * 64, h * 64:(h + 1) * 64], 1.0), partial=[c.bonesb])
    c.ones3, c.ones3b = sb(nc, es, "ones3", [128, 8, 128])
    k.op("pool", lambda e: e.memset(c.ones3[:], 1.0), writes=[c.ones3b])
    c.maskL, c.maskLb = sb(nc, es, "maskL", [128, 8, 64])
    c.maskNM, c.maskNMb = sb(nc, es, "maskNM", [128, 4, 128])
    c.i64, c.i64b = sb(nc, es, "i64", [128, 8, 64])
    for h in range(2):
        ps_ = slice(h * 64, (h + 1) * 64)
        k.op("pool", lambda e, ps_=ps_: e.affine_select(out=c.maskL[ps_], in_=c.ones3[ps_, :, 0:64], pattern=[[0, 8], [-1, 64]],
                                                        compare_op=ALU.is_gt, fill=0.0, base=0, channel_multiplier=1),
             reads=[c.ones3b], partial=[c.maskLb])
        k.op("pool", lambda e, ps_=ps_: e.affine_select(out=c.maskNM[ps_, :, 0:64], in_=c.ones3[ps_, 0:4, 0:64], pattern=[[0, 4], [1, 64]],
                                                        compare_op=ALU.is_gt, fill=0.0, base=0, channel_multiplier=-1),
             reads=[c.ones3b], partial=[c.maskNMb])
        k.op("pool", lambda e, ps_=ps_: e.affine_select(out=c.maskNM[ps_, :, 64:128], in_=c.ones3[ps_, 0:4, 0:64], pattern=[[0, 4], [1, 64]],
                                                        compare_op=ALU.is_ge, fill=0.0, base=0, channel_multiplier=-1),
             reads=[c.ones3b], partial=[c.maskNMb])
        k.op("pool", lambda e, ps_=ps_: e.affine_select(out=c.i64[ps_], in_=c.ones3[ps_, :, 0:64], pattern=[[0, 8], [-1, 64]],
                                                        compare_op=ALU.is_equal, fill=0.0, base=0, channel_multiplier=1),
             reads=[c.ones3b], partial=[c.i64b])


class PsRot:
    def __init__(self, c, banks):
        self.c = c
        self.banks = list(banks)
        self.i = 0

    def get(self):
        b = self.banks[self.i % len(self.banks)]
        self.i += 1
        return self.c.ps[b], self.c.psb[b]


FAST32 = False


def R32(ap):
    return ap.bitcast(mybir.dt.float32r) if FAST32 else ap


def umm(k, p, pb, chunks, width, terms, reads):
    first = True
    for j, cc in enumerate(chunks):
        for h in range(2):
            nt = len(terms)
            for ti, (lf, rf) in enumerate(terms):
                k.op("pe", lambda e, h=h, cc=cc, j=j, lf=lf, rf=rf, ti=ti, nt=nt: e.matmul(
                    p[h * 64:(h + 1) * 64, j * width:(j + 1) * width], lhsT=R32(lf(h, cc)), rhs=R32(rf(h, cc)),
                    start=(ti == 0), stop=(ti == nt - 1)),
                    reads=reads, writes=[pb] if first else (), partial=() if first else [pb])
                first = False


def phase_rwkv(nc, k, c, L, zT, vfT, mixT, l):
    HS = lambda h: slice(h * 64, (h + 1) * 64)
    with ExitStack() as es:
        rw, rwb = sb(nc, es, "rw", [128, 3, 512], BF16)
        k.dma("pool", rw[:], L["rw"], rwb, writes=[rwb])
        par, parb = c.par, c.parb
        pc = lambda nm, i: par[:, PCOL[nm][0] + i:PCOL[nm][0] + i + 1]
        omka, omkab = sb(nc, es, "omka", [128, 4])
        k.op("dve", lambda e: e.tensor_scalar(out=omka[:], in0=par[:, PCOL["ka"][0]:PCOL["ka"][0] + 4], scalar1=-1.0, scalar2=1.0,
                                              op0=ALU.mult, op1=ALU.add), reads=[parb], writes=[omkab])
        lnxe, lnxeb = sb(nc, es, "lnxe", [128, 2])
        k.op("dve", lambda e: e.memset(lnxe[:, 0:1], LNX_EPS), partial=[lnxeb])
        k.op("dve", lambda e: e.memset(lnxe[:, 1:2], 1e-24), partial=[lnxeb])
        T2 = lambda nm, shape=[128, 512], dt=F32: sb(nc, es, nm, shape, dt)
        wa, wab = T2("wa"); gd0, gd0b = T2("gd0"); gd1, gd1b = T2("gd1")
        twa, twab = T2("twa", dt=BF16); sg0, sg0b = T2("sg0", dt=BF16); sg1, sg1b = T2("sg1", dt=BF16)
        rt = [T2("rt%d" % i) for i in range(2)]
        kt = [T2("kt%d" % i) for i in range(2)]
        vt = [T2("vt%d" % i) for i in range(2)]
        vft, vftb = T2("vft")
        sgm, sgmb = T2("sgm"); aa, aab = T2("aa"); kkr, kkrb = T2("kkr"); sq, sqb = T2("sqr")
        rn, rnb = T2("rn"); kk, kkb = T2("kk"); kp, kpb = T2("kp"); bs, bsb = T2("bs"); tA, tAb = T2("tA")
        rk, rkb = T2("rk"); cs, csb = T2("cs"); q, qb = T2("q", [128, 8, 64]); qm, qmb = T2("qm", [128, 8, 64])
        basec, basecb = T2("basec", [128, 8])
        E1, E1b = T2("E1", [128, 8, 64]); E2, E2b = T2("E2", [128, 8, 64]); E3, E3b = T2("E3", [128, 8, 64]); Eh, Ehb = T2("Eh", [128, 8, 64])
        IF = [dict(AR=T2("AR%d" % i, [128, 8, 128]), BtT=T2("BtT%d" % i, [128, 8, 64]), KtT=T2("KtT%d" % i, [128, 8, 64]),
                   Xtok=T2("Xtok%d" % i, [128, 8, 128]), Bhk=T2("Bhk%d" % i, [128, 8, 64]), Khk=T2("Khk%d" % i, [128, 8, 64]),
                   Vtok=T2("Vtok%d" % i, [128, 8, 64]), gam=T2("gam%d" % i, [128, 8])) for i in range(2)]
        BhT, BhTb = T2("BhT", [128, 8, 64]); KhT, KhTb = T2("KhT", [128, 8, 64])
        Lp = [T2("Lp%d" % i, [128, 8, 64]) for i in range(2)]
        Np = [T2("Np%d" % i, [128, 8, 64]) for i in range(2)]
        Pp = [T2("Pp%d" % i, [128, 8, 64]) for i in range(2)]
        NM, NMb = T2("NM", [128, 8, 128]); KM, KMb = T2("KM", [128, 8, 128]); WU, WUb = T2("WU", [128, 8, 128])
        Igam, Igamb = T2("Igam", [128, 8, 64])
        GT, GTb = T2("GT", [128, 4, 8, 64]); HH, HHb = T2("HH", [128, 4, 8, 64])
        R2T, R2Tb = T2("R2T", [128, 4, 8, 64]); Y0T, Y0Tb = T2("Y0T", [128, 4, 8, 64])
        gg, ggb = T2("gg", [128, 2, 4, 512], BF16); bon, bonb = T2("bon", [128, 2, 4, 512], BF16); yT, yTb = T2("yT", [128, 4, 512])
        vp, vpb = T2("vp")
        S, Sb = T2("S", [128, 4, 64])
        dd, ddb = T2("gnd"); yo = [T2("yo%d" % i, dt=BF16) for i in range(2)]
        k.op("pool", lambda e: e.memset(S[:], 0.0), writes=[Sb])
        prP = PsRot(c, [0, 1])
        prC = PsRot(c, [2, 3, 4, 5])
        prS = PsRot(c, [6, 7])

        def P_body(tt, hp):
            ifs = IF[(tt * 4 + hp) % 2]
            AR, ARb = ifs["AR"]; BtT, BtTb = ifs["BtT"]; KtT, KtTb = ifs["KtT"]; Xtok, Xtokb = ifs["Xtok"]
            Bhk, Bhkb = ifs["Bhk"]; Khk, Khkb = ifs["Khk"]; Vtok, Vtokb = ifs["Vtok"]; gam, gamb = ifs["gam"]
            ts_ = slice(tt * 512, (tt + 1) * 512)
            if l == 0 and hp == 0 and c.cast_moe is not None:
                c.cast_moe(tt)
            if hp == 0:
                k.dma("sp", wa[:], zT[C_WA, :, ts_], wab, writes=[wab])
                k.dma("sp", gd0[:], zT[C_GD0, :, ts_], gd0b, writes=[gd0b])
                k.dma("sp", gd1[:], zT[C_GD1, :, ts_], gd1b, writes=[gd1b])
                k.op("act", lambda e: e.activation(out=twa[0:64], in_=wa[0:64], func=AF.Tanh), reads=[wab], writes=[twab])
                k.op("act", lambda e: e.copy(out=twa[64:128], in_=wa[64:128]), reads=[wab], partial=[twab])
                k.op("act", lambda e: e.activation(out=sg0[:], in_=gd0[:], func=AF.Sigmoid), reads=[gd0b], writes=[sg0b])
                k.op("act", lambda e: e.activation(out=sg1[0:32], in_=gd1[0:32], func=AF.Sigmoid), reads=[gd1b], writes=[sg1b])
                k.op("act", lambda e: e.copy(out=sg1[32:64], in_=gd1[32:64]), reads=[gd1b], partial=[sg1b])
            hc = slice(hp * 128, (hp + 1) * 128)
            ii = (tt * 4 + hp) % 2
            r_, rb = rt[ii]; k_, kb = kt[ii]; v_, vb = vt[ii]
            k.dma("sp", r_[:], zT[hp, :, ts_], rb, writes=[rb])
            k.dma("sp", k_[:], zT[4 + hp, :, ts_], kb, writes=[kb])
            k.dma("sp", v_[:], zT[8 + hp, :, ts_], vb, writes=[vb])
            p, pb = prP.get()
            k.op("pe", lambda e, p=p: e.matmul(p[:], lhsT=rw[0:64, 0, hc], rhs=twa[0:64], start=True, stop=True),
                 reads=[rwb, twab], writes=[pb])
            k.op("act", lambda e, p=p: e.activation(out=sgm[:], in_=p[:], func=AF.Sigmoid, bias=pc("w0", hp)),
                 reads=[pb, parb], writes=[sgmb])
            p, pb = prP.get()
            k.op("pe", lambda e, p=p: e.matmul(p[:], lhsT=rw[64:128, 0, hc], rhs=twa[64:128], start=True, stop=True),
                 reads=[rwb, twab], writes=[pb])
            k.op("act", lambda e, p=p: e.activation(out=aa[:], in_=p[:], func=AF.Sigmoid, bias=pc("a0", hp)),
                 reads=[pb, parb], writes=[aab])
            p, pb = prP.get()
            k.op("pe", lambda e, p=p: e.matmul(p[:], lhsT=rw[:, 1, hc], rhs=sg0[:], start=True, stop=False),
                 reads=[rwb, sg0b], writes=[pb])
            k.op("pe", lambda e, p=p: e.matmul(p[:], lhsT=rw[0:32, 2, hc], rhs=sg1[0:32], start=False, stop=True),
                 reads=[rwb, sg1b], partial=[pb])
            k.op("act", lambda e, p=p: e.copy(out=gg[:, tt % 2, hp, :], in_=p[:]), reads=[pb], partial=[ggb])
            if l == 0:
                k.dma("sp", vfT[hp, :, ts_], v_[:], vb, reads=[vb])
                vq, vqb = v_, vb
            else:
                k.dma("sp", vft[:], vfT[hp, :, ts_], vftb, writes=[vftb])
                p, pb = prP.get()
                k.op("pe", lambda e, p=p: e.matmul(p[:], lhsT=rw[32:64, 2, hc], rhs=sg1[32:64], start=True, stop=True),
                     reads=[rwb, sg1b], writes=[pb])
                k.op("act", lambda e, p=p: e.activation(out=vp[:], in_=p[:], func=AF.Sigmoid, bias=pc("v0", hp)),
                     reads=[pb, parb], writes=[vpb])
                k.op("pool", lambda e: e.tensor_tensor(out=vft[:], in0=vft[:], in1=v_[:], op=ALU.subtract),
                     reads=[vftb, vb], writes=[vftb])
                k.op("pool", lambda e: e.tensor_tensor(out=vp[:], in0=vp[:], in1=vft[:], op=ALU.mult),
                     reads=[vpb, vftb], writes=[vpb])
                k.op("pool", lambda e: e.tensor_tensor(out=vp[:], in0=vp[:], in1=v_[:], op=ALU.add),
                     reads=[vpb, vb], writes=[vpb])
                vq, vqb = vp, vpb
            k.op("dve", lambda e: e.tensor_scalar(out=kkr[:], in0=k_[:], scalar1=pc("kk", hp), scalar2=None, op0=ALU.mult),
                 reads=[kb, parb], writes=[kkrb])
            k.op("act", lambda e: e.activation(out=sq[:], in_=kkr[:], func=AF.Square), reads=[kkrb], writes=[sqb])
            p, pb = prP.get()
            k.op("pe", lambda e, p=p: e.matmul(p[:], lhsT=c.bones[:], rhs=sq[:], start=True, stop=True),
                 reads=[c.bonesb, sqb], writes=[pb])
            k.op("act", lambda e, p=p: e.activation(out=rn[:], in_=p[:], func=AF.Sqrt, bias=lnxe[:, 1:2]),
                 reads=[pb, lnxeb], writes=[rnb])
            k.op("dve", lambda e: e.reciprocal(out=rn[:], in_=rn[:]), reads=[rnb], writes=[rnb])
            k.op("dve", lambda e: e.tensor_tensor(out=kk[:], in0=kkr[:], in1=rn[:], op=ALU.mult), reads=[kkrb, rnb], writes=[kkb])
            k.op("dve", lambda e: e.tensor_scalar(out=tA[:], in0=aa[:], scalar1=pc("ka", hp), scalar2=omka[:, hp:hp + 1],
                                                  op0=ALU.mult, op1=ALU.add), reads=[aab, parb, omkab], writes=[tAb])
            k.op("pool", lambda e: e.tensor_tensor(out=kp[:], in0=k_[:], in1=tA[:], op=ALU.mult), reads=[kb, tAb], writes=[kpb])
            k.op("pool", lambda e: e.tensor_tensor(out=bs[:], in0=kk[:], in1=aa[:], op=ALU.mult), reads=[kkb, aab], writes=[bsb])
            k.op("dve", lambda e: e.scalar_tensor_tensor(out=rk[:], in0=r_[:], scalar=pc("rk", hp), in1=kp[:],
                                                         op0=ALU.mult, op1=ALU.mult), reads=[rb, kpb, parb], writes=[rkb])
            p, pb = prP.get()
            k.op("pe", lambda e, p=p: e.matmul(p[:], lhsT=c.bones[:], rhs=rk[:], start=True, stop=True),
                 reads=[c.bonesb, rkb], writes=[pb])
            k.op("dve", lambda e, p=p: e.tensor_tensor(out=bon[:, tt % 2, hp, :], in0=p[:], in1=vq[:], op=ALU.mult),
                 reads=[pb, vqb], partial=[bonb])
            k.op("dve", lambda e: e.tensor_tensor_scan(out=cs[:], data0=c.ones3[:, 0:4, :].rearrange("p a b -> p (a b)"), data1=sgm[:],
                                                       initial=0.0, op0=ALU.mult, op1=ALU.add),
                 reads=[c.ones3b, sgmb], writes=[csb])
            k.op("pool", lambda e: e.memset(basec[:, 0:1], 0.0), partial=[basecb])
            k.op("pool", lambda e: e.tensor_copy(out=basec[:, 1:8], in_=cs[:].rearrange("p (c t) -> p c t", t=64)[:, 0:7, 63]),
                 reads=[csb], partial=[basecb])
            k.op("dve", lambda e: e.tensor_tensor(out=q[:], in0=cs[:].rearrange("p (c t) -> p c t", t=64),
                                                  in1=basec[:].unsqueeze(2).to_broadcast([128, 8, 64]), op=ALU.subtract),
                 reads=[csb, basecb], writes=[qb])
            k.op("pool", lambda e: e.tensor_tensor(out=qm[:], in0=q[:], in1=sgm[:].rearrange("p (c t) -> p c t", t=64), op=ALU.subtract),
                 reads=[qb, sgmb], writes=[qmb])
            k.op("act", lambda e: e.activation(out=E1[:], in_=qm[:], func=AF.Exp, scale=-C0), reads=[qmb], writes=[E1b])
            k.op("act", lambda e: e.activation(out=E2[:], in_=q[:], func=AF.Exp, scale=C0), reads=[qb], writes=[E2b])
            k.op("act", lambda e: e.activation(out=E3[:], in_=q[:], func=AF.Exp, scale=-C0), reads=[qb], writes=[E3b])
            k.op("pool", lambda e: e.tensor_tensor(out=qm[:], in0=q[:], in1=q[:, :, 63:64].to_broadcast([128, 8, 64]), op=ALU.subtract),
                 reads=[qb, E1b], writes=[qmb])
            k.op("act", lambda e: e.activation(out=Eh[:], in_=qm[:], func=AF.Exp, scale=C0), reads=[qmb], writes=[Ehb])
            c3 = lambda t: t[:].rearrange("p (c t) -> p c t", t=64)
            k.op("dve", lambda e: e.scalar_tensor_tensor(out=R32(AR[:, :, 0:64]), in0=c3(kk), scalar=-1.0, in1=E1[:], op0=ALU.mult, op1=ALU.mult),
                 reads=[kkb, E1b], partial=[ARb])
            k.op("pool", lambda e: e.tensor_tensor(out=R32(AR[:, :, 64:128]), in0=c3(r_), in1=E3[:], op=ALU.mult), reads=[rb, E3b], partial=[ARb])
            k.op("dve", lambda e: e.tensor_tensor(out=R32(BtT[:]), in0=c3(bs), in1=E2[:], op=ALU.mult), reads=[bsb, E2b], writes=[BtTb])
            k.op("pool", lambda e: e.tensor_tensor(out=R32(KtT[:]), in0=c3(kp), in1=E2[:], op=ALU.mult), reads=[kpb, E2b], writes=[KtTb])
            k.op("dve", lambda e: e.tensor_tensor(out=BhT[:], in0=c3(bs), in1=Eh[:], op=ALU.mult), reads=[bsb, Ehb], writes=[BhTb])
            k.op("pool", lambda e: e.tensor_tensor(out=KhT[:], in0=c3(kp), in1=Eh[:], op=ALU.mult), reads=[kpb, Ehb], writes=[KhTb])
            for (src, srcb, srcf, dst, dstb, dstf) in [
                    (AR, ARb, lambda cc: AR[:, cc, 0:64], Xtok, Xtokb, lambda: Xtok[:, :, 0:64]),
                    (BhT, BhTb, lambda cc: BhT[:, cc, :], Bhk, Bhkb, lambda: Bhk[:]),
                    (KhT, KhTb, lambda cc: KhT[:, cc, :], Khk, Khkb, lambda: Khk[:]),
                    (vq, vqb, lambda cc: vq[:, cc * 64:(cc + 1) * 64], Vtok, Vtokb, lambda: Vtok[:])]:
                p, pb = prP.get()
                first = True
                for cc in range(8):
                    for h in range(2):
                        k.op("pe", lambda e, p=p, h=h, cc=cc, srcf=srcf: e.matmul(
                            p[HS(h), cc * 64:(cc + 1) * 64], lhsT=srcf(cc)[HS(h)], rhs=c.ident[HS(h), HS(h)], start=True, stop=True),
                            reads=[srcb, c.identb], writes=[pb] if first else (), partial=() if first else [pb])
                        first = False
                k.op("act", lambda e, p=p, dstf=dstf: e.copy(out=R32(dstf()), in_=p[:].rearrange("p (c t) -> p c t", t=64)),
                     reads=[pb], partial=[dstb])
            k.op("pool", lambda e: e.tensor_copy(out=gam[:], in_=E3[:, :, 63]), reads=[E3b], writes=[gamb])

        def C_body(tt, hp):
            ifs = IF[(tt * 4 + hp) % 2]
            AR, ARb = ifs["AR"]; BtT, BtTb = ifs["BtT"]; KtT, KtTb = ifs["KtT"]; Xtok, Xtokb = ifs["Xtok"]
            Bhk, Bhkb = ifs["Bhk"]; Khk, Khkb = ifs["Khk"]; Vtok, Vtokb = ifs["Vtok"]; gam, gamb = ifs["gam"]
            ts_ = slice(tt * 512, (tt + 1) * 512)
            p, pb = prC.get()
            umm(k, p, pb, range(8), 64, [(lambda h, cc: AR[HS(h), cc, 0:64], lambda h, cc: BtT[HS(h), cc, :])], [ARb, BtTb])
            Lc, Lcb = Lp[0]
            k.op("dve", lambda e, p=p: e.tensor_tensor(out=R32(Lc[:]), in0=p[:].rearrange("p (c t) -> p c t", t=64), in1=c.maskL[:], op=ALU.mult),
                 reads=[pb, c.maskLb], writes=[Lcb])
            for (lt, ltb, dst, dstb) in [(BtT, BtTb, NM, NMb), (KtT, KtTb, KM, KMb)]:
                for half in range(2):
                    p, pb = prC.get()
                    umm(k, p, pb, range(half * 4, half * 4 + 4), 128,
                        [(lambda h, cc, lt=lt: lt[HS(h), cc, :], lambda h, cc: AR[HS(h), cc, :])], [ltb, ARb])
                    k.op("dve", lambda e, p=p, dst=dst, half=half: e.tensor_tensor(
                        out=R32(dst[:, half * 4:half * 4 + 4, :]), in0=p[:].rearrange("p (c t) -> p c t", t=128), in1=c.maskNM[:], op=ALU.mult),
                        reads=[pb, c.maskNMb], partial=[dstb])
            Nc, Ncb = Np[0]
            k.op("pool", lambda e: e.tensor_copy(out=R32(Nc[:]), in_=NM[:, :, 0:64]), reads=[NMb], writes=[Ncb])
            Pc, Pcb = Pp[0]
            k.op("pool", lambda e: e.tensor_tensor(out=R32(Pc[:]), in0=NM[:, :, 0:64], in1=c.i64[:], op=ALU.add), reads=[NMb, c.i64b], writes=[Pcb])
            for j in range(5):
                Ln, Lnb = Lp[(j + 1) % 2]
                Nn, Nnb = Np[(j + 1) % 2]
                Pn, Pnb = Pp[(j + 1) % 2]
                p, pb = prC.get()
                umm(k, p, pb, range(8), 64, [(lambda h, cc, Nc=Nc: Nc[HS(h), cc, :], lambda h, cc, Lc=Lc: Lc[HS(h), cc, :])], [Ncb, Lcb])
                if j < 4:
                    p2, p2b = prC.get()
                    umm(k, p2, p2b, range(8), 64, [(lambda h, cc, Lc=Lc: Lc[HS(h), cc, :], lambda h, cc, Nc=Nc: Nc[HS(h), cc, :])], [Ncb, Lcb])
                k.op("act", lambda e, p=p, Ln=Ln: e.copy(out=R32(Ln[:]), in_=p[:].rearrange("p (c t) -> p c t", t=64)), reads=[pb], writes=[Lnb])
                if j < 4:
                    k.op("dve", lambda e, p2=p2, Nn=Nn: e.tensor_copy(out=R32(Nn[:]), in_=p2[:].rearrange("p (c t) -> p c t", t=64)), reads=[p2b], writes=[Nnb])
                p3, p3b = prC.get()
                umm(k, p3, p3b, range(8), 64, [(lambda h, cc, Ln=Ln: Ln[HS(h), cc, :], lambda h, cc, Pc=Pc: Pc[HS(h), cc, :])], [Lnb, Pcb])
                k.op("dve", lambda e, p3=p3, Pn=Pn, Pc=Pc: e.tensor_tensor(out=R32(Pn[:]), in0=p3[:].rearrange("p (c t) -> p c t", t=64), in1=Pc[:], op=ALU.add),
                     reads=[p3b, Pcb], writes=[Pnb])
                Lc, Lcb, Nc, Ncb, Pc, Pcb = Ln, Lnb, Nn, Nnb, Pn, Pnb
            p, pb = prC.get()
            umm(k, p, pb, range(8), 64, [(lambda h, cc: KM[HS(h), cc, 0:64], lambda h, cc: Vtok[HS(h), cc, :])], [KMb, Vtokb])
            k.op("act", lambda e, p=p: e.copy(out=R32(Xtok[:, :, 64:128]), in_=p[:].rearrange("p (c t) -> p c t", t=64)), reads=[pb], partial=[Xtokb])
            for half in range(2):
                p, pb = prC.get()
                umm(k, p, pb, range(half * 4, half * 4 + 4), 128,
                    [(lambda h, cc, Pc=Pc: Pc[HS(h), cc, :], lambda h, cc: Xtok[HS(h), cc, :])], [Pcb, Xtokb])
                eng = "act" if half == 0 else "dve"
                if eng == "act":
                    k.op("act", lambda e, p=p, half=half: e.copy(out=R32(WU[:, half * 4:half * 4 + 4, :]), in_=p[:].rearrange("p (c t) -> p c t", t=128)),
                         reads=[pb], partial=[WUb])
                else:
                    k.op("dve", lambda e, p=p, half=half: e.tensor_copy(out=R32(WU[:, half * 4:half * 4 + 4, :]), in_=p[:].rearrange("p (c t) -> p c t", t=128)),
                         reads=[pb], partial=[WUb])
            k.coop_wait("S")
            p, pb = prC.get()
            umm(k, p, pb, range(8), 64, [(lambda h, cc: WU[HS(h), cc, 0:64], lambda h, cc: NM[HS(h), cc, 64:128])], [WUb, NMb])
            k.op("dve", lambda e, p=p: e.tensor_tensor(out=R2T[:, hp], in0=p[:].rearrange("p (c t) -> p c t", t=64), in1=AR[:, :, 64:128], op=ALU.add),
                 reads=[pb, ARb], partial=[R2Tb])
            p, pb = prC.get()
            umm(k, p, pb, range(8), 64, [(lambda h, cc: WU[HS(h), cc, 64:128], lambda h, cc: NM[HS(h), cc, 64:128]),
                                         (lambda h, cc: Vtok[HS(h), cc, :], lambda h, cc: KM[HS(h), cc, 64:128])], [WUb, NMb, Vtokb, KMb])
            k.op("act", lambda e, p=p: e.copy(out=Y0T[:, hp], in_=p[:].rearrange("p (c t) -> p c t", t=64)), reads=[pb], partial=[Y0Tb])
            p, pb = prC.get()
            umm(k, p, pb, range(8), 64, [(lambda h, cc: WU[HS(h), cc, 0:64], lambda h, cc: Bhk[HS(h), cc, :])], [WUb, Bhkb])
            k.op("pool", lambda e: e.tensor_tensor(out=Igam[:], in0=c.i64[:], in1=gam[:].unsqueeze(2).to_broadcast([128, 8, 64]), op=ALU.mult),
                 reads=[c.i64b, gamb], writes=[Igamb])
            k.op("dve", lambda e, p=p: e.tensor_tensor(out=GT[:, hp], in0=p[:].rearrange("p (c t) -> p c t", t=64), in1=Igam[:], op=ALU.add),
                 reads=[pb, Igamb], partial=[GTb])
            p, pb = prC.get()
            umm(k, p, pb, range(8), 64, [(lambda h, cc: Bhk[HS(h), cc, :], lambda h, cc: WU[HS(h), cc, 64:128]),
                                         (lambda h, cc: Khk[HS(h), cc, :], lambda h, cc: Vtok[HS(h), cc, :])], [Bhkb, WUb, Khkb, Vtokb])
            k.op("act", lambda e, p=p: e.copy(out=HH[:, hp], in_=p[:].rearrange("p (c t) -> p c t", t=64)), reads=[pb], partial=[HHb])

        def S_body(tt):
            ts_ = slice(tt * 512, (tt + 1) * 512)
            for cc in range(8):
                pY, pYb = prS.get()
                pS, pSb = prS.get()
                first = True
                for hp in range(4):
                    for h in range(2):
                        k.op("pe", lambda e, hp=hp, h=h, pY=pY: e.matmul(pY[HS(h), hp * 64:(hp + 1) * 64], lhsT=S[HS(h), hp, :], rhs=R2T[HS(h), hp, cc, :],
                                                                           start=True, stop=True),
                             reads=[Sb, R2Tb], writes=[pYb] if first else (), partial=() if first else [pYb])
                        k.op("pe", lambda e, hp=hp, h=h, pS=pS: e.matmul(pS[HS(h), hp * 64:(hp + 1) * 64], lhsT=GT[HS(h), hp, cc, :], rhs=S[HS(h), hp, :],
                                                                           start=True, stop=True),
                             reads=[Sb, GTb], writes=[pSb] if first else (), partial=() if first else [pSb])
                        first = False
                k.op("dve", lambda e, pY=pY: e.tensor_tensor(out=yT[:, :, cc * 64:(cc + 1) * 64], in0=pY[:, 0:256].rearrange("p (a t) -> p a t", t=64),
                                                              in1=Y0T[:, :, cc, :], op=ALU.add), reads=[pYb, Y0Tb], partial=[yTb])
                k.op("dve", lambda e, pS=pS: e.tensor_tensor(out=S[:], in0=pS[:, 0:256].rearrange("p (a t) -> p a t", t=64),
                                                              in1=HH[:, :, cc, :], op=ALU.add), reads=[pSb, HHb], writes=[Sb])
            for hp in range(4):
                p, pb = prS.get()
                k.op("pe", lambda e, p=p: e.matmul(p[:], lhsT=c.bones[:], rhs=yT[:, hp, :], start=True, stop=True),
                     reads=[c.bonesb, yTb], writes=[pb])
                k.op("dve", lambda e, p=p: e.scalar_tensor_tensor(out=dd[:], in0=p[:], scalar=-1.0 / 64, in1=yT[:, hp, :], op0=ALU.mult, op1=ALU.add),
                     reads=[pb, yTb], writes=[ddb])
                k.op("act", lambda e: e.activation(out=sq[:], in_=dd[:], func=AF.Square), reads=[ddb], writes=[sqb])
                p, pb = prS.get()
                k.op("pe", lambda e, p=p: e.matmul(p[:], lhsT=c.bones[:], rhs=sq[:], start=True, stop=True),
                     reads=[c.bonesb, sqb], writes=[pb])
                k.op("act", lambda e, p=p: e.activation(out=rn[:], in_=p[:], func=AF.Sqrt, scale=1.0 / 64, bias=lnxe[:, 0:1]),
                     reads=[pb, lnxeb], writes=[rnb])
                k.op("dve", lambda e: e.reciprocal(out=rn[:], in_=rn[:]), reads=[rnb], writes=[rnb])
                k.op("dve", lambda e: e.tensor_tensor(out=dd[:], in0=dd[:], in1=rn[:], op=ALU.mult), reads=[ddb, rnb], writes=[ddb])
                k.op("dve", lambda e: e.tensor_scalar(out=dd[:], in0=dd[:], scalar1=pc("lng", hp), scalar2=pc("lnb", hp), op0=ALU.mult, op1=ALU.add),
                     reads=[ddb, parb], writes=[ddb])
                k.op("pool", lambda e: e.tensor_tensor(out=dd[:], in0=dd[:], in1=bon[:, tt % 2, hp, :], op=ALU.add), reads=[ddb, bonb], writes=[ddb])
                y_, yb = yo[hp % 2]
                k.op("pool", lambda e, y_=y_: e.tensor_tensor(out=y_[:], in0=dd[:], in1=gg[:, tt % 2, hp, :], op=ALU.mult), reads=[ddb, ggb], writes=[yb])
                k.dma("sp", mixT[hp, :, ts_], y_[:], yb, reads=[yb])

        its = [(tt, hp) for tt in range(NT) for hp in range(4)]
        P_body(*its[0])
        pendS = None
        for i, (tt, hp) in enumerate(its):
            streams = [("C", lambda tt=tt, hp=hp: C_body(tt, hp))]
            if i + 1 < len(its):
                streams.append(("P", lambda n=its[i + 1]: P_body(*n)))
            if pendS is not None:
                streams.append(("S", lambda t_=pendS: S_body(t_)))
                pendS = None
            k.coop_run(streams)
            if hp == 3:
                pendS = tt
        S_body(pendS)
        k.barrier()

TWO_PI = float(2 * np.pi)
CW1 = 6.28125
CW2 = float(2 * np.pi - 6.28125)


def host_consts():
    f32 = np.float32
    invf = (np.float32(10000.0) ** (-np.arange(0, 32, 2, dtype=np.float32) / np.float32(32))).astype(f32)
    cc = np.zeros((128, 40), f32)
    p = np.arange(128)
    cc[:, 0] = invf[p % 16]
    cc[:, 1] = np.where((p % 32) < 16, -1.0, 1.0)
    wins = {0: (2, 4), 1: (8, 16)}
    for pc in range(2):
        w = np.where(p < 64, wins[pc][0], wins[pc][1]).astype(f32)
        cc[:, 2 + pc] = 1.0 / w
        for t in range(16):
            cc[:, 4 + pc * 16 + t] = w / np.minimum(t + 1, w)
    return cc


def phase_rope_tables(nc, k, c, pos, csT, snT):
    with ExitStack() as es:
        pi_, pib = sb(nc, es, "posi", [128, 1024], I32)
        ang, angb = sb(nc, es, "ang", [128, 1024])
        a2, a2b = sb(nc, es, "ang2", [128, 1024])
        qi, qib = sb(nc, es, "qi", [128, 1024], I32)
        qf, qfb = sb(nc, es, "qf", [128, 1024])
        m, mb = sb(nc, es, "msk", [128, 1024])
        o, ob = sb(nc, es, "tab", [128, 1024])
        halfpi, hpb = sb(nc, es, "halfpi", [128, 2])
        for piece in range(4):
            sl = slice(piece * 1024, (piece + 1) * 1024)
            k.dma("sp", pi_[:], pos[sl].partition_broadcast(128), pib, writes=[pib])
            k.op("dve", lambda e: e.tensor_copy(out=ang[:], in_=pi_[:]), reads=[pib], writes=[angb])
            k.op("dve", lambda e: e.tensor_scalar(out=ang[:], in0=ang[:], scalar1=c.hc[:, 0:1], scalar2=None, op0=ALU.mult),
                 reads=[angb, c.hcb], writes=[angb])
            for which, dst in ((0, snT), (1, csT)):
                if which == 1:
                    k.op("dve", lambda e: e.tensor_scalar(out=a2[:], in0=ang[:], scalar1=float(np.pi / 2), scalar2=None, op0=ALU.add),
                         reads=[angb], writes=[a2b])
                else:
                    k.op("dve", lambda e: e.tensor_copy(out=a2[:], in_=ang[:]), reads=[angb], writes=[a2b])
                k.op("dve", lambda e: e.tensor_scalar(out=qf[:], in0=a2[:], scalar1=float(1.0 / TWO_PI), scalar2=None, op0=ALU.mult),
                     reads=[a2b], writes=[qfb])
                k.op("dve", lambda e: e.tensor_copy(out=qi[:], in_=qf[:]), reads=[qfb], writes=[qib])
                k.op("dve", lambda e: e.tensor_copy(out=qf[:], in_=qi[:]), reads=[qib], writes=[qfb])
                k.op("dve", lambda e: e.scalar_tensor_tensor(out=a2[:], in0=qf[:], scalar=-CW1, in1=a2[:], op0=ALU.mult, op1=ALU.add),
                     reads=[qfb, a2b], writes=[a2b])
                k.op("dve", lambda e: e.scalar_tensor_tensor(out=a2[:], in0=qf[:], scalar=-CW2, in1=a2[:], op0=ALU.mult, op1=ALU.add),
                     reads=[qfb, a2b], writes=[a2b])
                k.op("dve", lambda e: e.tensor_scalar(out=m[:], in0=a2[:], scalar1=float(np.pi), scalar2=-TWO_PI, op0=ALU.is_gt, op1=ALU.mult),
                     reads=[a2b], writes=[mb])
                k.op("dve", lambda e: e.tensor_tensor(out=a2[:], in0=a2[:], in1=m[:], op=ALU.add), reads=[a2b, mb], writes=[a2b])
                k.op("dve", lambda e: e.tensor_scalar(out=m[:], in0=a2[:], scalar1=float(-np.pi), scalar2=TWO_PI, op0=ALU.is_lt, op1=ALU.mult),
                     reads=[a2b], writes=[mb])
                k.op("dve", lambda e: e.tensor_tensor(out=a2[:], in0=a2[:], in1=m[:], op=ALU.add), reads=[a2b, mb], writes=[a2b])
                k.op("dve", lambda e: e.tensor_scalar(out=a2[:], in0=a2[:], scalar1=float(np.pi), scalar2=float(-np.pi), op0=ALU.min, op1=ALU.max),
                     reads=[a2b], writes=[a2b])
                k.op("act", lambda e: e.activation(out=o[:], in_=a2[:], func=AF.Sin), reads=[a2b], writes=[ob])
                if which == 0:
                    k.op("dve", lambda e: e.tensor_scalar(out=o[:], in0=o[:], scalar1=c.hc[:, 1:2], scalar2=None, op0=ALU.mult),
                         reads=[ob, c.hcb], writes=[ob])
                k.dma("sp", dst[:, sl], o[:], ob, reads=[ob])
        k.barrier()


def phase_pool_mla(nc, k, c, L, zT, csT, snT, mixT, l):
    SC = float(96 ** -0.5)
    with ExitStack() as es:
        par, parb = c.par, c.parb
        pc = lambda nm, i: par[:, PCOL[nm][0] + i:PCOL[nm][0] + i + 1]
        T2 = lambda nm, shape=[128, 512], dt=F32: sb(nc, es, nm, shape, dt)
        poolw, poolwb = T2("poolw", [128, 2, 128], BF16)
        k.dma("pool", poolw[:], L["poolw"], poolwb, writes=[poolwb])
        wq, wqb = T2("wq", [128, 2, 768], BF16)
        k.dma("pool", wq[:], L["wq"], wqb, writes=[wqb])
        wkv, wkvb = T2("wkv", [128, 512], BF16)
        k.dma("pool", wkv[:], L["wkv"], wkvb, writes=[wkvb])
        KTbs = [Buf("KTb%d" % i) for i in range(NT)]
        V1bs = [Buf("V1b%d" % i) for i in range(NT)]
        KT, KTb = T2("KT", [128, 4, T], BF16)
        V1, V1b = T2("V1", [128, 32, 4, 65], BF16)
        k.op("pool", lambda e: e.memset(V1[:, :, :, 64:65], 1.0), partial=V1bs)
        tri, trib = T2("tri", [128, 128], BF16)
        k.op("pool", lambda e: e.affine_select(out=tri[:], in_=c.onesbf[:], pattern=[[1, 128]], compare_op=ALU.is_ge, fill=0.0,
                                               base=0, channel_multiplier=-1), reads=[c.onesbfb], writes=[trib])
        ub = [T2("ub%d" % i, [128, 528]) for i in range(2)]
        s2, s2b = T2("s2", [128, 528]); s4, s4b = T2("s4", [128, 528]); s8, s8b = T2("s8", [128, 528])
        pp, ppb = T2("pp"); ppbf, ppbfb = T2("ppbf", dt=BF16)
        yo = [T2("pyo%d" % i, dt=BF16) for i in range(2)]
        ql, qlb = T2("ql", [128, 2, 512]); kvl, kvlb = T2("kvl")
        krA, krAb = T2("krA"); krB, krBb = T2("krB"); cst, cstb = T2("cst"); snt, sntb = T2("snt")
        sqb_, sqbb = T2("msq", [128, 2, 512], BF16); rs, rsb = T2("mrs")
        qn, qnb = T2("qn", [128, 2, 512], BF16); kvn, kvnb = T2("kvn", dt=BF16)
        kr, krb = T2("kr"); t1, t1b = T2("mt1"); t2, t2b = T2("mt2")
        pre, preb = T2("pre"); QTs = [T2("QT%d" % i, [128, 4, 512], BF16) for i in range(2)]
        PT = [T2("PT%d" % i, dt=BF16) for i in range(2)]
        rec, recb = T2("rec", [128, 4]); ytok, ytokb = T2("ytok", [128, 4, 256])
        ymT = [T2("ymT%d" % i, [128, 512], BF16) for i in range(2)]
        for u_, ubb in ub:
            k.op("pool", lambda e, u_=u_: e.memset(u_[:], 0.0), writes=[ubb])
        prm = PsRot(c, [4, 5, 6])
        prl = PsRot(c, [7])
        hcol = lambda i: c.hc[:, i:i + 1]

        def rms_rows(src, srcb, nrows, nko, dim):
            p, pb = prm.get()
            for ko in range(nko):
                s_ap = src[0:nrows, ko, :] if nko > 1 else src[0:nrows]
                q_ap = sqb_[0:nrows, ko, :]
                k.op("act", lambda e, s_ap=s_ap, q_ap=q_ap: e.activation(out=q_ap, in_=s_ap, func=AF.Square), reads=[srcb],
                     writes=[sqbb] if ko == 0 else (), partial=[sqbb] if ko else ())
            for ko in range(nko):
                k.op("pe", lambda e, p=p, ko=ko: e.matmul(p[0:nrows, :], lhsT=c.onesbf[0:nrows, 0:nrows], rhs=sqb_[0:nrows, ko, :],
                                                          start=(ko == 0), stop=(ko == nko - 1)),
                     reads=[sqbb, c.onesbfb], writes=[pb] if ko == 0 else (), partial=[pb] if ko else ())
            k.op("act", lambda e, p=p: e.activation(out=rs[0:nrows], in_=p[0:nrows, :], func=AF.Sqrt, scale=1.0 / dim, bias=c.epsc[0:nrows, 0:1]),
                 reads=[pb, c.epscb], writes=[rsb])
            k.op("dve", lambda e: e.reciprocal(out=rs[0:nrows], in_=rs[0:nrows]), reads=[rsb], writes=[rsb])

        def pool_body(tt):
            ts_ = slice(tt * 512, (tt + 1) * 512)
            for pcn in range(2):
                u_, ubb = ub[pcn]
                if tt > 0:
                    k.op("pool", lambda e, u_=u_: e.tensor_copy(out=u_[:, 0:16], in_=u_[:, 512:528]), reads=[ubb], writes=[ubb])
                k.dma("sp", u_[:, 16:528], zT[C_POOL + pcn, :, ts_], ubb, partial=[ubb], reads=[ubb])
                k.op("pool", lambda e, u_=u_: e.tensor_tensor(out=s2[:, 1:528], in0=u_[:, 1:528], in1=u_[:, 0:527], op=ALU.add), reads=[ubb], writes=[s2b])
                if pcn == 0:
                    k.op("pool", lambda e: e.tensor_tensor(out=s2[64:128, 3:528], in0=s2[64:128, 3:528], in1=s2[64:128, 1:526], op=ALU.add),
                         reads=[s2b], writes=[s2b])
                    res, resb = s2, s2b
                else:
                    k.op("pool", lambda e: e.tensor_tensor(out=s4[:, 3:528], in0=s2[:, 3:528], in1=s2[:, 1:526], op=ALU.add), reads=[s2b], writes=[s4b])
                    k.op("pool", lambda e: e.tensor_tensor(out=s8[:, 7:528], in0=s4[:, 7:528], in1=s4[:, 3:524], op=ALU.add), reads=[s4b], writes=[s8b])
                    k.op("pool", lambda e: e.tensor_tensor(out=s8[64:128, 15:528], in0=s8[64:128, 15:528], in1=s8[64:128, 7:520], op=ALU.add),
                         reads=[s8b], writes=[s8b])
                    res, resb = s8, s8b
                if tt == 0:
                    k.op("dve", lambda e, res=res, pcn=pcn: e.tensor_tensor(out=res[:, 16:32], in0=res[:, 16:32], in1=c.hc[:, 4 + pcn * 16:20 + pcn * 16], op=ALU.mult),
                         reads=[resb, c.hcb], writes=[resb])
                k.op("dve", lambda e, res=res, u_=u_, pcn=pcn: e.scalar_tensor_tensor(out=ppbf[:], in0=res[:, 16:528], scalar=hcol(2 + pcn), in1=u_[:, 16:528],
                                                                               op0=ALU.mult, op1=ALU.subtract), reads=[resb, ubb, c.hcb], writes=[ppbfb])
                p, pb = prl.get()
                k.op("pe", lambda e, p=p, pcn=pcn: e.matmul(p[:], lhsT=poolw[:, pcn, :], rhs=ppbf[:], start=True, stop=True), reads=[poolwb, ppbfb], writes=[pb])
                y_, yb = yo[pcn]
                k.op("act", lambda e, p=p, y_=y_, pcn=pcn: e.activation(out=y_[:], in_=p[:], func=AF.Copy, scale=pc("pscale", pcn)), reads=[pb, parb], writes=[yb])
                k.dma("sp", mixT[4 + pcn, :, ts_], y_[:], yb, reads=[yb])

        def prep_body(tt):
            ts_ = slice(tt * 512, (tt + 1) * 512)
            QT, QTb = QTs[tt % 2]
            k.dma("sp", ql[:], zT[C_QLAT:C_QLAT + 2, :, ts_].rearrange("k p t -> p k t"), qlb, writes=[qlb])
            k.dma("sp", kvl[:], zT[C_KVLAT, :, ts_], kvlb, writes=[kvlb])
            k.dma("sp", krA[64:96], zT[C_KROPE, 0:32, ts_], krAb, writes=[krAb])
            k.dma("sp", krB[64:96], zT[C_KROPE, 32:64, ts_], krBb, writes=[krBb])
            k.dma("sp", cst[:], csT[:, ts_], cstb, writes=[cstb])
            k.dma("sp", snt[:], snT[:, ts_], sntb, writes=[sntb])
            rms_rows(ql, qlb, 128, 2, 256.0)
            for ko in range(2):
                k.op("dve", lambda e, ko=ko: e.scalar_tensor_tensor(out=qn[:, ko, :], in0=ql[:, ko, :], scalar=pc("qlg", ko), in1=rs[:],
                                                                    op0=ALU.mult, op1=ALU.mult), reads=[qlb, rsb, parb],
                     writes=[qnb] if ko == 0 else (), partial=[qnb] if ko else ())
            rms_rows(kvl, kvlb, 128, 1, 128.0) if False else None
            p, pb = prm.get()
            k.op("act", lambda e: e.activation(out=sqb_[:, 0, :], in_=kvl[:], func=AF.Square), reads=[kvlb], writes=[sqbb])
            k.op("pe", lambda e, p=p: e.matmul(p[:], lhsT=c.onesbf[:], rhs=sqb_[:, 0, :], start=True, stop=True), reads=[sqbb, c.onesbfb], writes=[pb])
            k.op("act", lambda e, p=p: e.activation(out=rs[:], in_=p[:], func=AF.Sqrt, scale=1.0 / 128, bias=c.epsc[:, 0:1]), reads=[pb, c.epscb], writes=[rsb])
            k.op("dve", lambda e: e.reciprocal(out=rs[:], in_=rs[:]), reads=[rsb], writes=[rsb])
            k.op("dve", lambda e: e.scalar_tensor_tensor(out=kvn[:], in0=kvl[:], scalar=pc("kvlg", 0), in1=rs[:], op0=ALU.mult, op1=ALU.mult),
                 reads=[kvlb, rsb, parb], writes=[kvnb])
            R_ = slice(64, 96)
            k.op("dve", lambda e: e.tensor_tensor(out=t1[R_], in0=krA[R_], in1=cst[R_], op=ALU.mult), reads=[krAb, cstb], writes=[t1b])
            k.op("dve", lambda e: e.tensor_tensor(out=t2[R_], in0=krB[R_], in1=snt[R_], op=ALU.mult), reads=[krBb, sntb], writes=[t2b])
            k.op("dve", lambda e: e.tensor_tensor(out=kr[R_], in0=t1[R_], in1=t2[R_], op=ALU.add), reads=[t1b, t2b], writes=[krb])
            for j in range(4):
                blk = tt * 4 + j
                p, pb = prm.get()
                k.op("pe", lambda e, p=p, j=j: e.matmul(p[:, 0:256], lhsT=kvn[:, j * 128:(j + 1) * 128], rhs=wkv[:, 256:512], start=True, stop=True),
                     reads=[kvnb, wkvb], writes=[pb])
                k.op("act", lambda e, p=p, blk=blk: e.copy(out=V1[:, blk, :, 0:64], in_=p[:, 0:256].rearrange("p (h d) -> p h d", d=64)),
                     reads=[pb], partial=[V1bs[tt]])
            for h in range(4):
                p, pb = prm.get()
                k.op("pe", lambda e, p=p, h=h: e.matmul(p[0:64, :], lhsT=wkv[:, h * 64:(h + 1) * 64], rhs=kvn[:], start=True, stop=True),
                     reads=[wkvb, kvnb], writes=[pb])
                k.op("act", lambda e, p=p: e.copy(out=pre[0:64], in_=p[0:64, :]), reads=[pb], writes=[preb])
                k.op("pool", lambda e: e.tensor_copy(out=pre[R_], in_=kr[R_]), reads=[krb], partial=[preb])
                p, pb = prm.get()
                k.op("act", lambda e: e.activation(out=sqb_[0:96, 0, :], in_=pre[0:96], func=AF.Square), reads=[preb], writes=[sqbb])
                k.op("pe", lambda e, p=p: e.matmul(p[0:96, :], lhsT=c.onesbf[0:96, 0:96], rhs=sqb_[0:96, 0, :], start=True, stop=True),
                     reads=[sqbb, c.onesbfb], writes=[pb])
                k.op("act", lambda e, p=p: e.activation(out=rs[0:96], in_=p[0:96, :], func=AF.Sqrt, scale=1.0 / 96, bias=c.epsc[0:96, 0:1]),
                     reads=[pb, c.epscb], writes=[rsb])
                k.op("dve", lambda e: e.reciprocal(out=rs[0:96], in_=rs[0:96]), reads=[rsb], writes=[rsb])
                k.op("dve", lambda e, h=h: e.scalar_tensor_tensor(out=KT[0:96, h, ts_], in0=pre[0:96], scalar=par[0:96, PCOL["qkk"][0]:PCOL["qkk"][0] + 1],
                                                                  in1=rs[0:96], op0=ALU.mult, op1=ALU.mult), reads=[preb, rsb, parb], partial=[KTbs[tt]])
                pA, pAb = prm.get()
                pB, pBb = prm.get()
                for ko in range(2):
                    k.op("pe", lambda e, pA=pA, ko=ko, h=h: e.matmul(pA[0:96, :], lhsT=wq[:, ko, (h * 2) * 96:(h * 2 + 1) * 96], rhs=qn[:, ko, :],
                                                                     start=(ko == 0), stop=(ko == 1)),
                         reads=[wqb, qnb], writes=[pAb] if ko == 0 else (), partial=[pAb] if ko else ())
                for ko in range(2):
                    k.op("pe", lambda e, pB=pB, ko=ko, h=h: e.matmul(pB[0:96, :], lhsT=wq[:, ko, (h * 2 + 1) * 96:(h * 2 + 2) * 96], rhs=qn[:, ko, :],
                                                                     start=(ko == 0), stop=(ko == 1)),
                         reads=[wqb, qnb], writes=[pBb] if ko == 0 else (), partial=[pBb] if ko else ())
                k.op("act", lambda e, pA=pA: e.copy(out=pre[0:64], in_=pA[0:64, :]), reads=[pAb], writes=[preb])
                k.op("dve", lambda e, pA=pA: e.tensor_tensor(out=t1[R_], in0=pA[R_, :], in1=cst[R_], op=ALU.mult), reads=[pAb, cstb], writes=[t1b])
                k.op("dve", lambda e, pB=pB: e.tensor_tensor(out=t2[R_], in0=pB[R_, :], in1=snt[R_], op=ALU.mult), reads=[pBb, sntb], writes=[t2b])
                k.op("dve", lambda e: e.tensor_tensor(out=pre[R_], in0=t1[R_], in1=t2[R_], op=ALU.add), reads=[t1b, t2b], partial=[preb])
                p, pb = prm.get()
                k.op("act", lambda e: e.activation(out=sqb_[0:96, 0, :], in_=pre[0:96], func=AF.Square), reads=[preb], writes=[sqbb])
                k.op("pe", lambda e, p=p: e.matmul(p[0:96, :], lhsT=c.onesbf[0:96, 0:96], rhs=sqb_[0:96, 0, :], start=True, stop=True),
                     reads=[sqbb, c.onesbfb], writes=[pb])
                k.op("act", lambda e, p=p: e.activation(out=rs[0:96], in_=p[0:96, :], func=AF.Sqrt, scale=1.0 / 96, bias=c.epsc[0:96, 0:1]),
                     reads=[pb, c.epscb], writes=[rsb])
                k.op("dve", lambda e: e.reciprocal(out=rs[0:96], in_=rs[0:96]), reads=[rsb], writes=[rsb])
                k.op("dve", lambda e: e.tensor_scalar(out=rs[0:96], in0=rs[0:96], scalar1=SC, scalar2=None, op0=ALU.mult), reads=[rsb], writes=[rsb])
                k.op("dve", lambda e, h=h: e.scalar_tensor_tensor(out=QT[0:96, h, :], in0=pre[0:96], scalar=par[0:96, PCOL["qkq"][0]:PCOL["qkq"][0] + 1],
                                                                  in1=rs[0:96], op0=ALU.mult, op1=ALU.mult), reads=[preb, rsb, parb], partial=[QTb])

        def attn_body(tt):
            ts_ = slice(tt * 512, (tt + 1) * 512)
            QT, QTb = QTs[tt % 2]
            for h in range(4):
                po, pob = c.ps[2 + h % 2], c.psb[2 + h % 2]
                nj = 4 * tt + 4
                for j in range(nj):
                    pS, pSb = c.ps[j % 2], c.psb[j % 2]
                    k.op("pe", lambda e, pS=pS, j=j, h=h: e.matmul(pS[:], lhsT=KT[0:96, h, j * 128:(j + 1) * 128], rhs=QT[0:96, h, :], start=True, stop=True),
                         reads=[KTbs[j // 4], QTb], writes=[pSb])
                    P_, Pb_ = PT[j % 2]
                    k.op("act", lambda e, pS=pS, P_=P_: e.activation(out=P_[:], in_=pS[:], func=AF.Exp), reads=[pSb], writes=[Pb_])
                    r = j - 4 * tt
                    if r >= 0:
                        k.op("pool", lambda e, P_=P_, r=r: e.tensor_tensor(out=P_[:, r * 128:(r + 1) * 128], in0=P_[:, r * 128:(r + 1) * 128], in1=tri[:], op=ALU.mult),
                             reads=[Pb_, trib], writes=[Pb_])
                    for i_ in range(max(r, 0), 4):
                        first = (j == 0 and i_ == max(r, 0))
                        k.op("pe", lambda e, P_=P_, i_=i_, j=j, h=h, po=po: e.matmul(po[:, i_ * 65:(i_ + 1) * 65], lhsT=P_[:, i_ * 128:(i_ + 1) * 128], rhs=V1[:, j, h, :],
                                                                                 start=(j == 0 and i_ == 0), stop=(j == 4 * tt + i_), skip_group_check=True),
                             reads=[Pb_, V1bs[j // 4]], writes=[pob] if first else (), partial=() if first else [pob])
                k.op("dve", lambda e, po=po: e.reciprocal(out=rec[:], in_=po[:, 0:260].rearrange("p (i d) -> p i d", d=65)[:, :, 64]), reads=[pob], writes=[recb])
                for i_ in range(4):
                    k.op("dve", lambda e, po=po, i_=i_, h=h: e.tensor_scalar(out=ytok[:, i_, h * 64:(h + 1) * 64], in0=po[:, i_ * 65:i_ * 65 + 64],
                                                                             scalar1=rec[:, i_:i_ + 1], scalar2=None, op0=ALU.mult),
                         reads=[pob, recb], partial=[ytokb])
            for half in range(2):
                p, pb = prm.get()
                for i_ in range(4):
                    k.op("pe", lambda e, p=p, i_=i_, half=half: e.transpose(out=p[:, i_ * 128:(i_ + 1) * 128], in_=ytok[:, i_, half * 128:(half + 1) * 128],
                                                                            identity=c.ident[:]),
                         reads=[ytokb, c.identb], writes=[pb] if i_ == 0 else (), partial=[pb] if i_ else ())
                y_, yb = ymT[half]
                k.op("act", lambda e, p=p, y_=y_: e.copy(out=y_[:], in_=p[:]), reads=[pb], writes=[yb])
                k.dma("sp", mixT[6 + half, :, ts_], y_[:], yb, reads=[yb])

        pool_body(0)
        prep_body(0)
        for tt in range(NT):
            streams = [("A", lambda tt=tt: attn_body(tt))]
            if tt + 1 < NT:
                streams.append(("P", lambda tt=tt: prep_body(tt + 1)))
                streams.append(("L", lambda tt=tt: pool_body(tt + 1)))
            k.coop_run(streams)
        k.barrier()

def phase_ffn(nc, k, c, L, xT, mixT, l, W):
    moe = (l == 1)
    if moe:
        NF = DFE // 128
        experts = [(W["moe_w_in_b"][e], W["moe_w_out_b"][e]) for e in range(NE)]
        F = DFE
    else:
        NF = DFF // 128
        experts = [(W["ffn_w_in_b"], W["ffn_w_out_b"])]
        F = DFF
    NG = NF // 2
    with ExitStack() as es:
        T2 = lambda nm, shape=[128, 512], dt=F32: sb(nc, es, nm, shape, dt)
        wout, woutb = T2("wout", [128, 8, 1024], BF16)
        for ko in range(8):
            k.dma("pool", wout[:, ko, :], L["wout"][:, ko, :], woutb, partial=[woutb])
        x1, x1b = T2("x1", [128, 8, 512]); mixt, mixtb = T2("mixt", [128, 8, 512], BF16)
        tmp, tmpb = T2("ftmp", [128, 8, 512]); sq, sqb = T2("fsq", [128, 8, 512], BF16)
        hT, hTb = T2("fhT", [128, 8, 512], BF16); rstd, rstdb = T2("frstd")
        acc, accb = T2("acc", [128, 8, 512]); aT, aTb = T2("aT", [128, NF, 512], BF16)
        wi = [T2("wi%d" % i, [128, 8, 2, 256], BF16) for i in range(2)]
        wo = [T2("wo%d" % i, [128, NF, 256], BF16) for i in range(2)]
        sg = [T2("sg%d" % i) for i in range(2)]
        ta = [T2("ta%d" % i, dt=BF16) for i in range(2)]
        Gb = [T2("Gb%d" % i, dt=BF16) for i in range(2)]
        if moe:
            rt_, rtb = T2("router", [128, 8, 8])
            k.dma("sp", rt_[:], W["moe_router"].rearrange("(ko p) e -> p ko e", p=128), rtb, writes=[rtb])
            Rp, Rpb = T2("Rp", [128, 8, 8])
            for ko in range(8):
                k.op("dve", lambda e, ko=ko: e.tensor_scalar(out=Rp[:, ko, :], in0=rt_[:, ko, :], scalar1=c.mod[:, 56 + ko:57 + ko], scalar2=None, op0=ALU.mult),
                     reads=[rtb, c.modb], writes=[Rpb] if ko == 0 else (), partial=[Rpb] if ko else ())
            brow, browb = T2("brow", [128, 8])
            p, pb = c.ps[6], c.psb[6]
            for ko in range(8):
                k.op("pe", lambda e, ko=ko: e.matmul(p[0:1, 0:8], lhsT=c.mod[:, 24 + ko:25 + ko], rhs=rt_[:, ko, :], start=(ko == 0), stop=(ko == 7)),
                     reads=[c.modb, rtb], writes=[pb] if ko == 0 else (), partial=[pb] if ko else ())
            k.op("dve", lambda e: e.tensor_copy(out=brow[0:1, :], in_=p[0:1, 0:8]), reads=[pb], writes=[browb])
            sel8, sel8b = T2("sel8", [128, 8, 128])
            k.op("dve", lambda e: e.tensor_copy(out=sel8[0:8], in_=c.ident[0:8, 0:8].unsqueeze(2).to_broadcast([8, 8, 128])), reads=[c.identb], writes=[sel8b])
            Lg, Lgb = T2("Lg", [128, 4, 8]); L2, L2b = T2("L2", [128, 4, 8]); mk, mkb = T2("mk", [128, 4, 8])
            m1, m1b = T2("m1", [128, 4]); m2, m2b = T2("m2", [128, 4]); ex, exb = T2("ex", [128, 4, 8])
            den, denb = T2("den", [128, 4]); Gt, Gtb = T2("Gt", [128, 4, 8]); GT, GTb = T2("GTr", [128, 512])
        wq_i = 0
        woq_i = 0
        ps1 = PsRot(c, [0, 1, 2, 3])
        ps2 = PsRot(c, [4, 5])
        yst = [T2("yst%d" % i, [128, 1024]) for i in range(2)] if (moe and c.out_ap is not None) else None
        for tt in range(NT):
            ts_ = slice(tt * 512, (tt + 1) * 512)
            k.dma("sp", x1[:], xT[:, :, ts_].rearrange("k p t -> p k t"), x1b, writes=[x1b])
            k.dma("sp", mixt[:], mixT[:, :, ts_].rearrange("k p t -> p k t"), mixtb, writes=[mixtb])
            for n in range(8):
                p, pb = ps1.get()
                for ko in range(8):
                    k.op("pe", lambda e, p=p, n=n, ko=ko: e.matmul(p[:], lhsT=wout[:, ko, n * 128:(n + 1) * 128], rhs=mixt[:, ko, :], start=(ko == 0), stop=(ko == 7)),
                         reads=[woutb, mixtb], writes=[pb] if ko == 0 else (), partial=[pb] if ko else ())
                k.op("dve", lambda e, p=p, n=n: e.scalar_tensor_tensor(out=x1[:, n, :], in0=p[:], scalar=c.mod[:, 16 + n:17 + n], in1=x1[:, n, :],
                                                                      op0=ALU.mult, op1=ALU.add), reads=[pb, c.modb, x1b], partial=[x1b])
            norm_mod(nc, k, c, x1, x1b, tmp, tmpb, sq, sqb, rstd, rstdb, hT, hTb, 7, 56, 24)
            if moe:
                p, pb = c.ps[6], c.psb[6]
                first = True
                for blk in range(4):
                    for ko in range(8):
                        k.op("pe", lambda e, blk=blk, ko=ko: e.matmul(p[:, blk * 8:(blk + 1) * 8], lhsT=tmp[:, ko, blk * 128:(blk + 1) * 128], rhs=Rp[:, ko, :],
                                                                      start=(ko == 0), stop=False),
                             reads=[tmpb, Rpb], writes=[pb] if first else (), partial=() if first else [pb])
                        first = False
                    k.op("pe", lambda e, blk=blk: e.matmul(p[:, blk * 8:(blk + 1) * 8], lhsT=c.ones[0:1, 0:128], rhs=brow[0:1, 0:8], start=False, stop=True),
                         reads=[c.onesb, browb], partial=[pb])
                k.op("dve", lambda e: e.tensor_copy(out=Lg[:], in_=p[:, 0:32].rearrange("p (b e) -> p b e", e=8)), reads=[pb], writes=[Lgb])
                k.op("dve", lambda e: e.tensor_reduce(out=m1[:], in_=Lg[:], axis=AX.X, op=ALU.max), reads=[Lgb], writes=[m1b])
                k.op("dve", lambda e: e.tensor_tensor(out=mk[:], in0=Lg[:], in1=m1[:].unsqueeze(2).to_broadcast([128, 4, 8]), op=ALU.is_ge), reads=[Lgb, m1b], writes=[mkb])
                k.op("dve", lambda e: e.scalar_tensor_tensor(out=L2[:], in0=mk[:], scalar=-1e30, in1=Lg[:], op0=ALU.mult, op1=ALU.add), reads=[mkb, Lgb], writes=[L2b])
                k.op("dve", lambda e: e.tensor_reduce(out=m2[:], in_=L2[:], axis=AX.X, op=ALU.max), reads=[L2b], writes=[m2b])
                k.op("dve", lambda e: e.tensor_tensor(out=mk[:], in0=Lg[:], in1=m2[:].unsqueeze(2).to_broadcast([128, 4, 8]), op=ALU.is_ge), reads=[Lgb, m2b], writes=[mkb])
                k.op("dve", lambda e: e.tensor_tensor(out=L2[:], in0=Lg[:], in1=m1[:].unsqueeze(2).to_broadcast([128, 4, 8]), op=ALU.subtract), reads=[Lgb, m1b], writes=[L2b])
                k.op("act", lambda e: e.activation(out=ex[:], in_=L2[:], func=AF.Exp), reads=[L2b], writes=[exb])
                k.op("dve", lambda e: e.tensor_tensor(out=den[:], in0=m2[:], in1=m1[:], op=ALU.subtract), reads=[m1b, m2b], writes=[denb])
                k.op("act", lambda e: e.activation(out=den[:], in_=den[:], func=AF.Exp), reads=[denb], writes=[denb])
                k.op("dve", lambda e: e.tensor_scalar(out=den[:], in0=den[:], scalar1=1.0, scalar2=None, op0=ALU.add), reads=[denb], writes=[denb])
                k.op("dve", lambda e: e.reciprocal(out=den[:], in_=den[:]), reads=[denb], writes=[denb])
                k.op("dve", lambda e: e.tensor_tensor(out=Gt[:], in0=ex[:], in1=mk[:], op=ALU.mult), reads=[exb, mkb], writes=[Gtb])
                k.op("dve", lambda e: e.tensor_tensor(out=Gt[:], in0=Gt[:], in1=den[:].unsqueeze(2).to_broadcast([128, 4, 8]), op=ALU.mult), reads=[Gtb, denb], writes=[Gtb])
                p, pb = c.ps[6], c.psb[6]
                for blk in range(4):
                    k.op("pe", lambda e, blk=blk: e.transpose(out=p[0:8, blk * 128:(blk + 1) * 128], in_=Gt[:, blk, :], identity=c.ident[:]),
                         reads=[Gtb, c.identb], writes=[pb] if blk == 0 else (), partial=[pb] if blk else ())
                k.op("dve", lambda e: e.tensor_copy(out=GT[0:8, :], in_=p[0:8, :]), reads=[pb], writes=[GTb])
            for ei, (w_in, w_out) in enumerate(experts):
                if moe:
                    p, pb = c.ps[6], c.psb[6]
                    k.op("pe", lambda e, ei=ei: e.matmul(p[:], lhsT=sel8[0:8, ei, :], rhs=GT[0:8, :], start=True, stop=True), reads=[sel8b, GTb], writes=[pb])
                    g_, gb_ = Gb[ei % 2]
                    k.op("act", lambda e, g_=g_: e.copy(out=g_[:], in_=p[:]), reads=[pb], writes=[gb_])
                w_in_v = w_in.rearrange("(ko p) n -> p ko n", p=128)
                w_out_v = w_out.rearrange("(f p) n -> p f n", p=128)
                for grp in range(NG):
                    w_, wb_ = wi[wq_i % 2]
                    wq_i += 1
                    f0 = grp * 256
                    k.dma("sp", w_[:, :, 0, :], w_in_v[:, :, f0:f0 + 256], wb_, writes=[wb_], reads=[c.castb1 if moe else c.castb])
                    k.dma("sp", w_[:, :, 1, :], w_in_v[:, :, F + f0:F + f0 + 256], wb_, partial=[wb_])
                    for fc in range(2):
                        f = grp * 2 + fc
                        pG, pGb = ps1.get()
                        pU, pUb = ps1.get()
                        for ko in range(8):
                            k.op("pe", lambda e, pG=pG, w_=w_, ko=ko, fc=fc: e.matmul(pG[:], lhsT=w_[:, ko, 0, fc * 128:(fc + 1) * 128], rhs=hT[:, ko, :],
                                                                                     start=(ko == 0), stop=(ko == 7)),
                                 reads=[wb_, hTb], writes=[pGb] if ko == 0 else (), partial=[pGb] if ko else ())
                        for ko in range(8):
                            k.op("pe", lambda e, pU=pU, w_=w_, ko=ko, fc=fc: e.matmul(pU[:], lhsT=w_[:, ko, 1, fc * 128:(fc + 1) * 128], rhs=hT[:, ko, :],
                                                                                     start=(ko == 0), stop=(ko == 7)),
                                 reads=[wb_, hTb], writes=[pUb] if ko == 0 else (), partial=[pUb] if ko else ())
                        s_, sb_ = sg[f % 2]
                        k.op("act", lambda e, pG=pG, s_=s_: e.activation(out=s_[:], in_=pG[:], func=AF.Silu), reads=[pGb], writes=[sb_])
                        if moe:
                            t_, tb_ = ta[f % 2]
                            k.op("dve", lambda e, pU=pU, s_=s_, t_=t_: e.tensor_tensor(out=t_[:], in0=pU[:], in1=s_[:], op=ALU.mult), reads=[pUb, sb_], writes=[tb_])
                            k.op("pool", lambda e, t_=t_, g_=g_, f=f: e.tensor_tensor(out=aT[:, f, :], in0=t_[:], in1=g_[:], op=ALU.mult), reads=[tb_, gb_], partial=[aTb])
                        else:
                            k.op("dve", lambda e, pU=pU, s_=s_, f=f: e.tensor_tensor(out=aT[:, f, :], in0=pU[:], in1=s_[:], op=ALU.mult), reads=[pUb, sb_], partial=[aTb])
                for qn_ in range(4):
                    w_, wb_ = wo[woq_i % 2]
                    woq_i += 1
                    k.dma("sp", w_[:], w_out_v[:, :, qn_ * 256:(qn_ + 1) * 256], wb_, writes=[wb_], reads=[c.castb1 if moe else c.castb])
                    for nn in range(2):
                        n = qn_ * 2 + nn
                        p, pb = ps2.get()
                        for f in range(NF):
                            k.op("pe", lambda e, p=p, w_=w_, f=f, nn=nn: e.matmul(p[:], lhsT=w_[:, f, nn * 128:(nn + 1) * 128], rhs=aT[:, f, :],
                                                                                 start=(f == 0), stop=(f == NF - 1)),
                                 reads=[wb_, aTb], writes=[pb] if f == 0 else (), partial=[pb] if f else ())
                        if ei == 0:
                            k.op("act", lambda e, p=p, n=n: e.copy(out=acc[:, n, :], in_=p[:]), reads=[pb], partial=[accb])
                        else:
                            k.op("dve", lambda e, p=p, n=n: e.tensor_tensor(out=acc[:, n, :], in0=p[:], in1=acc[:, n, :], op=ALU.add), reads=[pb, accb], partial=[accb])
            for n in range(8):
                k.op("dve", lambda e, n=n: e.scalar_tensor_tensor(out=acc[:, n, :], in0=acc[:, n, :], scalar=c.mod[:, 40 + n:41 + n], in1=x1[:, n, :],
                                                                 op0=ALU.mult, op1=ALU.add), reads=[accb, c.modb, x1b], partial=[accb])
            if c.out_ap is None or not moe:
                k.dma("sp", xT[:, :, ts_].rearrange("k p t -> p k t"), acc[:], accb, reads=[accb])
            else:
                for j in range(4):
                    tb = tt * 4 + j
                    s_, sb_ = yst[tb % 2]
                    for half in range(2):
                        p, pb = ps1.get()
                        for q_ in range(4):
                            ko = half * 4 + q_
                            k.op("pe", lambda e, p=p, ko=ko, q_=q_, j=j: e.transpose(
                                out=p[:, q_ * 128:(q_ + 1) * 128], in_=acc[:, ko, j * 128:(j + 1) * 128], identity=c.ident[:]),
                                reads=[accb, c.identb], writes=[pb] if q_ == 0 else (), partial=[pb] if q_ else ())
                        if half == 0:
                            k.op("dve", lambda e, p=p, s_=s_, half=half: e.tensor_copy(out=s_[:, half * 512:(half + 1) * 512], in_=p[:]),
                                 reads=[pb], partial=[sb_])
                        else:
                            k.op("act", lambda e, p=p, s_=s_, half=half: e.copy(out=s_[:, half * 512:(half + 1) * 512], in_=p[:]),
                                 reads=[pb], partial=[sb_])
                    k.dma("sp", c.out_ap[tb * 128:(tb + 1) * 128, :], s_[:], sb_, reads=[sb_])
        k.barrier()

def declare_inputs(nc, dbg):
    A = {}
    A["x"] = nc.dram_tensor("x", [T, D], F32, kind="ExternalInput").ap()
    A["cT"] = nc.dram_tensor("cT", [128, 8], F32, kind="ExternalInput").ap()
    A["pos"] = nc.dram_tensor("pos", [T], I32, kind="ExternalInput").ap()
    A["hc"] = nc.dram_tensor("hc", [128, 40], F32, kind="ExternalInput").ap()
    A["W"] = {}
    for nm, shp in [("ffn_w_in", [D, 2 * DFF]), ("ffn_w_out", [DFF, D]), ("moe_router", [D, NE]),
                    ("moe_w_in", [NE, D, 2 * DFE]), ("moe_w_out", [NE, DFE, D])]:
        A["W"][nm] = nc.dram_tensor(nm, shp, F32, kind="ExternalInput").ap()
    A["L"] = []
    for l in range(2):
        Ld = {}
        for nm, shp in LAYER_SHAPES.items():
            Ld[nm] = nc.dram_tensor("%s%d" % (nm, l), shp, F32, kind="ExternalInput").ap()
        A["L"].append(Ld)
    return A


def scratch(nc, name, shape, dt, dbg):
    kind = "ExternalOutput" if (dbg and name in dbg) else "Internal"
    return nc.dram_tensor(name, shape, dt, kind=kind).ap()


def build(nc, sigset, dbg=None, stop=None):
    k = K(nc, sigset)
    c = Ctx()
    A = declare_inputs(nc, dbg)
    out = nc.dram_tensor("out", [T, D], F32, kind="ExternalOutput").ap()
    xT = scratch(nc, "xT", [KO, 128, T], F32, dbg)
    c.out_ap = out if stop is None else None
    zT = scratch(nc, "zT", [NZ, 128, T], F32, dbg)
    vfT = scratch(nc, "vfT", [4, 128, T], F32, dbg)
    mixT = scratch(nc, "mixT", [8, 128, T], BF16, dbg)
    csT = scratch(nc, "csT", [128, T], F32, dbg)
    c.dbgK = scratch(nc, "dbgK", [128, 4, T], BF16, dbg) if (dbg and "dbgK" in dbg) else None
    c.dbgQ = scratch(nc, "dbgQ", [128, 4, T], BF16, dbg) if (dbg and "dbgK" in dbg) else None
    snT = scratch(nc, "snT", [128, T], F32, dbg)
    with ExitStack() as es:
        setup_common(nc, k, es, c)
        c.cT, c.cTb = sb(nc, es, "cT", [128, 8])
        k.dma("sp", c.cT[:], A["cT"], c.cTb, writes=[c.cTb])
        c.onesbf, c.onesbfb = sb(nc, es, "onesbf", [128, 128], BF16)
        k.op("dve", lambda e: e.tensor_copy(out=c.onesbf[:], in_=c.ones[:]), reads=[c.onesb], writes=[c.onesbfb])
        c.epsc, c.epscb = sb(nc, es, "epsc", [128, 4])
        k.op("dve", lambda e: e.memset(c.epsc[:, 0:1], EPS), partial=[c.epscb])
        setup_rwkv_consts(nc, k, es, c)
        c.hc, c.hcb = sb(nc, es, "hc", [128, 40])
        k.dma("sp", c.hc[:], A["hc"], c.hcb, writes=[c.hcb])
        k.op("dve", lambda e: e.memset(c.epsc[:, 1:2], 0.0), partial=[c.epscb])
        c.castb = Buf("cast")
        c.castb.late = True
        c.castb1 = Buf("cast1")
        c.castb1.late = True
        W = A["W"]
        W["ffn_w_in_b"] = nc.dram_tensor("ffn_w_in_b", [D, 2 * DFF], BF16).ap()
        W["ffn_w_out_b"] = nc.dram_tensor("ffn_w_out_b", [DFF, D], BF16).ap()
        W["moe_w_in_b"] = nc.dram_tensor("moe_w_in_b", [NE, D, 2 * DFE], BF16).ap()
        W["moe_w_out_b"] = nc.dram_tensor("moe_w_out_b", [NE, DFE, D], BF16).ap()
        k.dma("pool", W["ffn_w_in_b"], W["ffn_w_in"], c.castb, partial=[c.castb], nodeps=True)
        k.dma("pool", W["ffn_w_out_b"], W["ffn_w_out"], c.castb, partial=[c.castb], nodeps=True)
        phase_rope_tables(nc, k, c, A["pos"], csT, snT)
        if stop is None:
            c.x_ap = A["x"]
        else:
            c.x_ap = None
            phase_transpose_in(nc, k, c, A["x"], xT)
        for l in range(2):
            with ExitStack() as esl:
                load_params(nc, k, esl, c, A["L"][l])
                phase_inproj(nc, k, c, A["L"][l], xT, zT, l)
                if stop == "inproj%d" % l:
                    break
                c.cast_moe = None
                if not (dbg and "skiprwkv" in dbg):
                    phase_rwkv(nc, k, c, A["L"][l], zT, vfT, mixT, l)
                if stop == "rwkv%d" % l:
                    break
                phase_pool_mla(nc, k, c, A["L"][l], zT, csT, snT, mixT, l)
                if stop == "mla%d" % l:
                    break
                if l == 0:
                    for e_ in range(NE):
                        k.dma("pool", W["moe_w_in_b"][e_], W["moe_w_in"][e_], c.castb1, partial=[c.castb1], nodeps=True)
                        k.dma("pool", W["moe_w_out_b"][e_], W["moe_w_out"][e_], c.castb1, partial=[c.castb1], nodeps=True)
                phase_ffn(nc, k, c, A["L"][l], xT, mixT, l, A["W"])
                if stop == "ffn%d" % l:
                    break
        if stop is not None:
            phase_transpose_out(nc, k, c, xT, out)
    return k


ALLSIG = False
MODE_IN = 1
MODE_OUT = 1


def make_nc(dbg=None, stop=None):
    nc0 = bass.Bass("TRN2", target_bir_lowering=False)
    k0 = build(nc0, None, dbg, stop)
    nc = bass.Bass("TRN2", target_bir_lowering=False)
    k = build(nc, (None if ALLSIG else k0.needed), dbg, stop)
    return nc, k


def host_prep(inputs):
    shared = {}
    for l in range(2):
        d = prep_layer(inputs, l)
        for nm, v in d.items():
            shared["%s%d" % (nm, l)] = v
    x = np.ascontiguousarray(inputs["x"], dtype=np.float32)
    c = np.asarray(inputs["c"], np.float32)
    pos = np.asarray(inputs["positions"], np.int32)
    shared["hc"] = host_consts()
    shared["ffn_w_in"] = np.ascontiguousarray(inputs["ffn_w_in"][0], dtype=np.float32)
    shared["ffn_w_out"] = np.ascontiguousarray(inputs["ffn_w_out"][0], dtype=np.float32)
    shared["moe_router"] = np.ascontiguousarray(inputs["moe_router"][0], dtype=np.float32)
    shared["moe_w_in"] = np.ascontiguousarray(inputs["moe_w_in"][0], dtype=np.float32)
    shared["moe_w_out"] = np.ascontiguousarray(inputs["moe_w_out"][0], dtype=np.float32)
    maps = []
    for b in range(8):
        m = dict(shared)
        m["x"] = x[b]
        m["cT"] = np.ascontiguousarray(c[b].reshape(8, 128).T)
        m["pos"] = np.ascontiguousarray(pos[b])
        maps.append(m)
    return maps


def kernel(**inputs):
    nc, k = make_nc()
    in_maps = host_prep(inputs)
    res = run_bass_kernel_spmd(nc, in_maps, core_ids=list(range(8)))
    return np.stack([r["out"] for r in res.results], axis=0)
```

```python
import threading
import numpy as np
from contextlib import ExitStack
import concourse.bass as bass
import concourse.mybir as mybir
from concourse.bass_utils import run_bass_kernel_spmd

F32 = mybir.dt.float32
BF16 = mybir.dt.bfloat16
I32 = mybir.dt.int32
ALU = mybir.AluOpType
AF = mybir.ActivationFunctionType
AX = mybir.AxisListType


class Buf:
    __slots__ = ("name", "writers", "readers", "sem", "cnt", "old", "genr", "late")

    def __init__(self, name):
        self.name = name
        self.writers = []
        self.readers = []
        self.sem = None
        self.cnt = 0
        self.old = []
        self.genr = False
        self.late = False


class Rec:
    __slots__ = ("idx", "eng", "dma", "sem", "val", "ins", "buf")


COMPUTE = ("pe", "act", "dve", "pool")


class K:
    def __init__(self, nc, sigset=None):
        self.nc = nc
        self.engs = {"pe": nc.tensor, "act": nc.scalar, "dve": nc.vector,
                     "pool": nc.gpsimd, "sp": nc.sync}
        self.esem = {}
        for e in self.engs:
            self.esem[e] = nc.alloc_semaphore("es_" + e)
        self.ecnt = {e: 0 for e in self.engs}
        self.last = {e: None for e in self.engs}
        self.widx = {e: {p: -1 for p in self.engs} for e in self.engs}
        self.wdma = {e: {} for e in self.engs}
        self.nops = 0
        self.needed = set()
        self.sigset = sigset
        self.sempool = []
        self.dmabufs = []
        self.nsem = 0
        self.ninstr = {e: 0 for e in self.engs}

    def _bufsem(self, b):
        if getattr(b, "late", False):
            if b.sem is None:
                b.sem = self.nc.alloc_semaphore("ls_%d" % self.nsem)
                self.nsem += 1
                b.cnt = 0
            return b.sem
        if b.sem is None:
            if self.sempool:
                b.sem, b.cnt = self.sempool.pop()
            else:
                b.sem = self.nc.alloc_semaphore("ds_%d" % self.nsem)
                self.nsem += 1
                b.cnt = 0
            self.dmabufs.append(b)
        return b.sem

    def release(self, bufs):
        for b in bufs:
            if b.sem is not None:
                self.sempool.append((b.sem, b.cnt))
                self.dmabufs.remove(b)
                b.sem = None

    def _wait(self, eng, r):
        e = self.engs[eng]
        if r.dma:
            key = r.sem.num
            val = r.val
            if r.buf.sem is r.sem and r.buf.cnt > val:
                val = r.buf.cnt
            cur = self.wdma[eng].get(key, 0)
            if cur >= val:
                return
            e.wait_ge(r.sem, val)
            self.ninstr[eng] += 1
            self.wdma[eng][key] = val
        else:
            if r.eng == eng and eng in ("pe", "sp"):
                return
            if self.widx[eng][r.eng] >= r.idx:
                return
            self.widx[eng][r.eng] = r.idx
            self.needed.add(r.idx)
            if r.val is None:
                raise RuntimeError("op %d needed but not signalled" % r.idx)
            e.wait_ge(self.esem[r.eng], r.val)
            self.ninstr[eng] += 1

    def _deps(self, eng, reads, writes, partial):
        deps = []
        for b in reads:
            deps.extend(b.writers)
        for b in writes:
            deps.extend(b.writers)
            deps.extend(b.readers)
        for b in partial:
            deps.extend(b.writers)
            deps.extend(b.readers)
            deps.extend(b.old)
        seen = set()
        for d in deps:
            if id(d) in seen:
                continue
            seen.add(id(d))
            self._wait(eng, d)

    def _post(self, rec, reads, writes, partial):
        for b in reads:
            b.readers.append(rec)
            b.old = []
            b.genr = True
        for b in writes:
            b.writers = [rec]
            b.readers = []
            b.old = []
            b.genr = False
        for b in partial:
            if b.genr:
                b.writers = [rec]
                b.genr = False
            else:
                b.writers.append(rec)
            b.old = b.old + b.readers
            b.readers = []

    def op(self, eng, fn, reads=(), writes=(), partial=()):
        self._deps(eng, reads, writes, partial)
        ins = fn(self.engs[eng])
        self.ninstr[eng] += 1
        rec = Rec()
        rec.idx = self.nops
        self.nops += 1
        rec.eng = eng
        rec.dma = False
        rec.sem = None
        rec.ins = ins
        if self.sigset is None or rec.idx in self.sigset:
            self.ecnt[eng] += 1
            ins.then_inc(self.esem[eng], 1)
            rec.val = self.ecnt[eng]
        else:
            rec.val = None
        self.last[eng] = rec
        self._post(rec, reads, writes, partial)
        if eng != "pe":
            self.yp()
        return rec

    def dma(self, q, out, in_, sembuf, reads=(), writes=(), partial=(), nodeps=False, **kw):
        if not nodeps:
            self._deps(q, reads, writes, partial)
        sem = self._bufsem(sembuf)
        ins = self.engs[q].dma_start(out=out, in_=in_, **kw)
        self.ninstr[q] += 1
        sembuf.cnt += 16
        ins.then_inc(sem, 16)
        rec = Rec()
        rec.idx = self.nops
        self.nops += 1
        rec.eng = q
        rec.dma = True
        rec.sem = sem
        rec.val = sembuf.cnt
        rec.buf = sembuf
        rec.ins = ins
        self._post(rec, reads, writes, partial)
        return rec

    def coop_run(self, streams):
        if len(streams) == 1:
            streams[0][1]()
            return
        main = threading.Semaphore(0)
        st = {}
        order = []
        err = []
        for name, fn in streams:
            sem = threading.Semaphore(0)
            d = {"sem": sem, "alive": True}
            st[name] = d
            order.append(name)

            def runner(fn=fn, d=d):
                d["sem"].acquire()
                try:
                    fn()
                except BaseException as ex:
                    err.append(ex)
                d["alive"] = False
                main.release()
            d["th"] = threading.Thread(target=runner)
            d["th"].start()
        self._coop = (st, main)
        while any(st[n]["alive"] for n in order):
            for n in order:
                if st[n]["alive"]:
                    self._cur = n
                    st[n]["sem"].release()
                    main.acquire()
        self._coop = None
        for n in order:
            st[n]["th"].join()
        if err:
            raise err[0]

    def yp(self):
        co = getattr(self, "_coop", None)
        if co is None:
            return
        st, main = co
        me = st[self._cur]
        main.release()
        me["sem"].acquire()

    def coop_wait(self, name):
        co = getattr(self, "_coop", None)
        if co is None:
            return
        st, main = co
        while name in st and st[name]["alive"]:
            self.yp()

    def barrier(self):
        sp = self.engs["sp"]
        for e in COMPUTE:
            if self.last[e] is not None:
                self._wait("sp", self.last[e])
        for b in self.dmabufs:
            if b.cnt > self.wdma["sp"].get(b.sem.num, 0):
                sp.wait_ge(b.sem, b.cnt)
                self.wdma["sp"][b.sem.num] = b.cnt
        ins = sp.nop()
        rec = Rec()
        rec.idx = self.nops
        self.nops += 1
        rec.eng = "sp"
        rec.dma = False
        rec.sem = None
        rec.ins = ins
        self.ecnt["sp"] += 1
        ins.then_inc(self.esem["sp"], 1)
        rec.val = self.ecnt["sp"]
        self.needed.add(rec.idx)
        for e in COMPUTE:
            self.engs[e].wait_ge(self.esem["sp"], rec.val)
            self.widx[e]["sp"] = rec.idx
            for p in COMPUTE:
                if self.last[p] is not None:
                    self.widx[e][p] = max(self.widx[e][p], self.last[p].idx)
            for b in self.dmabufs:
                self.wdma[e][b.sem.num] = b.cnt
        for b in self.dmabufs:
            self.sempool.append((b.sem, b.cnt))
            b.sem = None
        self.dmabufs = []
        return rec

T = 4096
D = 1024
NT = T // 512
KO = D // 128


class Ctx:
    pass


def setup_common(nc, k, es, c):
    c.ps = []
    c.psb = []
    for i in range(8):
        t = es.enter_context(nc.psum_tensor("ps%d" % i, [128, 512], F32))
        c.ps.append(t)
        c.psb.append(Buf("ps%d" % i))
    c.ident = es.enter_context(nc.sbuf_tensor("ident", [128, 128], F32))
    c.identb = Buf("ident")
    c.ones = es.enter_context(nc.sbuf_tensor("ones", [128, 128], F32))
    c.onesb = Buf("ones")
    k.op("pool", lambda e: e.memset(c.ones[:], 1.0), writes=[c.onesb])
    k.op("pool", lambda e: e.affine_select(out=c.ident[:], in_=c.ones[:], pattern=[[-1, 128]],
                                           compare_op=ALU.is_equal, fill=0.0, base=0,
                                           channel_multiplier=1),
         reads=[c.onesb], writes=[c.identb])


def phase_transpose_in(nc, k, c, x, xT):
    with ExitStack() as es:
        xin = [es.enter_context(nc.sbuf_tensor("xin%d" % i, [128, D], F32)) for i in range(2)]
        xinb = [Buf("xin%d" % i) for i in range(2)]
        st = [es.enter_context(nc.sbuf_tensor("xst%d" % i, [128, KO, 512], F32)) for i in range(2)]
        stb = [Buf("xst%d" % i) for i in range(2)]
        pi = 0
        for tt in range(NT):
            s = st[tt % 2]
            sb = stb[tt % 2]
            for j in range(4):
                tb = tt * 4 + j
                xi = xin[tb % 2]
                xib = xinb[tb % 2]
                k.dma("sp", xi[:], x[tb * 128:(tb + 1) * 128, :], xib, writes=[xib])
                for half in range(2):
                    p = c.ps[pi % 8]
                    pb = c.psb[pi % 8]
                    pi += 1
                    for q in range(4):
                        ko = half * 4 + q
                        k.op("pe", lambda e, p=p, xi=xi, ko=ko, q=q: e.transpose(
                            out=p[:, q * 128:(q + 1) * 128], in_=xi[:, ko * 128:(ko + 1) * 128],
                            identity=c.ident[:]),
                            reads=[xib, c.identb], writes=[pb] if q == 0 else (), partial=[pb] if q else ())
                    eng = "dve" if (half == 0 or MODE_IN == 0) else "act"
                    if eng == "dve":
                        k.op("dve", lambda e, p=p, s=s, half=half, j=j: e.tensor_copy(
                            out=s[:, half * 4:(half + 1) * 4, j * 128:(j + 1) * 128],
                            in_=p[:].rearrange("p (q t) -> p q t", q=4)),
                            reads=[pb], partial=[sb])
                    else:
                        k.op("act", lambda e, p=p, s=s, half=half, j=j: e.copy(
                            out=s[:, half * 4:(half + 1) * 4, j * 128:(j + 1) * 128],
                            in_=p[:].rearrange("p (q t) -> p q t", q=4)),
                            reads=[pb], partial=[sb])
            k.dma("sp", xT[:, :, tt * 512:(tt + 1) * 512].rearrange("k p t -> p k t"), s[:], sb, reads=[sb])
    k.barrier()


def phase_transpose_out(nc, k, c, xT, out):
    with ExitStack() as es:
        xin = [es.enter_context(nc.sbuf_tensor("yin%d" % i, [128, KO, 512], F32)) for i in range(2)]
        xinb = [Buf("yin%d" % i) for i in range(2)]
        st = [es.enter_context(nc.sbuf_tensor("yst%d" % i, [128, D], F32)) for i in range(2)]
        stb = [Buf("yst%d" % i) for i in range(2)]
        pi = 0
        for tt in range(NT):
            xi = xin[tt % 2]
            xib = xinb[tt % 2]
            k.dma("sp", xi[:], xT[:, :, tt * 512:(tt + 1) * 512].rearrange("k p t -> p k t"), xib, writes=[xib])
            for j in range(4):
                tb = tt * 4 + j
                s = st[tb % 2]
                sb = stb[tb % 2]
                for half in range(2):
                    p = c.ps[pi % 8]
                    pb = c.psb[pi % 8]
                    pi += 1
                    for q in range(4):
                        ko = half * 4 + q
                        k.op("pe", lambda e, p=p, xi=xi, ko=ko, q=q, j=j: e.transpose(
                            out=p[:, q * 128:(q + 1) * 128], in_=xi[:, ko, j * 128:(j + 1) * 128],
                            identity=c.ident[:]),
                            reads=[xib, c.identb], writes=[pb] if q == 0 else (), partial=[pb] if q else ())
                    if half == 0 or MODE_OUT == 0:
                        k.op("dve", lambda e, p=p, s=s, half=half: e.tensor_copy(
                            out=s[:, half * 512:(half + 1) * 512], in_=p[:]), reads=[pb], partial=[sb])
                    else:
                        k.op("act", lambda e, p=p, s=s, half=half: e.copy(
                            out=s[:, half * 512:(half + 1) * 512], in_=p[:]), reads=[pb], partial=[sb])
                k.dma("sp", out[tb * 128:(tb + 1) * 128, :], s[:], sb, reads=[sb])
    k.barrier()


RW = 512
NZ = 21
NSH = 15
C_WA, C_GD0, C_GD1, C_POOL, C_QLAT, C_KVLAT, C_KROPE = 12, 13, 14, 15, 17, 19, 20
DFF = 2816
DFE = 3584
NE = 8
EPS = 1e-6

PCOL = {}
_off = 0
for _n, _w in [("b_ada", 48), ("g1", 8), ("g2", 8), ("mu", NSH), ("w0", 4), ("a0", 4), ("kk", 4),
               ("ka", 4), ("rk", 4), ("lng", 4), ("lnb", 4), ("v0", 4), ("pscale", 2), ("qlg", 2),
               ("kvlg", 1), ("qkq", 1), ("qkk", 1)]:
    PCOL[_n] = (_off, _w)
    _off += _w
NPCOL = _off


def fm(v, n):
    return np.ascontiguousarray(np.asarray(v, np.float32).reshape(n, 128).T)


def prep_layer(inp, l):
    f32 = np.float32
    d = {}
    P = np.zeros((128, NPCOL), f32)

    def put(name, arr):
        o, w = PCOL[name]
        assert arr.shape == (128, w), (name, arr.shape)
        P[:, o:o + w] = arr
    put("b_ada", fm(inp["b_ada"][l], 48))
    put("g1", fm(inp["norm_gain"][l, 0], 8))
    put("g2", fm(inp["norm_gain"][l, 1], 8))
    w_in = inp["w_in_first"] if l == 0 else inp["w_in_rest"][l - 1]
    mu = inp["mu_shift"][l]
    cols = np.zeros((NZ * 128,), np.int64) - 1
    cols[0:1536] = np.arange(0, 1536)
    cols[1536:1536 + 128] = np.arange(1536, 1664)
    cols[1664:1664 + 128] = np.arange(1664, 1792)
    cols[1792:1792 + 32] = np.arange(1792, 1824)
    muc = np.zeros((NZ * 128,), f32)
    muc[0:1824] = mu
    if l > 0:
        cols[1824:1856] = np.arange(2496, 2528)
        muc[1824:1856] = inp["mu_shift_v"][l - 1]
    cols[1920:1920 + 256] = np.arange(1824, 2080)
    cols[2176:2176 + 256] = np.arange(2080, 2336)
    cols[2432:2432 + 128] = np.arange(2336, 2464)
    kr = 2464
    cols[2560:2560 + 32] = np.arange(kr, kr + 32)
    cols[2592:2592 + 16] = np.arange(kr + 16, kr + 32)
    cols[2608:2608 + 16] = np.arange(kr, kr + 16)
    W = np.zeros((D, NZ * 128), f32)
    m = cols >= 0
    W[:, m] = w_in[:, cols[m]]
    d["win"] = np.ascontiguousarray(W.reshape(8, 128, NZ * 128).transpose(1, 0, 2))
    put("mu", fm(muc[:NSH * 128], NSH))
    rv = inp["rwkv_vec"][l]
    for i, nm in enumerate(["w0", "a0", "kk", "ka", "rk", "lng", "lnb"]):
        put(nm, fm(rv[i], 4))
    if l > 0:
        put("v0", fm(inp["rwkv_v0"][l - 1], 4))
    put("pscale", fm(inp["pool_scale"][l], 2))
    put("qlg", fm(inp["mla_q_lat_gain"][l], 2))
    put("kvlg", fm(inp["mla_kv_lat_gain"][l], 1))
    g = np.zeros((128, 1), f32); g[:96, 0] = inp["mla_qk_gain"][l, 0]; put("qkq", g)
    g = np.zeros((128, 1), f32); g[:96, 0] = inp["mla_qk_gain"][l, 1]; put("qkk", g)
    d["par"] = P
    d["wada"] = np.ascontiguousarray(inp["w_ada"][l].reshape(8, 128, 6144).transpose(1, 0, 2))
    w2a2 = np.concatenate([inp["rwkv_w2"][l], inp["rwkv_a2"][l]], axis=0)
    g2 = inp["rwkv_g2"][l]
    g2b = np.zeros((128, 512), f32)
    g2b[0:32] = g2[128:160]
    if l > 0:
        g2b[32:64] = inp["rwkv_v2"][l - 1]
    d["rw"] = np.ascontiguousarray(np.stack([w2a2, g2[0:128], g2b], axis=1))
    pw = np.zeros((128, 2, 128), f32)
    for gi in range(4):
        pc, o = gi // 2, (gi % 2) * 64
        pw[o:o + 64, pc, o:o + 64] = inp["pool_w"][l, gi]
    d["poolw"] = pw
    wq = inp["mla_wq_up"][l]
    WQ = np.zeros((256, 4, 2, 96), f32)
    for h in range(4):
        WQ[:, h, 0, :] = wq[:, h * 96:(h + 1) * 96]
        WQ[:, h, 1, 64:80] = wq[:, h * 96 + 80:h * 96 + 96]
        WQ[:, h, 1, 80:96] = wq[:, h * 96 + 64:h * 96 + 80]
    d["wq"] = np.ascontiguousarray(WQ.reshape(2, 128, 4 * 2 * 96).transpose(1, 0, 2))
    wkv = inp["mla_wkv_up"][l].reshape(128, 4, 128)
    d["wkv"] = np.ascontiguousarray(np.concatenate(
        [wkv[:, :, :64].reshape(128, 256), wkv[:, :, 64:].reshape(128, 256)], axis=1))
    d["wout"] = np.ascontiguousarray(inp["w_out"][l].reshape(8, 128, 1024).transpose(1, 0, 2))
    return d


LAYER_SHAPES = {"win": [128, 8, NZ * 128], "par": [128, NPCOL], "wada": [128, 8, 6144], "rw": [128, 3, 512],
                "poolw": [128, 2, 128], "wq": [128, 2, 768], "wkv": [128, 512], "wout": [128, 8, 1024]}

_SBN = [0]


def sb(nc, es, name, shape, dt=F32):
    _SBN[0] += 1
    t = es.enter_context(nc.sbuf_tensor("s%d_%s" % (_SBN[0], name), shape, dt))
    return t, Buf(name)


def load_params(nc, k, es, c, L):
    par, parb = sb(nc, es, "par", [128, NPCOL])
    k.dma("sp", par[:], L["par"], parb, writes=[parb])
    c.par, c.parb = par, parb
    mod, modb = sb(nc, es, "mod", [128, 64])
    c.mod, c.modb = mod, modb
    cact, cactb = sb(nc, es, "cact", [128, 8])
    k.op("act", lambda e: e.activation(out=cact[:], in_=c.cT[:], func=AF.Silu), reads=[c.cTb], writes=[cactb])
    with ExitStack() as es2:
        wa = [sb(nc, es2, "wada%d" % i, [128, 8, 512]) for i in range(2)]
        p, pb = c.ps[0], c.psb[0]
        for nt in range(12):
            w, wb = wa[nt % 2]
            k.dma("sp", w[:], L["wada"][:, :, nt * 512:(nt + 1) * 512], wb, writes=[wb])
            for j in range(4):
                col = nt * 4 + j
                for ko in range(8):
                    k.op("pe", lambda e, w=w, j=j, ko=ko, col=col: e.matmul(
                        p[:, col:col + 1], lhsT=w[:, ko, j * 128:(j + 1) * 128], rhs=cact[:, ko:ko + 1],
                        start=(ko == 0), stop=(ko == 7)),
                        reads=[wb, cactb], writes=[pb] if (col == 0 and ko == 0) else (),
                        partial=() if (col == 0 and ko == 0) else [pb])
        o, _ = PCOL["b_ada"]
        k.op("dve", lambda e: e.tensor_tensor(out=mod[:, 0:48], in0=p[:, 0:48], in1=par[:, o:o + 48], op=ALU.add),
             reads=[pb, parb], partial=[modb])
        for (dst, sc, gn) in [(48, 8, "g1"), (56, 32, "g2")]:
            go, _ = PCOL[gn]
            k.op("dve", lambda e, dst=dst, sc=sc, go=go: e.scalar_tensor_tensor(
                out=mod[:, dst:dst + 8], in0=mod[:, sc:sc + 8], scalar=1.0, in1=par[:, go:go + 8],
                op0=ALU.add, op1=ALU.mult), reads=[modb, parb], partial=[modb])
        k.barrier()


def norm_mod(nc, k, c, xt, xtb, tmp, tmpb, sq, sqb, rstd, rstdb, hT, hTb, pbank, acol, bcol, h32=None, h32b=None):
    p, pb = c.ps[pbank], c.psb[pbank]
    k.op("act", lambda e: e.activation(out=sq[:], in_=xt[:], func=AF.Square), reads=[xtb], writes=[sqb])
    for ko in range(8):
        k.op("pe", lambda e, ko=ko: e.matmul(p[:], lhsT=c.onesbf[:], rhs=sq[:, ko, :], start=(ko == 0), stop=(ko == 7)),
             reads=[sqb, c.onesbfb], writes=[pb] if ko == 0 else (), partial=[pb] if ko else ())
    k.op("act", lambda e: e.activation(out=rstd[:], in_=p[:], func=AF.Sqrt, scale=1.0 / D, bias=c.epsc[:, 0:1]),
         reads=[pb, c.epscb], writes=[rstdb])
    k.op("dve", lambda e: e.reciprocal(out=rstd[:], in_=rstd[:]), reads=[rstdb], writes=[rstdb])
    k.op("dve", lambda e: e.tensor_tensor(out=tmp[:], in0=xt[:], in1=rstd[:].unsqueeze(1).to_broadcast([128, 8, 512]),
                                          op=ALU.mult), reads=[xtb, rstdb], writes=[tmpb])
    for ko in range(8):
        k.op("act", lambda e, ko=ko: e.activation(out=hT[:, ko, :], in_=tmp[:, ko, :], func=AF.Identity,
                                                  scale=c.mod[:, acol + ko:acol + ko + 1],
                                                  bias=c.mod[:, bcol + ko:bcol + ko + 1]),
             reads=[tmpb, c.modb], writes=[hTb] if ko == 0 else (), partial=[hTb] if ko else ())
        if h32 is not None:
            k.op("pool", lambda e, ko=ko: e.tensor_scalar(out=h32[:, ko, :], in0=tmp[:, ko, :],
                                                          scalar1=c.mod[:, acol + ko:acol + ko + 1],
                                                          scalar2=c.mod[:, bcol + ko:bcol + ko + 1],
                                                          op0=ALU.mult, op1=ALU.add),
                 reads=[tmpb, c.modb], writes=[h32b] if ko == 0 else (), partial=[h32b] if ko else ())


def phase_inproj(nc, k, c, L, xT, zT, l):
    with ExitStack() as es:
        win, winb = sb(nc, es, "win", [128, 8, NZ * 128], BF16)
        for ko in range(8):
            k.dma("pool", win[:, ko, :], L["win"][:, ko, :], winb, partial=[winb])
        xts = [sb(nc, es, "xt%d" % i, [128, 8, 512]) for i in range(2)]
        tmp, tmpb = sb(nc, es, "tmp", [128, 8, 512])
        sq, sqb = sb(nc, es, "sq", [128, 8, 512], BF16)
        rstd, rstdb = sb(nc, es, "rstd", [128, 512])
        hTs = [sb(nc, es, "hT%d" % i, [128, 8, 512], BF16) for i in range(2)]
        zc, zcb = sb(nc, es, "zc", [128, NSH, 513])
        dd = [sb(nc, es, "dd%d" % i, [128, 512]) for i in range(2)]
        zo = [sb(nc, es, "zo%d" % i, [128, 512]) for i in range(4)]
        k.op("pool", lambda e: e.memset(zc[:], 0.0), writes=[zcb])
        mo, _ = PCOL["mu"]
        zi = [0]

        fuse_in = (l == 0 and getattr(c, "x_ap", None) is not None)
        if fuse_in:
            xins = [sb(nc, es, "xin%d" % i, [128, D]) for i in range(2)]

        def norm_body(tt):
            xt, xtb = xts[tt % 2]
            hT, hTb = hTs[tt % 2]
            if fuse_in:
                for j in range(4):
                    tb = tt * 4 + j
                    xi, xib = xins[tb % 2]
                    k.dma("sp", xi[:], c.x_ap[tb * 128:(tb + 1) * 128, :], xib, writes=[xib])
                    for half in range(2):
                        bi_ = 5 + (j * 2 + half) % 2
                        p, pb = c.ps[bi_], c.psb[bi_]
                        for q_ in range(4):
                            ko = half * 4 + q_
                            k.op("pe", lambda e, xi=xi, ko=ko, q_=q_, p=p: e.transpose(
                                out=p[:, q_ * 128:(q_ + 1) * 128], in_=xi[:, ko * 128:(ko + 1) * 128], identity=c.ident[:]),
                                reads=[xib, c.identb], writes=[pb] if q_ == 0 else (), partial=[pb] if q_ else ())
                        if half == 0:
                            k.op("dve", lambda e, half=half, j=j, p=p: e.tensor_copy(
                                out=xt[:, half * 4:(half + 1) * 4, j * 128:(j + 1) * 128], in_=p[:].rearrange("p (q t) -> p q t", q=4)),
                                reads=[pb], partial=[xtb])
                        else:
                            k.op("act", lambda e, half=half, j=j, p=p: e.copy(
                                out=xt[:, half * 4:(half + 1) * 4, j * 128:(j + 1) * 128], in_=p[:].rearrange("p (q t) -> p q t", q=4)),
                                reads=[pb], partial=[xtb])
                k.dma("sp", xT[:, :, tt * 512:(tt + 1) * 512].rearrange("k p t -> p k t"), xt[:], xtb, reads=[xtb])
            else:
                k.dma("sp", xt[:], xT[:, :, tt * 512:(tt + 1) * 512].rearrange("k p t -> p k t"), xtb, writes=[xtb])
            norm_mod(nc, k, c, xt, xtb, tmp, tmpb, sq, sqb, rstd, rstdb, hT, hTb, 7, 48, 0)

        def mm_body(tt):
            hT, hTb = hTs[tt % 2]
            k.op("pool", lambda e: e.tensor_copy(out=zc[:, :, 0:1], in_=zc[:, :, 512:513]), reads=[zcb], writes=[zcb])
            for nch in range(NZ):
                nb_ = 5 if fuse_in else 6
                p, pb = c.ps[nch % nb_], c.psb[nch % nb_]
                for ko in range(8):
                    k.op("pe", lambda e, p=p, ko=ko, nch=nch: e.matmul(
                        p[:], lhsT=win[:, ko, nch * 128:(nch + 1) * 128], rhs=hT[:, ko, :],
                        start=(ko == 0), stop=(ko == 7)),
                        reads=[winb, hTb], writes=[pb] if ko == 0 else (), partial=[pb] if ko else ())
                z, zb = zo[zi[0] % 4]
                zi[0] += 1
                if nch < NSH:
                    d_, db = dd[nch % 2]
                    k.op("act", lambda e, p=p, nch=nch: e.copy(out=zc[:, nch, 1:513], in_=p[:]), reads=[pb], partial=[zcb])
                    k.op("dve", lambda e, d_=d_, nch=nch: e.tensor_tensor(out=d_[:], in0=zc[:, nch, 0:512], in1=zc[:, nch, 1:513],
                                                                     op=ALU.subtract), reads=[zcb], writes=[db])
                    k.op("dve", lambda e, d_=d_, z=z, nch=nch: e.scalar_tensor_tensor(
                        out=z[:], in0=d_[:], scalar=c.par[:, mo + nch:mo + nch + 1], in1=zc[:, nch, 1:513],
                        op0=ALU.mult, op1=ALU.add), reads=[db, zcb, c.parb], writes=[zb])
                else:
                    k.op("act", lambda e, p=p, z=z: e.copy(out=z[:], in_=p[:]), reads=[pb], writes=[zb])
                k.dma("sp", zT[nch, :, tt * 512:(tt + 1) * 512], z[:], zb, reads=[zb])

        norm_body(0)
        for tt in range(NT):
            streams = [("M", lambda tt=tt: mm_body(tt))]
            if tt + 1 < NT:
                streams.append(("N", lambda tt=tt: norm_body(tt + 1)))
            k.coop_run(streams)
        k.barrier()

C0 = float(np.exp(-0.5))
LNX_EPS = 64e-5


def setup_rwkv_consts(nc, k, es, c):
    c.bones, c.bonesb = sb(nc, es, "bones", [128, 128])
    k.op("pool", lambda e: e.memset(c.bones[:], 0.0), writes=[c.bonesb])
    for h in range(2):
        k.op("pool", lambda e, h=h: e.memset(c.bones[h * 64:(h + 1) * 64, h * 64:(h + 1) * 64], 1.0), partial=[c.bonesb])
    c.ones3, c.ones3b = sb(nc, es, "ones3", [128, 8, 128])
    k.op("pool", lambda e: e.memset(c.ones3[:], 1.0), writes=[c.ones3b])
    c.maskL, c.maskLb = sb(nc, es, "maskL", [128, 8, 64])
    c.maskNM, c.maskNMb = sb(nc, es, "maskNM", [128, 4, 128])
    c.i64, c.i64b = sb(nc, es, "i64", [128, 8, 64])
    for h in range(2):
        ps_ = slice(h * 64, (h + 1) * 64)
        k.op("pool", lambda e, ps_=ps_: e.affine_select(out=c.maskL[ps_], in_=c.ones3[ps_, :, 0:64], pattern=[[0, 8], [-1, 64]],
                                                        compare_op=ALU.is_gt, fill=0.0, base=0, channel_multiplier=1),
             reads=[c.ones3b], partial=[c.maskLb])
        k.op("pool", lambda e, ps_=ps_: e.affine_select(out=c.maskNM[ps_, :, 0:64], in_=c.ones3[ps_, 0:4, 0:64], pattern=[[0, 4], [1, 64]],
                                                        compare_op=ALU.is_gt, fill=0.0, base=0, channel_multiplier=-1),
             reads=[c.ones3b], partial=[c.maskNMb])
        k.op("pool", lambda e, ps_=ps_: e.affine_select(out=c.maskNM[ps_, :, 64:128], in_=c.ones3[ps_, 0:4, 0:64], pattern=[[0, 4], [1, 64]],
                                                        compare_op=ALU.is_ge, fill=0.0, base=0, channel_multiplier=-1),
             reads=[c.ones3b], partial=[c.maskNMb])
        k.op("pool", lambda e, ps_=ps_: e.affine_select(out=c.i64[ps_], in_=c.ones3[ps_, :, 0:64], pattern=[[0, 8], [-1, 64]],
                                                        compare_op=ALU.is_equal, fill=0.0, base=0, channel_multiplier=1),
             reads=[c.ones3b], partial=[c.i64b])


class PsRot:
    def __init__(self, c, banks):
        self.c = c
        self.banks = list(banks)
        self.i = 0

    def get(self):
        b = self.banks[self.i % len(self.banks)]
        self.i += 1
        return self.c.ps[b], self.c.psb[b]


FAST32 = False


def R32(ap):
    return ap.bitcast(mybir.dt.float32r) if FAST32 else ap


def umm(k, p, pb, chunks, width, terms, reads):
    first = True
    for j, cc in enumerate(chunks):
        for h in range(2):
            nt = len(terms)
            for ti, (lf, rf) in enumerate(terms):
                k.op("pe", lambda e, h=h, cc=cc, j=j, lf=lf, rf=rf, ti=ti, nt=nt: e.matmul(
                    p[h * 64:(h + 1) * 64, j * width:(j + 1) * width], lhsT=R32(lf(h, cc)), rhs=R32(rf(h, cc)),
                    start=(ti == 0), stop=(ti == nt - 1)),
                    reads=reads, writes=[pb] if first else (), partial=() if first else [pb])
                first = False


def phase_rwkv(nc, k, c, L, zT, vfT, mixT, l):
    HS = lambda h: slice(h * 64, (h + 1) * 64)
    with ExitStack() as es:
        rw, rwb = sb(nc, es, "rw", [128, 3, 512], BF16)
        k.dma("pool", rw[:], L["rw"], rwb, writes=[rwb])
        par, parb = c.par, c.parb
        pc = lambda nm, i: par[:, PCOL[nm][0] + i:PCOL[nm][0] + i + 1]
        omka, omkab = sb(nc, es, "omka", [128, 4])
        k.op("dve", lambda e: e.tensor_scalar(out=omka[:], in0=par[:, PCOL["ka"][0]:PCOL["ka"][0] + 4], scalar1=-1.0, scalar2=1.0,
                                              op0=ALU.mult, op1=ALU.add), reads=[parb], writes=[omkab])
        lnxe, lnxeb = sb(nc, es, "lnxe", [128, 2])
        k.op("dve", lambda e: e.memset(lnxe[:, 0:1], LNX_EPS), partial=[lnxeb])
        k.op("dve", lambda e: e.memset(lnxe[:, 1:2], 1e-24), partial=[lnxeb])
        T2 = lambda nm, shape=[128, 512], dt=F32: sb(nc, es, nm, shape, dt)
        wa, wab = T2("wa"); gd0, gd0b = T2("gd0"); gd1, gd1b = T2("gd1")
        twa, twab = T2("twa", dt=BF16); sg0, sg0b = T2("sg0", dt=BF16); sg1, sg1b = T2("sg1", dt=BF16)
        rt = [T2("rt%d" % i) for i in range(2)]
        kt = [T2("kt%d" % i) for i in range(2)]
        vt = [T2("vt%d" % i) for i in range(2)]
        vft, vftb = T2("vft")
        sgm, sgmb = T2("sgm"); aa, aab = T2("aa"); kkr, kkrb = T2("kkr"); sq, sqb = T2("sqr")
        rn, rnb = T2("rn"); kk, kkb = T2("kk"); kp, kpb = T2("kp"); bs, bsb = T2("bs"); tA, tAb = T2("tA")
        rk, rkb = T2("rk"); cs, csb = T2("cs"); q, qb = T2("q", [128, 8, 64]); qm, qmb = T2("qm", [128, 8, 64])
        basec, basecb = T2("basec", [128, 8])
        E1, E1b = T2("E1", [128, 8, 64]); E2, E2b = T2("E2", [128, 8, 64]); E3, E3b = T2("E3", [128, 8, 64]); Eh, Ehb = T2("Eh", [128, 8, 64])
        IF = [dict(AR=T2("AR%d" % i, [128, 8, 128]), BtT=T2("BtT%d" % i, [128, 8, 64]), KtT=T2("KtT%d" % i, [128, 8, 64]),
                   Xtok=T2("Xtok%d" % i, [128, 8, 128]), Bhk=T2("Bhk%d" % i, [128, 8, 64]), Khk=T2("Khk%d" % i, [128, 8, 64]),
                   Vtok=T2("Vtok%d" % i, [128, 8, 64]), gam=T2("gam%d" % i, [128, 8])) for i in range(2)]
        BhT, BhTb = T2("BhT", [128, 8, 64]); KhT, KhTb = T2("KhT", [128, 8, 64])
        Lp = [T2("Lp%d" % i, [128, 8, 64]) for i in range(2)]
        Np = [T2("Np%d" % i, [128, 8, 64]) for i in range(2)]
        Pp = [T2("Pp%d" % i, [128, 8, 64]) for i in range(2)]
        NM, NMb = T2("NM", [128, 8, 128]); KM, KMb = T2("KM", [128, 8, 128]); WU, WUb = T2("WU", [128, 8, 128])
        Igam, Igamb = T2("Igam", [128, 8, 64])
        GT, GTb = T2("GT", [128, 4, 8, 64]); HH, HHb = T2("HH", [128, 4, 8, 64])
        R2T, R2Tb = T2("R2T", [128, 4, 8, 64]); Y0T, Y0Tb = T2("Y0T", [128, 4, 8, 64])
        gg, ggb = T2("gg", [128, 2, 4, 512], BF16); bon, bonb = T2("bon", [128, 2, 4, 512], BF16); yT, yTb = T2("yT", [128, 4, 512])
        vp, vpb = T2("vp")
        S, Sb = T2("S", [128, 4, 64])
        dd, ddb = T2("gnd"); yo = [T2("yo%d" % i, dt=BF16) for i in range(2)]
        k.op("pool", lambda e: e.memset(S[:], 0.0), writes=[Sb])
        prP = PsRot(c, [0, 1])
        prC = PsRot(c, [2, 3, 4, 5])
        prS = PsRot(c, [6, 7])

        def P_body(tt, hp):
            ifs = IF[(tt * 4 + hp) % 2]
            AR, ARb = ifs["AR"]; BtT, BtTb = ifs["BtT"]; KtT, KtTb = ifs["KtT"]; Xtok, Xtokb = ifs["Xtok"]
            Bhk, Bhkb = ifs["Bhk"]; Khk, Khkb = ifs["Khk"]; Vtok, Vtokb = ifs["Vtok"]; gam, gamb = ifs["gam"]
            ts_ = slice(tt * 512, (tt + 1) * 512)
            if l == 0 and hp == 0 and c.cast_moe is not None:
                c.cast_moe(tt)
            if hp == 0:
                k.dma("sp", wa[:], zT[C_WA, :, ts_], wab, writes=[wab])
                k.dma("sp", gd0[:], zT[C_GD0, :, ts_], gd0b, writes=[gd0b])
                k.dma("sp", gd1[:], zT[C_GD1, :, ts_], gd1b, writes=[gd1b])
                k.op("act", lambda e: e.activation(out=twa[0:64], in_=wa[0:64], func=AF.Tanh), reads=[wab], writes=[twab])
                k.op("act", lambda e: e.copy(out=twa[64:128], in_=wa[64:128]), reads=[wab], partial=[twab])
                k.op("act", lambda e: e.activation(out=sg0[:], in_=gd0[:], func=AF.Sigmoid), reads=[gd0b], writes=[sg0b])
                k.op("act", lambda e: e.activation(out=sg1[0:32], in_=gd1[0:32], func=AF.Sigmoid), reads=[gd1b], writes=[sg1b])
                k.op("act", lambda e: e.copy(out=sg1[32:64], in_=gd1[32:64]), reads=[gd1b], partial=[sg1b])
            hc = slice(hp * 128, (hp + 1) * 128)
            ii = (tt * 4 + hp) % 2
            r_, rb = rt[ii]; k_, kb = kt[ii]; v_, vb = vt[ii]
            k.dma("sp", r_[:], zT[hp, :, ts_], rb, writes=[rb])
            k.dma("sp", k_[:], zT[4 + hp, :, ts_], kb, writes=[kb])
            k.dma("sp", v_[:], zT[8 + hp, :, ts_], vb, writes=[vb])
            p, pb = prP.get()
            k.op("pe", lambda e, p=p: e.matmul(p[:], lhsT=rw[0:64, 0, hc], rhs=twa[0:64], start=True, stop=True),
                 reads=[rwb, twab], writes=[pb])
            k.op("act", lambda e, p=p: e.activation(out=sgm[:], in_=p[:], func=AF.Sigmoid, bias=pc("w0", hp)),
                 reads=[pb, parb], writes=[sgmb])
            p, pb = prP.get()
            k.op("pe", lambda e, p=p: e.matmul(p[:], lhsT=rw[64:128, 0, hc], rhs=twa[64:128], start=True, stop=True),
                 reads=[rwb, twab], writes=[pb])
            k.op("act", lambda e, p=p: e.activation(out=aa[:], in_=p[:], func=AF.Sigmoid, bias=pc("a0", hp)),
                 reads=[pb, parb], writes=[aab])
            p, pb = prP.get()
            k.op("pe", lambda e, p=p: e.matmul(p[:], lhsT=rw[:, 1, hc], rhs=sg0[:], start=True, stop=False),
                 reads=[rwb, sg0b], writes=[pb])
            k.op("pe", lambda e, p=p: e.matmul(p[:], lhsT=rw[0:32, 2, hc], rhs=sg1[0:32], start=False, stop=True),
                 reads=[rwb, sg1b], partial=[pb])
            k.op("act", lambda e, p=p: e.copy(out=gg[:, tt % 2, hp, :], in_=p[:]), reads=[pb], partial=[ggb])
            if l == 0:
                k.dma("sp", vfT[hp, :, ts_], v_[:], vb, reads=[vb])
                vq, vqb = v_, vb
            else:
                k.dma("sp", vft[:], vfT[hp, :, ts_], vftb, writes=[vftb])
                p, pb = prP.get()
                k.op("pe", lambda e, p=p: e.matmul(p[:], lhsT=rw[32:64, 2, hc], rhs=sg1[32:64], start=True, stop=True),
                     reads=[rwb, sg1b], writes=[pb])
                k.op("act", lambda e, p=p: e.activation(out=vp[:], in_=p[:], func=AF.Sigmoid, bias=pc("v0", hp)),
                     reads=[pb, parb], writes=[vpb])
                k.op("pool", lambda e: e.tensor_tensor(out=vft[:], in0=vft[:], in1=v_[:], op=ALU.subtract),
                     reads=[vftb, vb], writes=[vftb])
                k.op("pool", lambda e: e.tensor_tensor(out=vp[:], in0=vp[:], in1=vft[:], op=ALU.mult),
                     reads=[vpb, vftb], writes=[vpb])
                k.op("pool", lambda e: e.tensor_tensor(out=vp[:], in0=vp[:], in1=v_[:], op=ALU.add),
                     reads=[vpb, vb], writes=[vpb])
                vq, vqb = vp, vpb
            k.op("dve", lambda e: e.tensor_scalar(out=kkr[:], in0=k_[:], scalar1=pc("kk", hp), scalar2=None, op0=ALU.mult),
                 reads=[kb, parb], writes=[kkrb])
            k.op("act", lambda e: e.activation(out=sq[:], in_=kkr[:], func=AF.Square), reads=[kkrb], writes=[sqb])
            p, pb = prP.get()
            k.op("pe", lambda e, p=p: e.matmul(p[:], lhsT=c.bones[:], rhs=sq[:], start=True, stop=True),
                 reads=[c.bonesb, sqb], writes=[pb])
            k.op("act", lambda e, p=p: e.activation(out=rn[:], in_=p[:], func=AF.Sqrt, bias=lnxe[:, 1:2]),
                 reads=[pb, lnxeb], writes=[rnb])
            k.op("dve", lambda e: e.reciprocal(out=rn[:], in_=rn[:]), reads=[rnb], writes=[rnb])
            k.op("dve", lambda e: e.tensor_tensor(out=kk[:], in0=kkr[:], in1=rn[:], op=ALU.mult), reads=[kkrb, rnb], writes=[kkb])
            k.op("dve", lambda e: e.tensor_scalar(out=tA[:], in0=aa[:], scalar1=pc("ka", hp), scalar2=omka[:, hp:hp + 1],
                                                  op0=ALU.mult, op1=ALU.add), reads=[aab, parb, omkab], writes=[tAb])
            k.op("pool", lambda e: e.tensor_tensor(out=kp[:], in0=k_[:], in1=tA[:], op=ALU.mult), reads=[kb, tAb], writes=[kpb])
            k.op("pool", lambda e: e.tensor_tensor(out=bs[:], in0=kk[:], in1=aa[:], op=ALU.mult), reads=[kkb, aab], writes=[bsb])
            k.op("dve", lambda e: e.scalar_tensor_tensor(out=rk[:], in0=r_[:], scalar=pc("rk", hp), in1=kp[:],
                                                         op0=ALU.mult, op1=ALU.mult), reads=[rb, kpb, parb], writes=[rkb])
            p, pb = prP.get()
            k.op("pe", lambda e, p=p: e.matmul(p[:], lhsT=c.bones[:], rhs=rk[:], start=True, stop=True),
                 reads=[c.bonesb, rkb], writes=[pb])
            k.op("dve", lambda e, p=p: e.tensor_tensor(out=bon[:, tt % 2, hp, :], in0=p[:], in1=vq[:], op=ALU.mult),
                 reads=[pb, vqb], partial=[bonb])
            k.op("dve", lambda e: e.tensor_tensor_scan(out=cs[:], data0=c.ones3[:, 0:4, :].rearrange("p a b -> p (a b)"), data1=sgm[:],
                                                       initial=0.0, op0=ALU.mult, op1=ALU.add),
                 reads=[c.ones3b, sgmb], writes=[csb])
            k.op("pool", lambda e: e.memset(basec[:, 0:1], 0.0), partial=[basecb])
            k.op("pool", lambda e: e.tensor_copy(out=basec[:, 1:8], in_=cs[:].rearrange("p (c t) -> p c t", t=64)[:, 0:7, 63]),
                 reads=[csb], partial=[basecb])
            k.op("dve", lambda e: e.tensor_tensor(out=q[:], in0=cs[:].rearrange("p (c t) -> p c t", t=64),
                                                  in1=basec[:].unsqueeze(2).to_broadcast([128, 8, 64]), op=ALU.subtract),
                 reads=[csb, basecb], writes=[qb])
            k.op("pool", lambda e: e.tensor_tensor(out=qm[:], in0=q[:], in1=sgm[:].rearrange("p (c t) -> p c t", t=64), op=ALU.subtract),
                 reads=[qb, sgmb], writes=[qmb])
            k.op("act", lambda e: e.activation(out=E1[:], in_=qm[:], func=AF.Exp, scale=-C0), reads=[qmb], writes=[E1b])
            k.op("act", lambda e: e.activation(out=E2[:], in_=q[:], func=AF.Exp, scale=C0), reads=[qb], writes=[E2b])
            k.op("act", lambda e: e.activation(out=E3[:], in_=q[:], func=AF.Exp, scale=-C0), reads=[qb], writes=[E3b])
            k.op("pool", lambda e: e.tensor_tensor(out=qm[:], in0=q[:], in1=q[:, :, 63:64].to_broadcast([128, 8, 64]), op=ALU.subtract),
                 reads=[qb, E1b], writes=[qmb])
            k.op("act", lambda e: e.activation(out=Eh[:], in_=qm[:], func=AF.Exp, scale=C0), reads=[qmb], writes=[Ehb])
            c3 = lambda t: t[:].rearrange("p (c t) -> p c t", t=64)
            k.op("dve", lambda e: e.scalar_tensor_tensor(out=R32(AR[:, :, 0:64]), in0=c3(kk), scalar=-1.0, in1=E1[:], op0=ALU.mult, op1=ALU.mult),
                 reads=[kkb, E1b], partial=[ARb])
            k.op("pool", lambda e: e.tensor_tensor(out=R32(AR[:, :, 64:128]), in0=c3(r_), in1=E3[:], op=ALU.mult), reads=[rb, E3b], partial=[ARb])
            k.op("dve", lambda e: e.tensor_tensor(out=R32(BtT[:]), in0=c3(bs), in1=E2[:], op=ALU.mult), reads=[bsb, E2b], writes=[BtTb])
            k.op("pool", lambda e: e.tensor_tensor(out=R32(KtT[:]), in0=c3(kp), in1=E2[:], op=ALU.mult), reads=[kpb, E2b], writes=[KtTb])
            k.op("dve", lambda e: e.tensor_tensor(out=BhT[:], in0=c3(bs), in1=Eh[:], op=ALU.mult), reads=[bsb, Ehb], writes=[BhTb])
            k.op("pool", lambda e: e.tensor_tensor(out=KhT[:], in0=c3(kp), in1=Eh[:], op=ALU.mult), reads=[kpb, Ehb], writes=[KhTb])
            for (src, srcb, srcf, dst, dstb, dstf) in [
                    (AR, ARb, lambda cc: AR[:, cc, 0:64], Xtok, Xtokb, lambda: Xtok[:, :, 0:64]),
                    (BhT, BhTb, lambda cc: BhT[:, cc, :], Bhk, Bhkb, lambda: Bhk[:]),
                    (KhT, KhTb, lambda cc: KhT[:, cc, :], Khk, Khkb, lambda: Khk[:]),
                    (vq, vqb, lambda cc: vq[:, cc * 64:(cc + 1) * 64], Vtok, Vtokb, lambda: Vtok[:])]:
                p, pb = prP.get()
                first = True
                for cc in range(8):
                    for h in range(2):
                        k.op("pe", lambda e, p=p, h=h, cc=cc, srcf=srcf: e.matmul(
                            p[HS(h), cc * 64:(cc + 1) * 64], lhsT=srcf(cc)[HS(h)], rhs=c.ident[HS(h), HS(h)], start=True, stop=True),
                            reads=[srcb, c.identb], writes=[pb] if first else (), partial=() if first else [pb])
                        first = False
                k.op("act", lambda e, p=p, dstf=dstf: e.copy(out=R32(dstf()), in_=p[:].rearrange("p (c t) -> p c t", t=64)),
                     reads=[pb], partial=[dstb])
            k.op("pool", lambda e: e.tensor_copy(out=gam[:], in_=E3[:, :, 63]), reads=[E3b], writes=[gamb])

        def C_body(tt, hp):
            ifs = IF[(tt * 4 + hp) % 2]
            AR, ARb = ifs["AR"]; BtT, BtTb = ifs["BtT"]; KtT, KtTb = ifs["KtT"]; Xtok, Xtokb = ifs["Xtok"]
            Bhk, Bhkb = ifs["Bhk"]; Khk, Khkb = ifs["Khk"]; Vtok, Vtokb = ifs["Vtok"]; gam, gamb = ifs["gam"]
            ts_ = slice(tt * 512, (tt + 1) * 512)
            p, pb = prC.get()
            umm(k, p, pb, range(8), 64, [(lambda h, cc: AR[HS(h), cc, 0:64], lambda h, cc: BtT[HS(h), cc, :])], [ARb, BtTb])
            Lc, Lcb = Lp[0]
            k.op("dve", lambda e, p=p: e.tensor_tensor(out=R32(Lc[:]), in0=p[:].rearrange("p (c t) -> p c t", t=64), in1=c.maskL[:], op=ALU.mult),
                 reads=[pb, c.maskLb], writes=[Lcb])
            for (lt, ltb, dst, dstb) in [(BtT, BtTb, NM, NMb), (KtT, KtTb, KM, KMb)]:
                for half in range(2):
                    p, pb = prC.get()
                    umm(k, p, pb, range(half * 4, half * 4 + 4), 128,
                        [(lambda h, cc, lt=lt: lt[HS(h), cc, :], lambda h, cc: AR[HS(h), cc, :])], [ltb, ARb])
                    k.op("dve", lambda e, p=p, dst=dst, half=half: e.tensor_tensor(
                        out=R32(dst[:, half * 4:half * 4 + 4, :]), in0=p[:].rearrange("p (c t) -> p c t", t=128), in1=c.maskNM[:], op=ALU.mult),
                        reads=[pb, c.maskNMb], partial=[dstb])
            Nc, Ncb = Np[0]
            k.op("pool", lambda e: e.tensor_copy(out=R32(Nc[:]), in_=NM[:, :, 0:64]), reads=[NMb], writes=[Ncb])
            Pc, Pcb = Pp[0]
            k.op("pool", lambda e: e.tensor_tensor(out=R32(Pc[:]), in0=NM[:, :, 0:64], in1=c.i64[:], op=ALU.add), reads=[NMb, c.i64b], writes=[Pcb])
            for j in range(5):
                Ln, Lnb = Lp[(j + 1) % 2]
                Nn, Nnb = Np[(j + 1) % 2]
                Pn, Pnb = Pp[(j + 1) % 2]
                p, pb = prC.get()
                umm(k, p, pb, range(8), 64, [(lambda h, cc, Nc=Nc: Nc[HS(h), cc, :], lambda h, cc, Lc=Lc: Lc[HS(h), cc, :])], [Ncb, Lcb])
                if j < 4:
                    p2, p2b = prC.get()
                    umm(k, p2, p2b, range(8), 64, [(lambda h, cc, Lc=Lc: Lc[HS(h), cc, :], lambda h, cc, Nc=Nc: Nc[HS(h), cc, :])], [Ncb, Lcb])
                k.op("act", lambda e, p=p, Ln=Ln: e.copy(out=R32(Ln[:]), in_=p[:].rearrange("p (c t) -> p c t", t=64)), reads=[pb], writes=[Lnb])
                if j < 4:
                    k.op("dve", lambda e, p2=p2, Nn=Nn: e.tensor_copy(out=R32(Nn[:]), in_=p2[:].rearrange("p (c t) -> p c t", t=64)), reads=[p2b], writes=[Nnb])
                p3, p3b = prC.get()
                umm(k, p3, p3b, range(8), 64, [(lambda h, cc, Ln=Ln: Ln[HS(h), cc, :], lambda h, cc, Pc=Pc: Pc[HS(h), cc, :])], [Lnb, Pcb])
                k.op("dve", lambda e, p3=p3, Pn=Pn, Pc=Pc: e.tensor_tensor(out=R32(Pn[:]), in0=p3[:].rearrange("p (c t) -> p c t", t=64), in1=Pc[:], op=ALU.add),
                     reads=[p3b, Pcb], writes=[Pnb])
                Lc, Lcb, Nc, Ncb, Pc, Pcb = Ln, Lnb, Nn, Nnb, Pn, Pnb
            p, pb = prC.get()
            umm(k, p, pb, range(8), 64, [(lambda h, cc: KM[HS(h), cc, 0:64], lambda h, cc: Vtok[HS(h), cc, :])], [KMb, Vtokb])
            k.op("act", lambda e, p=p: e.copy(out=R32(Xtok[:, :, 64:128]), in_=p[:].rearrange("p (c t) -> p c t", t=64)), reads=[pb], partial=[Xtokb])
            for half in range(2):
                p, pb = prC.get()
                umm(k, p, pb, range(half * 4, half * 4 + 4), 128,
                    [(lambda h, cc, Pc=Pc: Pc[HS(h), cc, :], lambda h, cc: Xtok[HS(h), cc, :])], [Pcb, Xtokb])
                eng = "act" if half == 0 else "dve"
                if eng == "act":
                    k.op("act", lambda e, p=p, half=half: e.copy(out=R32(WU[:, half * 4:half * 4 + 4, :]), in_=p[:].rearrange("p (c t) -> p c t", t=128)),
                         reads=[pb], partial=[WUb])
                else:
                    k.op("dve", lambda e, p=p, half=half: e.tensor_copy(out=R32(WU[:, half * 4:half * 4 + 4, :]), in_=p[:].rearrange("p (c t) -> p c t", t=128)),
                         reads=[pb], partial=[WUb])
            k.coop_wait("S")
            p, pb = prC.get()
            umm(k, p, pb, range(8), 64, [(lambda h, cc: WU[HS(h), cc, 0:64], lambda h, cc: NM[HS(h), cc, 64:128])], [WUb, NMb])
            k.op("dve", lambda e, p=p: e.tensor_tensor(out=R2T[:, hp], in0=p[:].rearrange("p (c t) -> p c t", t=64), in1=AR[:, :, 64:128], op=ALU.add),
                 reads=[pb, ARb], partial=[R2Tb])
            p, pb = prC.get()
            umm(k, p, pb, range(8), 64, [(lambda h, cc: WU[HS(h), cc, 64:128], lambda h, cc: NM[HS(h), cc, 64:128]),
                                         (lambda h, cc: Vtok[HS(h), cc, :], lambda h, cc: KM[HS(h), cc, 64:128])], [WUb, NMb, Vtokb, KMb])
            k.op("act", lambda e, p=p: e.copy(out=Y0T[:, hp], in_=p[:].rearrange("p (c t) -> p c t", t=64)), reads=[pb], partial=[Y0Tb])
            p, pb = prC.get()
            umm(k, p, pb, range(8), 64, [(lambda h, cc: WU[HS(h), cc, 0:64], lambda h, cc: Bhk[HS(h), cc, :])], [WUb, Bhkb])
            k.op("pool", lambda e: e.tensor_tensor(out=Igam[:], in0=c.i64[:], in1=gam[:].unsqueeze(2).to_broadcast([128, 8, 64]), op=ALU.mult),
                 reads=[c.i64b, gamb], writes=[Igamb])
            k.op("dve", lambda e, p=p: e.tensor_tensor(out=GT[:, hp], in0=p[:].rearrange("p (c t) -> p c t", t=64), in1=Igam[:], op=ALU.add),
                 reads=[pb, Igamb], partial=[GTb])
            p, pb = prC.get()
            umm(k, p, pb, range(8), 64, [(lambda h, cc: Bhk[HS(h), cc, :], lambda h, cc: WU[HS(h), cc, 64:128]),
                                         (lambda h, cc: Khk[HS(h), cc, :], lambda h, cc: Vtok[HS(h), cc, :])], [Bhkb, WUb, Khkb, Vtokb])
            k.op("act", lambda e, p=p: e.copy(out=HH[:, hp], in_=p[:].rearrange("p (c t) -> p c t", t=64)), reads=[pb], partial=[HHb])

        def S_body(tt):
            ts_ = slice(tt * 512, (tt + 1) * 512)
            for cc in range(8):
                pY, pYb = prS.get()
                pS, pSb = prS.get()
                first = True
                for hp in range(4):
                    for h in range(2):
                        k.op("pe", lambda e, hp=hp, h=h, pY=pY: e.matmul(pY[HS(h), hp * 64:(hp + 1) * 64], lhsT=S[HS(h), hp, :], rhs=R2T[HS(h), hp, cc, :],
                                                                           start=True, stop=True),
                             reads=[Sb, R2Tb], writes=[pYb] if first else (), partial=() if first else [pYb])
                        k.op("pe", lambda e, hp=hp, h=h, pS=pS: e.matmul(pS[HS(h), hp * 64:(hp + 1) * 64], lhsT=GT[HS(h), hp, cc, :], rhs=S[HS(h), hp, :],
                                                                           start=True, stop=True),
                             reads=[Sb, GTb], writes=[pSb] if first else (), partial=() if first else [pSb])
                        first = False
                k.op("dve", lambda e, pY=pY: e.tensor_tensor(out=yT[:, :, cc * 64:(cc + 1) * 64], in0=pY[:, 0:256].rearrange("p (a t) -> p a t", t=64),
                                                              in1=Y0T[:, :, cc, :], op=ALU.add), reads=[pYb, Y0Tb], partial=[yTb])
                k.op("dve", lambda e, pS=pS: e.tensor_tensor(out=S[:], in0=pS[:, 0:256].rearrange("p (a t) -> p a t", t=64),
                                                              in1=HH[:, :, cc, :], op=ALU.add), reads=[pSb, HHb], writes=[Sb])
            for hp in range(4):
                p, pb = prS.get()
                k.op("pe", lambda e, p=p: e.matmul(p[:], lhsT=c.bones[:], rhs=yT[:, hp, :], start=True, stop=True),
                     reads=[c.bonesb, yTb], writes=[pb])
                k.op("dve", lambda e, p=p: e.scalar_tensor_tensor(out=dd[:], in0=p[:], scalar=-1.0 / 64, in1=yT[:, hp, :], op0=ALU.mult, op1=ALU.add),
                     reads=[pb, yTb], writes=[ddb])
                k.op("act", lambda e: e.activation(out=sq[:], in_=dd[:], func=AF.Square), reads=[ddb], writes=[sqb])
                p, pb = prS.get()
                k.op("pe", lambda e, p=p: e.matmul(p[:], lhsT=c.bones[:], rhs=sq[:], start=True, stop=True),
                     reads=[c.bonesb, sqb], writes=[pb])
                k.op("act", lambda e, p=p: e.activation(out=rn[:], in_=p[:], func=AF.Sqrt, scale=1.0 / 64, bias=lnxe[:, 0:1]),
                     reads=[pb, lnxeb], writes=[rnb])
                k.op("dve", lambda e: e.reciprocal(out=rn[:], in_=rn[:]), reads=[rnb], writes=[rnb])
                k.op("dve", lambda e: e.tensor_tensor(out=dd[:], in0=dd[:], in1=rn[:], op=ALU.mult), reads=[ddb, rnb], writes=[ddb])
                k.op("dve", lambda e: e.tensor_scalar(out=dd[:], in0=dd[:], scalar1=pc("lng", hp), scalar2=pc("lnb", hp), op0=ALU.mult, op1=ALU.add),
                     reads=[ddb, parb], writes=[ddb])
                k.op("pool", lambda e: e.tensor_tensor(out=dd[:], in0=dd[:], in1=bon[:, tt % 2, hp, :], op=ALU.add), reads=[ddb, bonb], writes=[ddb])
                y_, yb = yo[hp % 2]
                k.op("pool", lambda e, y_=y_: e.tensor_tensor(out=y_[:], in0=dd[:], in1=gg[:, tt % 2, hp, :], op=ALU.mult), reads=[ddb, ggb], writes=[yb])
                k.dma("sp", mixT[hp, :, ts_], y_[:], yb, reads=[yb])

        its = [(tt, hp) for tt in range(NT) for hp in range(4)]
        P_body(*its[0])
        pendS = None
        for i, (tt, hp) in enumerate(its):
            streams = [("C", lambda tt=tt, hp=hp: C_body(tt, hp))]
            if i + 1 < len(its):
                streams.append(("P", lambda n=its[i + 1]: P_body(*n)))
            if pendS is not None:
                streams.append(("S", lambda t_=pendS: S_body(t_)))
                pendS = None
            k.coop_run(streams)
            if hp == 3:
                pendS = tt
        S_body(pendS)
        k.barrier()

TWO_PI = float(2 * np.pi)
CW1 = 6.28125
CW2 = float(2 * np.pi - 6.28125)


def host_consts():
    f32 = np.float32
    invf = (np.float32(10000.0) ** (-np.arange(0, 32, 2, dtype=np.float32) / np.float32(32))).astype(f32)
    cc = np.zeros((128, 40), f32)
    p = np.arange(128)
    cc[:, 0] = invf[p % 16]
    cc[:, 1] = np.where((p % 32) < 16, -1.0, 1.0)
    wins = {0: (2, 4), 1: (8, 16)}
    for pc in range(2):
        w = np.where(p < 64, wins[pc][0], wins[pc][1]).astype(f32)
        cc[:, 2 + pc] = 1.0 / w
        for t in range(16):
            cc[:, 4 + pc * 16 + t] = w / np.minimum(t + 1, w)
    return cc


def phase_rope_tables(nc, k, c, pos, csT, snT):
    with ExitStack() as es:
        pi_, pib = sb(nc, es, "posi", [128, 1024], I32)
        ang, angb = sb(nc, es, "ang", [128, 1024])
        a2, a2b = sb(nc, es, "ang2", [128, 1024])
        qi, qib = sb(nc, es, "qi", [128, 1024], I32)
        qf, qfb = sb(nc, es, "qf", [128, 1024])
        m, mb = sb(nc, es, "msk", [128, 1024])
        o, ob = sb(nc, es, "tab", [128, 1024])
        halfpi, hpb = sb(nc, es, "halfpi", [128, 2])
        for piece in range(4):
            sl = slice(piece * 1024, (piece + 1) * 1024)
            k.dma("sp", pi_[:], pos[sl].partition_broadcast(128), pib, writes=[pib])
            k.op("dve", lambda e: e.tensor_copy(out=ang[:], in_=pi_[:]), reads=[pib], writes=[angb])
            k.op("dve", lambda e: e.tensor_scalar(out=ang[:], in0=ang[:], scalar1=c.hc[:, 0:1], scalar2=None, op0=ALU.mult),
                 reads=[angb, c.hcb], writes=[angb])
            for which, dst in ((0, snT), (1, csT)):
                if which == 1:
                    k.op("dve", lambda e: e.tensor_scalar(out=a2[:], in0=ang[:], scalar1=float(np.pi / 2), scalar2=None, op0=ALU.add),
                         reads=[angb], writes=[a2b])
                else:
                    k.op("dve", lambda e: e.tensor_copy(out=a2[:], in_=ang[:]), reads=[angb], writes=[a2b])
                k.op("dve", lambda e: e.tensor_scalar(out=qf[:], in0=a2[:], scalar1=float(1.0 / TWO_PI), scalar2=None, op0=ALU.mult),
                     reads=[a2b], writes=[qfb])
                k.op("dve", lambda e: e.tensor_copy(out=qi[:], in_=qf[:]), reads=[qfb], writes=[qib])
                k.op("dve", lambda e: e.tensor_copy(out=qf[:], in_=qi[:]), reads=[qib], writes=[qfb])
                k.op("dve", lambda e: e.scalar_tensor_tensor(out=a2[:], in0=qf[:], scalar=-CW1, in1=a2[:], op0=ALU.mult, op1=ALU.add),
                     reads=[qfb, a2b], writes=[a2b])
                k.op("dve", lambda e: e.scalar_tensor_tensor(out=a2[:], in0=qf[:], scalar=-CW2, in1=a2[:], op0=ALU.mult, op1=ALU.add),
                     reads=[qfb, a2b], writes=[a2b])
                k.op("dve", lambda e: e.tensor_scalar(out=m[:], in0=a2[:], scalar1=float(np.pi), scalar2=-TWO_PI, op0=ALU.is_gt, op1=ALU.mult),
                     reads=[a2b], writes=[mb])
                k.op("dve", lambda e: e.tensor_tensor(out=a2[:], in0=a2[:], in1=m[:], op=ALU.add), reads=[a2b, mb], writes=[a2b])
                k.op("dve", lambda e: e.tensor_scalar(out=m[:], in0=a2[:], scalar1=float(-np.pi), scalar2=TWO_PI, op0=ALU.is_lt, op1=ALU.mult),
                     reads=[a2b], writes=[mb])
                k.op("dve", lambda e: e.tensor_tensor(out=a2[:], in0=a2[:], in1=m[:], op=ALU.add), reads=[a2b, mb], writes=[a2b])
                k.op("dve", lambda e: e.tensor_scalar(out=a2[:], in0=a2[:], scalar1=float(np.pi), scalar2=float(-np.pi), op0=ALU.min, op1=ALU.max),
                     reads=[a2b], writes=[a2b])
                k.op("act", lambda e: e.activation(out=o[:], in_=a2[:], func=AF.Sin), reads=[a2b], writes=[ob])
                if which == 0:
                    k.op("dve", lambda e: e.tensor_scalar(out=o[:], in0=o[:], scalar1=c.hc[:, 1:2], scalar2=None, op0=ALU.mult),
                         reads=[ob, c.hcb], writes=[ob])
                k.dma("sp", dst[:, sl], o[:], ob, reads=[ob])
        k.barrier()


def phase_pool_mla(nc, k, c, L, zT, csT, snT, mixT, l):
    SC = float(96 ** -0.5)
    with ExitStack() as es:
        par, parb = c.par, c.parb
        pc = lambda nm, i: par[:, PCOL[nm][0] + i:PCOL[nm][0] + i + 1]
        T2 = lambda nm, shape=[128, 512], dt=F32: sb(nc, es, nm, shape, dt)
        poolw, poolwb = T2("poolw", [128, 2, 128], BF16)
        k.dma("pool", poolw[:], L["poolw"], poolwb, writes=[poolwb])
        wq, wqb = T2("wq", [128, 2, 768], BF16)
        k.dma("pool", wq[:], L["wq"], wqb, writes=[wqb])
        wkv, wkvb = T2("wkv", [128, 512], BF16)
        k.dma("pool", wkv[:], L["wkv"], wkvb, writes=[wkvb])
        KTbs = [Buf("KTb%d" % i) for i in range(NT)]
        V1bs = [Buf("V1b%d" % i) for i in range(NT)]
        KT, KTb = T2("KT", [128, 4, T], BF16)
        V1, V1b = T2("V1", [128, 32, 4, 65], BF16)
        k.op("pool", lambda e: e.memset(V1[:, :, :, 64:65], 1.0), partial=V1bs)
        tri, trib = T2("tri", [128, 128], BF16)
        k.op("pool", lambda e: e.affine_select(out=tri[:], in_=c.onesbf[:], pattern=[[1, 128]], compare_op=ALU.is_ge, fill=0.0,
                                               base=0, channel_multiplier=-1), reads=[c.onesbfb], writes=[trib])
        ub = [T2("ub%d" % i, [128, 528]) for i in range(2)]
        s2, s2b = T2("s2", [128, 528]); s4, s4b = T2("s4", [128, 528]); s8, s8b = T2("s8", [128, 528])
        pp, ppb = T2("pp"); ppbf, ppbfb = T2("ppbf", dt=BF16)
        yo = [T2("pyo%d" % i, dt=BF16) for i in range(2)]
        ql, qlb = T2("ql", [128, 2, 512]); kvl, kvlb = T2("kvl")
        krA, krAb = T2("krA"); krB, krBb = T2("krB"); cst, cstb = T2("cst"); snt, sntb = T2("snt")
        sqb_, sqbb = T2("msq", [128, 2, 512], BF16); rs, rsb = T2("mrs")
        qn, qnb = T2("qn", [128, 2, 512], BF16); kvn, kvnb = T2("kvn", dt=BF16)
        kr, krb = T2("kr"); t1, t1b = T2("mt1"); t2, t2b = T2("mt2")
        pre, preb = T2("pre"); QTs = [T2("QT%d" % i, [128, 4, 512], BF16) for i in range(2)]
        PT = [T2("PT%d" % i, dt=BF16) for i in range(2)]
        rec, recb = T2("rec", [128, 4]); ytok, ytokb = T2("ytok", [128, 4, 256])
        ymT = [T2("ymT%d" % i, [128, 512], BF16) for i in range(2)]
        for u_, ubb in ub:
            k.op("pool", lambda e, u_=u_: e.memset(u_[:], 0.0), writes=[ubb])
        prm = PsRot(c, [4, 5, 6])
        prl = PsRot(c, [7])
        hcol = lambda i: c.hc[:, i:i + 1]

        def rms_rows(src, srcb, nrows, nko, dim):
            p, pb = prm.get()
            for ko in range(nko):
                s_ap = src[0:nrows, ko, :] if nko > 1 else src[0:nrows]
                q_ap = sqb_[0:nrows, ko, :]
                k.op("act", lambda e, s_ap=s_ap, q_ap=q_ap: e.activation(out=q_ap, in_=s_ap, func=AF.Square), reads=[srcb],
                     writes=[sqbb] if ko == 0 else (), partial=[sqbb] if ko else ())
            for ko in range(nko):
                k.op("pe", lambda e, p=p, ko=ko: e.matmul(p[0:nrows, :], lhsT=c.onesbf[0:nrows, 0:nrows], rhs=sqb_[0:nrows, ko, :],
                                                          start=(ko == 0), stop=(ko == nko - 1)),
                     reads=[sqbb, c.onesbfb], writes=[pb] if ko == 0 else (), partial=[pb] if ko else ())
            k.op("act", lambda e, p=p: e.activation(out=rs[0:nrows], in_=p[0:nrows, :], func=AF.Sqrt, scale=1.0 / dim, bias=c.epsc[0:nrows, 0:1]),
                 reads=[pb, c.epscb], writes=[rsb])
            k.op("dve", lambda e: e.reciprocal(out=rs[0:nrows], in_=rs[0:nrows]), reads=[rsb], writes=[rsb])

        def pool_body(tt):
            ts_ = slice(tt * 512, (tt + 1) * 512)
            for pcn in range(2):
                u_, ubb = ub[pcn]
                if tt > 0:
                    k.op("pool", lambda e, u_=u_: e.tensor_copy(out=u_[:, 0:16], in_=u_[:, 512:528]), reads=[ubb], writes=[ubb])
                k.dma("sp", u_[:, 16:528], zT[C_POOL + pcn, :, ts_], ubb, partial=[ubb], reads=[ubb])
                k.op("pool", lambda e, u_=u_: e.tensor_tensor(out=s2[:, 1:528], in0=u_[:, 1:528], in1=u_[:, 0:527], op=ALU.add), reads=[ubb], writes=[s2b])
                if pcn == 0:
                    k.op("pool", lambda e: e.tensor_tensor(out=s2[64:128, 3:528], in0=s2[64:128, 3:528], in1=s2[64:128, 1:526], op=ALU.add),
                         reads=[s2b], writes=[s2b])
                    res, resb = s2, s2b
                else:
                    k.op("pool", lambda e: e.tensor_tensor(out=s4[:, 3:528], in0=s2[:, 3:528], in1=s2[:, 1:526], op=ALU.add), reads=[s2b], writes=[s4b])
                    k.op("pool", lambda e: e.tensor_tensor(out=s8[:, 7:528], in0=s4[:, 7:528], in1=s4[:, 3:524], op=ALU.add), reads=[s4b], writes=[s8b])
                    k.op("pool", lambda e: e.tensor_tensor(out=s8[64:128, 15:528], in0=s8[64:128, 15:528], in1=s8[64:128, 7:520], op=ALU.add),
                         reads=[s8b], writes=[s8b])
                    res, resb = s8, s8b
                if tt == 0:
                    k.op("dve", lambda e, res=res, pcn=pcn: e.tensor_tensor(out=res[:, 16:32], in0=res[:, 16:32], in1=c.hc[:, 4 + pcn * 16:20 + pcn * 16], op=ALU.mult),
                         reads=[resb, c.hcb], writes=[resb])
                k.op("dve", lambda e, res=res, u_=u_, pcn=pcn: e.scalar_tensor_tensor(out=ppbf[:], in0=res[:, 16:528], scalar=hcol(2 + pcn), in1=u_[:, 16:528],
                                                                               op0=ALU.mult, op1=ALU.subtract), reads=[resb, ubb, c.hcb], writes=[ppbfb])
                p, pb = prl.get()
                k.op("pe", lambda e, p=p, pcn=pcn: e.matmul(p[:], lhsT=poolw[:, pcn, :], rhs=ppbf[:], start=True, stop=True), reads=[poolwb, ppbfb], writes=[pb])
                y_, yb = yo[pcn]
                k.op("act", lambda e, p=p, y_=y_, pcn=pcn: e.activation(out=y_[:], in_=p[:], func=AF.Copy, scale=pc("pscale", pcn)), reads=[pb, parb], writes=[yb])
                k.dma("sp", mixT[4 + pcn, :, ts_], y_[:], yb, reads=[yb])

        def prep_body(tt):
            ts_ = slice(tt * 512, (tt + 1) * 512)
            QT, QTb = QTs[tt % 2]
            k.dma("sp", ql[:], zT[C_QLAT:C_QLAT + 2, :, ts_].rearrange("k p t -> p k t"), qlb, writes=[qlb])
            k.dma("sp", kvl[:], zT[C_KVLAT, :, ts_], kvlb, writes=[kvlb])
            k.dma("sp", krA[64:96], zT[C_KROPE, 0:32, ts_], krAb, writes=[krAb])
            k.dma("sp", krB[64:96], zT[C_KROPE, 32:64, ts_], krBb, writes=[krBb])
            k.dma("sp", cst[:], csT[:, ts_], cstb, writes=[cstb])
            k.dma("sp", snt[:], snT[:, ts_], sntb, writes=[sntb])
            rms_rows(ql, qlb, 128, 2, 256.0)
            for ko in range(2):
                k.op("dve", lambda e, ko=ko: e.scalar_tensor_tensor(out=qn[:, ko, :], in0=ql[:, ko, :], scalar=pc("qlg", ko), in1=rs[:],
                                                                    op0=ALU.mult, op1=ALU.mult), reads=[qlb, rsb, parb],
                     writes=[qnb] if ko == 0 else (), partial=[qnb] if ko else ())
            rms_rows(kvl, kvlb, 128, 1, 128.0) if False else None
            p, pb = prm.get()
            k.op("act", lambda e: e.activation(out=sqb_[:, 0, :], in_=kvl[:], func=AF.Square), reads=[kvlb], writes=[sqbb])
            k.op("pe", lambda e, p=p: e.matmul(p[:], lhsT=c.onesbf[:], rhs=sqb_[:, 0, :], start=True, stop=True), reads=[sqbb, c.onesbfb], writes=[pb])
            k.op("act", lambda e, p=p: e.activation(out=rs[:], in_=p[:], func=AF.Sqrt, scale=1.0 / 128, bias=c.epsc[:, 0:1]), reads=[pb, c.epscb], writes=[rsb])
            k.op("dve", lambda e: e.reciprocal(out=rs[:], in_=rs[:]), reads=[rsb], writes=[rsb])
            k.op("dve", lambda e: e.scalar_tensor_tensor(out=kvn[:], in0=kvl[:], scalar=pc("kvlg", 0), in1=rs[:], op0=ALU.mult, op1=ALU.mult),
                 reads=[kvlb, rsb, parb], writes=[kvnb])
            R_ = slice(64, 96)
            k.op("dve", lambda e: e.tensor_tensor(out=t1[R_], in0=krA[R_], in1=cst[R_], op=ALU.mult), reads=[krAb, cstb], writes=[t1b])
            k.op("dve", lambda e: e.tensor_tensor(out=t2[R_], in0=krB[R_], in1=snt[R_], op=ALU.mult), reads=[krBb, sntb], writes=[t2b])
            k.op("dve", lambda e: e.tensor_tensor(out=kr[R_], in0=t1[R_], in1=t2[R_], op=ALU.add), reads=[t1b, t2b], writes=[krb])
            for j in range(4):
                blk = tt * 4 + j
                p, pb = prm.get()
                k.op("pe", lambda e, p=p, j=j: e.matmul(p[:, 0:256], lhsT=kvn[:, j * 128:(j + 1) * 128], rhs=wkv[:, 256:512], start=True, stop=True),
                     reads=[kvnb, wkvb], writes=[pb])
                k.op("act", lambda e, p=p, blk=blk: e.copy(out=V1[:, blk, :, 0:64], in_=p[:, 0:256].rearrange("p (h d) -> p h d", d=64)),
                     reads=[pb], partial=[V1bs[tt]])
            for h in range(4):
                p, pb = prm.get()
                k.op("pe", lambda e, p=p, h=h: e.matmul(p[0:64, :], lhsT=wkv[:, h * 64:(h + 1) * 64], rhs=kvn[:], start=True, stop=True),
                     reads=[wkvb, kvnb], writes=[pb])
                k.op("act", lambda e, p=p: e.copy(out=pre[0:64], in_=p[0:64, :]), reads=[pb], writes=[preb])
                k.op("pool", lambda e: e.tensor_copy(out=pre[R_], in_=kr[R_]), reads=[krb], partial=[preb])
                p, pb = prm.get()
                k.op("act", lambda e: e.activation(out=sqb_[0:96, 0, :], in_=pre[0:96], func=AF.Square), reads=[preb], writes=[sqbb])
                k.op("pe", lambda e, p=p: e.matmul(p[0:96, :], lhsT=c.onesbf[0:96, 0:96], rhs=sqb_[0:96, 0, :], start=True, stop=True),
                     reads=[sqbb, c.onesbfb], writes=[pb])
                k.op("act", lambda e, p=p: e.activation(out=rs[0:96], in_=p[0:96, :], func=AF.Sqrt, scale=1.0 / 96, bias=c.epsc[0:96, 0:1]),
                     reads=[pb, c.epscb], writes=[rsb])
                k.op("dve", lambda e: e.reciprocal(out=rs[0:96], in_=rs[0:96]), reads=[rsb], writes=[rsb])
                k.op("dve", lambda e, h=h: e.scalar_tensor_tensor(out=KT[0:96, h, ts_], in0=pre[0:96], scalar=par[0:96, PCOL["qkk"][0]:PCOL["qkk"][0] + 1],
                                                                  in1=rs[0:96], op0=ALU.mult, op1=ALU.mult), reads=[preb, rsb, parb], partial=[KTbs[tt]])
                pA, pAb = prm.get()
                pB, pBb = prm.get()
                for ko in range(2):
                    k.op("pe", lambda e, pA=pA, ko=ko, h=h: e.matmul(pA[0:96, :], lhsT=wq[:, ko, (h * 2) * 96:(h * 2 + 1) * 96], rhs=qn[:, ko, :],
                                                                     start=(ko == 0), stop=(ko == 1)),
                         reads=[wqb, qnb], writes=[pAb] if ko == 0 else (), partial=[pAb] if ko else ())
                for ko in range(2):
                    k.op("pe", lambda e, pB=pB, ko=ko, h=h: e.matmul(pB[0:96, :], lhsT=wq[:, ko, (h * 2 + 1) * 96:(h * 2 + 2) * 96], rhs=qn[:, ko, :],
                                                                     start=(ko == 0), stop=(ko == 1)),
                         reads=[wqb, qnb], writes=[pBb] if ko == 0 else (), partial=[pBb] if ko else ())
                k.op("act", lambda e, pA=pA: e.copy(out=pre[0:64], in_=pA[0:64, :]), reads=[pAb], writes=[preb])
                k.op("dve", lambda e, pA=pA: e.tensor_tensor(out=t1[R_], in0=pA[R_, :], in1=cst[R_], op=ALU.mult), reads=[pAb, cstb], writes=[t1b])
                k.op("dve", lambda e, pB=pB: e.tensor_tensor(out=t2[R_], in0=pB[R_, :], in1=snt[R_], op=ALU.mult), reads=[pBb, sntb], writes=[t2b])
                k.op("dve", lambda e: e.tensor_tensor(out=pre[R_], in0=t1[R_], in1=t2[R_], op=ALU.add), reads=[t1b, t2b], partial=[preb])
                p, pb = prm.get()
                k.op("act", lambda e: e.activation(out=sqb_[0:96, 0, :], in_=pre[0:96], func=AF.Square), reads=[preb], writes=[sqbb])
                k.op("pe", lambda e, p=p: e.matmul(p[0:96, :], lhsT=c.onesbf[0:96, 0:96], rhs=sqb_[0:96, 0, :], start=True, stop=True),
                     reads=[sqbb, c.onesbfb], writes=[pb])
                k.op("act", lambda e, p=p: e.activation(out=rs[0:96], in_=p[0:96, :], func=AF.Sqrt, scale=1.0 / 96, bias=c.epsc[0:96, 0:1]),
                     reads=[pb, c.epscb], writes=[rsb])
                k.op("dve", lambda e: e.reciprocal(out=rs[0:96], in_=rs[0:96]), reads=[rsb], writes=[rsb])
                k.op("dve", lambda e: e.tensor_scalar(out=rs[0:96], in0=rs[0:96], scalar1=SC, scalar2=None, op0=ALU.mult), reads=[rsb], writes=[rsb])
                k.op("dve", lambda e, h=h: e.scalar_tensor_tensor(out=QT[0:96, h, :], in0=pre[0:96], scalar=par[0:96, PCOL["qkq"][0]:PCOL["qkq"][0] + 1],
                                                                  in1=rs[0:96], op0=ALU.mult, op1=ALU.mult), reads=[preb, rsb, parb], partial=[QTb])

        def attn_body(tt):
            ts_ = slice(tt * 512, (tt + 1) * 512)
            QT, QTb = QTs[tt % 2]
            for h in range(4):
                po, pob = c.ps[2 + h % 2], c.psb[2 + h % 2]
                nj = 4 * tt + 4
                for j in range(nj):
                    pS, pSb = c.ps[j % 2], c.psb[j % 2]
                    k.op("pe", lambda e, pS=pS, j=j, h=h: e.matmul(pS[:], lhsT=KT[0:96, h, j * 128:(j + 1) * 128], rhs=QT[0:96, h, :], start=True, stop=True),
                         reads=[KTbs[j // 4], QTb], writes=[pSb])
                    P_, Pb_ = PT[j % 2]
                    k.op("act", lambda e, pS=pS, P_=P_: e.activation(out=P_[:], in_=pS[:], func=AF.Exp), reads=[pSb], writes=[Pb_])
                    r = j - 4 * tt
                    if r >= 0:
                        k.op("pool", lambda e, P_=P_, r=r: e.tensor_tensor(out=P_[:, r * 128:(r + 1) * 128], in0=P_[:, r * 128:(r + 1) * 128], in1=tri[:], op=ALU.mult),
                             reads=[Pb_, trib], writes=[Pb_])
                    for i_ in range(max(r, 0), 4):
                        first = (j == 0 and i_ == max(r, 0))
                        k.op("pe", lambda e, P_=P_, i_=i_, j=j, h=h, po=po: e.matmul(po[:, i_ * 65:(i_ + 1) * 65], lhsT=P_[:, i_ * 128:(i_ + 1) * 128], rhs=V1[:, j, h, :],
                                                                                 start=(j == 0 and i_ == 0), stop=(j == 4 * tt + i_), skip_group_check=True),
                             reads=[Pb_, V1bs[j // 4]], writes=[pob] if first else (), partial=() if first else [pob])
                k.op("dve", lambda e, po=po: e.reciprocal(out=rec[:], in_=po[:, 0:260].rearrange("p (i d) -> p i d", d=65)[:, :, 64]), reads=[pob], writes=[recb])
                for i_ in range(4):
                    k.op("dve", lambda e, po=po, i_=i_, h=h: e.tensor_scalar(out=ytok[:, i_, h * 64:(h + 1) * 64], in0=po[:, i_ * 65:i_ * 65 + 64],
                                                                             scalar1=rec[:, i_:i_ + 1], scalar2=None, op0=ALU.mult),
                         reads=[pob, recb], partial=[ytokb])
            for half in range(2):
                p, pb = prm.get()
                for i_ in range(4):
                    k.op("pe", lambda e, p=p, i_=i_, half=half: e.transpose(out=p[:, i_ * 128:(i_ + 1) * 128], in_=ytok[:, i_, half * 128:(half + 1) * 128],
                                                                            identity=c.ident[:]),
                         reads=[ytokb, c.identb], writes=[pb] if i_ == 0 else (), partial=[pb] if i_ else ())
                y_, yb = ymT[half]
                k.op("act", lambda e, p=p, y_=y_: e.copy(out=y_[:], in_=p[:]), reads=[pb], writes=[yb])
                k.dma("sp", mixT[6 + half, :, ts_], y_[:], yb, reads=[yb])

        pool_body(0)
        prep_body(0)
        for tt in range(NT):
            streams = [("A", lambda tt=tt: attn_body(tt))]
            if tt + 1 < NT:
                streams.append(("P", lambda tt=tt: prep_body(tt + 1)))
                streams.append(("L", lambda tt=tt: pool_body(tt + 1)))
            k.coop_run(streams)
        k.barrier()

def phase_ffn(nc, k, c, L, xT, mixT, l, W):
    moe = (l == 1)
    if moe:
        NF = DFE // 128
        experts = [(W["moe_w_in_b"][e], W["moe_w_out_b"][e]) for e in range(NE)]
        F = DFE
    else:
        NF = DFF // 128
        experts = [(W["ffn_w_in_b"], W["ffn_w_out_b"])]
        F = DFF
    NG = NF // 2
    with ExitStack() as es:
        T2 = lambda nm, shape=[128, 512], dt=F32: sb(nc, es, nm, shape, dt)
        wout, woutb = T2("wout", [128, 8, 1024], BF16)
        for ko in range(8):
            k.dma("pool", wout[:, ko, :], L["wout"][:, ko, :], woutb, partial=[woutb])
        x1, x1b = T2("x1", [128, 8, 512]); mixt, mixtb = T2("mixt", [128, 8, 512], BF16)
        tmp, tmpb = T2("ftmp", [128, 8, 512]); sq, sqb = T2("fsq", [128, 8, 512], BF16)
        hT, hTb = T2("fhT", [128, 8, 512], BF16); rstd, rstdb = T2("frstd")
        acc, accb = T2("acc", [128, 8, 512]); aT, aTb = T2("aT", [128, NF, 512], BF16)
        wi = [T2("wi%d" % i, [128, 8, 2, 256], BF16) for i in range(2)]
        wo = [T2("wo%d" % i, [128, NF, 256], BF16) for i in range(2)]
        sg = [T2("sg%d" % i) for i in range(2)]
        ta = [T2("ta%d" % i, dt=BF16) for i in range(2)]
        Gb = [T2("Gb%d" % i, dt=BF16) for i in range(2)]
        if moe:
            rt_, rtb = T2("router", [128, 8, 8])
            k.dma("sp", rt_[:], W["moe_router"].rearrange("(ko p) e -> p ko e", p=128), rtb, writes=[rtb])
            Rp, Rpb = T2("Rp", [128, 8, 8])
            for ko in range(8):
                k.op("dve", lambda e, ko=ko: e.tensor_scalar(out=Rp[:, ko, :], in0=rt_[:, ko, :], scalar1=c.mod[:, 56 + ko:57 + ko], scalar2=None, op0=ALU.mult),
                     reads=[rtb, c.modb], writes=[Rpb] if ko == 0 else (), partial=[Rpb] if ko else ())
            brow, browb = T2("brow", [128, 8])
            p, pb = c.ps[6], c.psb[6]
            for ko in range(8):
                k.op("pe", lambda e, ko=ko: e.matmul(p[0:1, 0:8], lhsT=c.mod[:, 24 + ko:25 + ko], rhs=rt_[:, ko, :], start=(ko == 0), stop=(ko == 7)),
                     reads=[c.modb, rtb], writes=[pb] if ko == 0 else (), partial=[pb] if ko else ())
            k.op("dve", lambda e: e.tensor_copy(out=brow[0:1, :], in_=p[0:1, 0:8]), reads=[pb], writes=[browb])
            sel8, sel8b = T2("sel8", [128, 8, 128])
            k.op("dve", lambda e: e.tensor_copy(out=sel8[0:8], in_=c.ident[0:8, 0:8].unsqueeze(2).to_broadcast([8, 8, 128])), reads=[c.identb], writes=[sel8b])
            Lg, Lgb = T2("Lg", [128, 4, 8]); L2, L2b = T2("L2", [128, 4, 8]); mk, mkb = T2("mk", [128, 4, 8])
            m1, m1b = T2("m1", [128, 4]); m2, m2b = T2("m2", [128, 4]); ex, exb = T2("ex", [128, 4, 8])
            den, denb = T2("den", [128, 4]); Gt, Gtb = T2("Gt", [128, 4, 8]); GT, GTb = T2("GTr", [128, 512])
        wq_i = 0
        woq_i = 0
        ps1 = PsRot(c, [0, 1, 2, 3])
        ps2 = PsRot(c, [4, 5])
        yst = [T2("yst%d" % i, [128, 1024]) for i in range(2)] if (moe and c.out_ap is not None) else None
        for tt in range(NT):
            ts_ = slice(tt * 512, (tt + 1) * 512)
            k.dma("sp", x1[:], xT[:, :, ts_].rearrange("k p t -> p k t"), x1b, writes=[x1b])
            k.dma("sp", mixt[:], mixT[:, :, ts_].rearrange("k p t -> p k t"), mixtb, writes=[mixtb])
            for n in range(8):
                p, pb = ps1.get()
                for ko in range(8):
                    k.op("pe", lambda e, p=p, n=n, ko=ko: e.matmul(p[:], lhsT=wout[:, ko, n * 128:(n + 1) * 128], rhs=mixt[:, ko, :], start=(ko == 0), stop=(ko == 7)),
                         reads=[woutb, mixtb], writes=[pb] if ko == 0 else (), partial=[pb] if ko else ())
                k.op("dve", lambda e, p=p, n=n: e.scalar_tensor_tensor(out=x1[:, n, :], in0=p[:], scalar=c.mod[:, 16 + n:17 + n], in1=x1[:, n, :],
                                                                      op0=ALU.mult, op1=ALU.add), reads=[pb, c.modb, x1b], partial=[x1b])
            norm_mod(nc, k, c, x1, x1b, tmp, tmpb, sq, sqb, rstd, rstdb, hT, hTb, 7, 56, 24)
            if moe:
                p, pb = c.ps[6], c.psb[6]
                first = True
                for blk in range(4):
                    for ko in range(8):
                        k.op("pe", lambda e, blk=blk, ko=ko: e.matmul(p[:, blk * 8:(blk + 1) * 8], lhsT=tmp[:, ko, blk * 128:(blk + 1) * 128], rhs=Rp[:, ko, :],
                                                                      start=(ko == 0), stop=False),
                             reads=[tmpb, Rpb], writes=[pb] if first else (), partial=() if first else [pb])
                        first = False
                    k.op("pe", lambda e, blk=blk: e.matmul(p[:, blk * 8:(blk + 1) * 8], lhsT=c.ones[0:1, 0:128], rhs=brow[0:1, 0:8], start=False, stop=True),
                         reads=[c.onesb, browb], partial=[pb])
                k.op("dve", lambda e: e.tensor_copy(out=Lg[:], in_=p[:, 0:32].rearrange("p (b e) -> p b e", e=8)), reads=[pb], writes=[Lgb])
                k.op("dve", lambda e: e.tensor_reduce(out=m1[:], in_=Lg[:], axis=AX.X, op=ALU.max), reads=[Lgb], writes=[m1b])
                k.op("dve", lambda e: e.tensor_tensor(out=mk[:], in0=Lg[:], in1=m1[:].unsqueeze(2).to_broadcast([128, 4, 8]), op=ALU.is_ge), reads=[Lgb, m1b], writes=[mkb])
                k.op("dve", lambda e: e.scalar_tensor_tensor(out=L2[:], in0=mk[:], scalar=-1e30, in1=Lg[:], op0=ALU.mult, op1=ALU.add), reads=[mkb, Lgb], writes=[L2b])
                k.op("dve", lambda e: e.tensor_reduce(out=m2[:], in_=L2[:], axis=AX.X, op=ALU.max), reads=[L2b], writes=[m2b])
                k.op("dve", lambda e: e.tensor_tensor(out=mk[:], in0=Lg[:], in1=m2[:].unsqueeze(2).to_broadcast([128, 4, 8]), op=ALU.is_ge), reads=[Lgb, m2b], writes=[mkb])
                k.op("dve", lambda e: e.tensor_tensor(out=L2[:], in0=Lg[:], in1=m1[:].unsqueeze(2).to_broadcast([128, 4, 8]), op=ALU.subtract), reads=[Lgb, m1b], writes=[L2b])
                k.op("act", lambda e: e.activation(out=ex[:], in_=L2[:], func=AF.Exp), reads=[L2b], writes=[exb])
                k.op("dve", lambda e: e.tensor_tensor(out=den[:], in0=m2[:], in1=m1[:], op=ALU.subtract), reads=[m1b, m2b], writes=[denb])
                k.op("act", lambda e: e.activation(out=den[:], in_=den[:], func=AF.Exp), reads=[denb], writes=[denb])
                k.op("dve", lambda e: e.tensor_scalar(out=den[:], in0=den[:], scalar1=1.0, scalar2=None, op0=ALU.add), reads=[denb], writes=[denb])
                k.op("dve", lambda e: e.reciprocal(out=den[:], in_=den[:]), reads=[denb], writes=[denb])
                k.op("dve", lambda e: e.tensor_tensor(out=Gt[:], in0=ex[:], in1=mk[:], op=ALU.mult), reads=[exb, mkb], writes=[Gtb])
                k.op("dve", lambda e: e.tensor_tensor(out=Gt[:], in0=Gt[:], in1=den[:].unsqueeze(2).to_broadcast([128, 4, 8]), op=ALU.mult), reads=[Gtb, denb], writes=[Gtb])
                p, pb = c.ps[6], c.psb[6]
                for blk in range(4):
                    k.op("pe", lambda e, blk=blk: e.transpose(out=p[0:8, blk * 128:(blk + 1) * 128], in_=Gt[:, blk, :], identity=c.ident[:]),
                         reads=[Gtb, c.identb], writes=[pb] if blk == 0 else (), partial=[pb] if blk else ())
                k.op("dve", lambda e: e.tensor_copy(out=GT[0:8, :], in_=p[0:8, :]), reads=[pb], writes=[GTb])
            for ei, (w_in, w_out) in enumerate(experts):
                if moe:
                    p, pb = c.ps[6], c.psb[6]
                    k.op("pe", lambda e, ei=ei: e.matmul(p[:], lhsT=sel8[0:8, ei, :], rhs=GT[0:8, :], start=True, stop=True), reads=[sel8b, GTb], writes=[pb])
                    g_, gb_ = Gb[ei % 2]
                    k.op("act", lambda e, g_=g_: e.copy(out=g_[:], in_=p[:]), reads=[pb], writes=[gb_])
                w_in_v = w_in.rearrange("(ko p) n -> p ko n", p=128)
                w_out_v = w_out.rearrange("(f p) n -> p f n", p=128)
                for grp in range(NG):
                    w_, wb_ = wi[wq_i % 2]
                    wq_i += 1
                    f0 = grp * 256
                    k.dma("sp", w_[:, :, 0, :], w_in_v[:, :, f0:f0 + 256], wb_, writes=[wb_], reads=[c.castb1 if moe else c.castb])
                    k.dma("sp", w_[:, :, 1, :], w_in_v[:, :, F + f0:F + f0 + 256], wb_, partial=[wb_])
                    for fc in range(2):
                        f = grp * 2 + fc
                        pG, pGb = ps1.get()
                        pU, pUb = ps1.get()
                        for ko in range(8):
                            k.op("pe", lambda e, pG=pG, w_=w_, ko=ko, fc=fc: e.matmul(pG[:], lhsT=w_[:, ko, 0, fc * 128:(fc + 1) * 128], rhs=hT[:, ko, :],
                                                                                     start=(ko == 0), stop=(ko == 7)),
                                 reads=[wb_, hTb], writes=[pGb] if ko == 0 else (), partial=[pGb] if ko else ())
                        for ko in range(8):
                            k.op("pe", lambda e, pU=pU, w_=w_, ko=ko, fc=fc: e.matmul(pU[:], lhsT=w_[:, ko, 1, fc * 128:(fc + 1) * 128], rhs=hT[:, ko, :],
                                                                                     start=(ko == 0), stop=(ko == 7)),
                                 reads=[wb_, hTb], writes=[pUb] if ko == 0 else (), partial=[pUb] if ko else ())
                        s_, sb_ = sg[f % 2]
                        k.op("act", lambda e, pG=pG, s_=s_: e.activation(out=s_[:], in_=pG[:], func=AF.Silu), reads=[pGb], writes=[sb_])
                        if moe:
                            t_, tb_ = ta[f % 2]
                            k.op("dve", lambda e, pU=pU, s_=s_, t_=t_: e.tensor_tensor(out=t_[:], in0=pU[:], in1=s_[:], op=ALU.mult), reads=[pUb, sb_], writes=[tb_])
                            k.op("pool", lambda e, t_=t_, g_=g_, f=f: e.tensor_tensor(out=aT[:, f, :], in0=t_[:], in1=g_[:], op=ALU.mult), reads=[tb_, gb_], partial=[aTb])
                        else:
                            k.op("dve", lambda e, pU=pU, s_=s_, f=f: e.tensor_tensor(out=aT[:, f, :], in0=pU[:], in1=s_[:], op=ALU.mult), reads=[pUb, sb_], partial=[aTb])
                for qn_ in range(4):
                    w_, wb_ = wo[woq_i % 2]
                    woq_i += 1
                    k.dma("sp", w_[:], w_out_v[:, :, qn_ * 256:(qn_ + 1) * 256], wb_, writes=[wb_], reads=[c.castb1 if moe else c.castb])
                    for nn in range(2):
                        n = qn_ * 2 + nn
                        p, pb = ps2.get()
                        for f in range(NF):
                            k.op("pe", lambda e, p=p, w_=w_, f=f, nn=nn: e.matmul(p[:], lhsT=w_[:, f, nn * 128:(nn + 1) * 128], rhs=aT[:, f, :],
                                                                                 start=(f == 0), stop=(f == NF - 1)),
                                 reads=[wb_, aTb], writes=[pb] if f == 0 else (), partial=[pb] if f else ())
                        if ei == 0:
                            k.op("act", lambda e, p=p, n=n: e.copy(out=acc[:, n, :], in_=p[:]), reads=[pb], partial=[accb])
                        else:
                            k.op("dve", lambda e, p=p, n=n: e.tensor_tensor(out=acc[:, n, :], in0=p[:], in1=acc[:, n, :], op=ALU.add), reads=[pb, accb], partial=[accb])
            for n in range(8):
                k.op("dve", lambda e, n=n: e.scalar_tensor_tensor(out=acc[:, n, :], in0=acc[:, n, :], scalar=c.mod[:, 40 + n:41 + n], in1=x1[:, n, :],
                                                                 op0=ALU.mult, op1=ALU.add), reads=[accb, c.modb, x1b], partial=[accb])
            if c.out_ap is None or not moe:
                k.dma("sp", xT[:, :, ts_].rearrange("k p t -> p k t"), acc[:], accb, reads=[accb])
            else:
                for j in range(4):
                    tb = tt * 4 + j
                    s_, sb_ = yst[tb % 2]
                    for half in range(2):
                        p, pb = ps1.get()
                        for q_ in range(4):
                            ko = half * 4 + q_
                            k.op("pe", lambda e, p=p, ko=ko, q_=q_, j=j: e.transpose(
                                out=p[:, q_ * 128:(q_ + 1) * 128], in_=acc[:, ko, j * 128:(j + 1) * 128], identity=c.ident[:]),
                                reads=[accb, c.identb], writes=[pb] if q_ == 0 else (), partial=[pb] if q_ else ())
                        if half == 0:
                            k.op("dve", lambda e, p=p, s_=s_, half=half: e.tensor_copy(out=s_[:, half * 512:(half + 1) * 512], in_=p[:]),
                                 reads=[pb], partial=[sb_])
                        else:
                            k.op("act", lambda e, p=p, s_=s_, half=half: e.copy(out=s_[:, half * 512:(half + 1) * 512], in_=p[:]),
                                 reads=[pb], partial=[sb_])
                    k.dma("sp", c.out_ap[tb * 128:(tb + 1) * 128, :], s_[:], sb_, reads=[sb_])
        k.barrier()

def declare_inputs(nc, dbg):
    A = {}
    A["x"] = nc.dram_tensor("x", [T, D], F32, kind="ExternalInput").ap()
    A["cT"] = nc.dram_tensor("cT", [128, 8], F32, kind="ExternalInput").ap()
    A["pos"] = nc.dram_tensor("pos", [T], I32, kind="ExternalInput").ap()
    A["hc"] = nc.dram_tensor("hc", [128, 40], F32, kind="ExternalInput").ap()
    A["W"] = {}
    for nm, shp in [("ffn_w_in", [D, 2 * DFF]), ("ffn_w_out", [DFF, D]), ("moe_router", [D, NE]),
                    ("moe_w_in", [NE, D, 2 * DFE]), ("moe_w_out", [NE, DFE, D])]:
        A["W"][nm] = nc.dram_tensor(nm, shp, F32, kind="ExternalInput").ap()
    A["L"] = []
    for l in range(2):
        Ld = {}
        for nm, shp in LAYER_SHAPES.items():
            Ld[nm] = nc.dram_tensor("%s%d" % (nm, l), shp, F32, kind="ExternalInput").ap()
        A["L"].append(Ld)
    return A


def scratch(nc, name, shape, dt, dbg):
    kind = "ExternalOutput" if (dbg and name in dbg) else "Internal"
    return nc.dram_tensor(name, shape, dt, kind=kind).ap()


def build(nc, sigset, dbg=None, stop=None):
    k = K(nc, sigset)
    c = Ctx()
    A = declare_inputs(nc, dbg)
    out = nc.dram_tensor("out", [T, D], F32, kind="ExternalOutput").ap()
    xT = scratch(nc, "xT", [KO, 128, T], F32, dbg)
    c.out_ap = out if stop is None else None
    zT = scratch(nc, "zT", [NZ, 128, T], F32, dbg)
    vfT = scratch(nc, "vfT", [4, 128, T], F32, dbg)
    mixT = scratch(nc, "mixT", [8, 128, T], BF16, dbg)
    csT = scratch(nc, "csT", [128, T], F32, dbg)
    c.dbgK = scratch(nc, "dbgK", [128, 4, T], BF16, dbg) if (dbg and "dbgK" in dbg) else None
    c.dbgQ = scratch(nc, "dbgQ", [128, 4, T], BF16, dbg) if (dbg and "dbgK" in dbg) else None
    snT = scratch(nc, "snT", [128, T], F32, dbg)
    with ExitStack() as es:
        setup_common(nc, k, es, c)
        c.cT, c.cTb = sb(nc, es, "cT", [128, 8])
        k.dma("sp", c.cT[:], A["cT"], c.cTb, writes=[c.cTb])
        c.onesbf, c.onesbfb = sb(nc, es, "onesbf", [128, 128], BF16)
        k.op("dve", lambda e: e.tensor_copy(out=c.onesbf[:], in_=c.ones[:]), reads=[c.onesb], writes=[c.onesbfb])
        c.epsc, c.epscb = sb(nc, es, "epsc", [128, 4])
        k.op("dve", lambda e: e.memset(c.epsc[:, 0:1], EPS), partial=[c.epscb])
        setup_rwkv_consts(nc, k, es, c)
        c.hc, c.hcb = sb(nc, es, "hc", [128, 40])
        k.dma("sp", c.hc[:], A["hc"], c.hcb, writes=[c.hcb])
        k.op("dve", lambda e: e.memset(c.epsc[:, 1:2], 0.0), partial=[c.epscb])
        c.castb = Buf("cast")
        c.castb.late = True
        c.castb1 = Buf("cast1")
        c.castb1.late = True
        W = A["W"]
        W["ffn_w_in_b"] = nc.dram_tensor("ffn_w_in_b", [D, 2 * DFF], BF16).ap()
        W["ffn_w_out_b"] = nc.dram_tensor("ffn_w_out_b", [DFF, D], BF16).ap()
        W["moe_w_in_b"] = nc.dram_tensor("moe_w_in_b", [NE, D, 2 * DFE], BF16).ap()
        W["moe_w_out_b"] = nc.dram_tensor("moe_w_out_b", [NE, DFE, D], BF16).ap()
        k.dma("pool", W["ffn_w_in_b"], W["ffn_w_in"], c.castb, partial=[c.castb], nodeps=True)
        k.dma("pool", W["ffn_w_out_b"], W["ffn_w_out"], c.castb, partial=[c.castb], nodeps=True)
        phase_rope_tables(nc, k, c, A["pos"], csT, snT)
        if stop is None:
            c.x_ap = A["x"]
        else:
            c.x_ap = None
            phase_transpose_in(nc, k, c, A["x"], xT)
        for l in range(2):
            with ExitStack() as esl:
                load_params(nc, k, esl, c, A["L"][l])
                phase_inproj(nc, k, c, A["L"][l], xT, zT, l)
                if stop == "inproj%d" % l:
                    break
                c.cast_moe = None
                if not (dbg and "skiprwkv" in dbg):
                    phase_rwkv(nc, k, c, A["L"][l], zT, vfT, mixT, l)
                if stop == "rwkv%d" % l:
                    break
                phase_pool_mla(nc, k, c, A["L"][l], zT, csT, snT, mixT, l)
                if stop == "mla%d" % l:
                    break
                if l == 0:
                    for e_ in range(NE):
                        k.dma("pool", W["moe_w_in_b"][e_], W["moe_w_in"][e_], c.castb1, partial=[c.castb1], nodeps=True)
                        k.dma("pool", W["moe_w_out_b"][e_], W["moe_w_out"][e_], c.castb1, partial=[c.castb1], nodeps=True)
                phase_ffn(nc, k, c, A["L"][l], xT, mixT, l, A["W"])
                if stop == "ffn%d" % l:
                    break
        if stop is not None:
            phase_transpose_out(nc, k, c, xT, out)
    return k


ALLSIG = False
MODE_IN = 1
MODE_OUT = 1


def make_nc(dbg=None, stop=None):
    nc0 = bass.Bass("TRN2", target_bir_lowering=False)
    k0 = build(nc0, None, dbg, stop)
    nc = bass.Bass("TRN2", target_bir_lowering=False)
    k = build(nc, (None if ALLSIG else k0.needed), dbg, stop)
    return nc, k


def host_prep(inputs):
    shared = {}
    for l in range(2):
        d = prep_layer(inputs, l)
        for nm, v in d.items():
            shared["%s%d" % (nm, l)] = v
    x = np.ascontiguousarray(inputs["x"], dtype=np.float32)
    c = np.asarray(inputs["c"], np.float32)
    pos = np.asarray(inputs["positions"], np.int32)
    shared["hc"] = host_consts()
    shared["ffn_w_in"] = np.ascontiguousarray(inputs["ffn_w_in"][0], dtype=np.float32)
    shared["ffn_w_out"] = np.ascontiguousarray(inputs["ffn_w_out"][0], dtype=np.float32)
    shared["moe_router"] = np.ascontiguousarray(inputs["moe_router"][0], dtype=np.float32)
    shared["moe_w_in"] = np.ascontiguousarray(inputs["moe_w_in"][0], dtype=np.float32)
    shared["moe_w_out"] = np.ascontiguousarray(inputs["moe_w_out"][0], dtype=np.float32)
    maps = []
    for b in range(8):
        m = dict(shared)
        m["x"] = x[b]
        m["cT"] = np.ascontiguousarray(c[b].reshape(8, 128).T)
        m["pos"] = np.ascontiguousarray(pos[b])
        maps.append(m)
    return maps


def kernel(**inputs):
    nc, k = make_nc()
    in_maps = host_prep(inputs)
    res = run_bass_kernel_spmd(nc, in_maps, core_ids=list(range(8)))
    return np.stack([r["out"] for r in res.results], axis=0)
```
